# Optimizing a Trainium2 kernel written in Bass

```python
import jax, jax.numpy as jnp
from jax import lax
import numpy as np

D_MODEL = 1024
BATCH = 8
SEQ = 8192
DEPTH = 1

CHUNK = 64

RWKV_WIDTH = D_MODEL // 2
RWKV_HEAD = 64
RWKV_HEADS = RWKV_WIDTH // RWKV_HEAD
DECAY_LORA = 64
AAA_LORA = 64
GATE_LORA = 128
GMLP_WIDTH = D_MODEL // 2
GMLP_BLOCK = 128
GMLP_GROUPS = 8
GMLP_GROUP_DIM = GMLP_WIDTH // GMLP_GROUPS
N_BRANCH = 2
RWKV_COLS = 3 * RWKV_WIDTH + DECAY_LORA + AAA_LORA + GATE_LORA
IN_COLS = RWKV_COLS + 2 * GMLP_WIDTH + N_BRANCH * D_MODEL
N_EXPERTS = 256
TOP_K = 8
N_EXPERT_GROUPS = 8
TOPK_GROUPS = 4
EXPERT_FF = D_MODEL // 4
SHARED_FF = D_MODEL // 4
ROUTED_SCALE = 2.5
DISPATCH_BLOCK = 128
RMS_EPS = 1e-6
LN_EPS = 1e-5
GN_EPS = 64e-5

kernel_name = "hybrid_rwkv7_gmlp_moe_adaln"


def rmsnorm(x, g):
    xf = x.astype(jnp.float32)
    y = xf * lax.rsqrt(jnp.mean(xf * xf, axis=-1, keepdims=True) + RMS_EPS)
    return (y * g.astype(jnp.float32)).astype(x.dtype)


def layernorm(x, g, b, eps):
    xf = x.astype(jnp.float32)
    mu = jnp.mean(xf, axis=-1, keepdims=True)
    var = jnp.mean(jnp.square(xf - mu), axis=-1, keepdims=True)
    return (xf - mu) * lax.rsqrt(var + eps) * g.astype(jnp.float32) + b.astype(jnp.float32)


def token_shift(p, mu):
    prev = jnp.pad(p, ((0, 0), (1, 0), (0, 0)))[:, :-1]
    return p + (prev - p) * mu


def rwkv7_scan(r, w, k, v, a, b):
    bsz, _, nh, n = r.shape

    def step(state, inp):
        r_t, w_t, k_t, v_t, a_t, b_t = inp
        sa = jnp.einsum('bhvk,bhk->bhv', state, a_t)
        state = (state * w_t[:, :, None, :] + sa[..., None] * b_t[:, :, None, :]
                 + v_t[..., None] * k_t[:, :, None, :])
        y = jnp.einsum('bhvk,bhk->bhv', state, r_t)
        return state, y

    s0 = jnp.zeros((bsz, nh, n, n), jnp.float32)
    xs = tuple(jnp.swapaxes(t, 0, 1) for t in (r, w, k, v, a, b))
    _, y = lax.scan(step, s0, xs)
    return jnp.swapaxes(y, 0, 1)


def rwkv7_mixer(p, w0, w_up, a0, a_up, g_up, k_k, k_a, r_k, ln_g, ln_b):
    p = p.astype(jnp.float32)
    bsz, s, _ = p.shape
    r, k, v, xw, xa, xg = jnp.split(
        p, [RWKV_WIDTH, 2 * RWKV_WIDTH, 3 * RWKV_WIDTH, 3 * RWKV_WIDTH + DECAY_LORA,
            3 * RWKV_WIDTH + DECAY_LORA + AAA_LORA], axis=-1)
    f32 = lambda t: t.astype(jnp.float32)
    w = -jax.nn.softplus(-(f32(w0) + jnp.tanh(xw) @ f32(w_up))) - 0.5
    decay = jnp.exp(-jnp.exp(w))
    a = jax.nn.sigmoid(f32(a0) + xa @ f32(a_up))
    g = jax.nn.sigmoid(xg) @ f32(g_up)
    hs = lambda t: t.reshape(bsz, s, RWKV_HEADS, RWKV_HEAD)
    kk = hs(k * f32(k_k))
    kk = kk / jnp.maximum(jnp.sqrt(jnp.sum(kk * kk, axis=-1, keepdims=True)), 1e-12)
    k = k * (1.0 + (a - 1.0) * f32(k_a))
    rh, kh, vh, ah = hs(r), hs(k), hs(v), hs(a)
    y = rwkv7_scan(rh, hs(decay), kh, vh, -kk, kk * ah)
    mu = jnp.mean(y, axis=-1, keepdims=True)
    var = jnp.mean(jnp.square(y - mu), axis=-1, keepdims=True)
    y = ((y - mu) * lax.rsqrt(var + GN_EPS)).reshape(bsz, s, RWKV_WIDTH)
    y = y * f32(ln_g) + f32(ln_b)
    bonus = jnp.sum(rh * kh * f32(r_k), axis=-1, keepdims=True) * vh
    y = y + bonus.reshape(bsz, s, RWKV_WIDTH)
    return y * g


def gmlp_mixer(u, v, ln_g, ln_b, ws, bs):
    bsz, s, _ = v.shape
    u = jax.nn.gelu(u.astype(jnp.float32))
    v = layernorm(jax.nn.gelu(v.astype(jnp.float32)), ln_g, ln_b, LN_EPS)
    nb = s // GMLP_BLOCK
    vb = v.reshape(bsz, nb, GMLP_BLOCK, GMLP_GROUPS, GMLP_GROUP_DIM)
    mask = jnp.tril(jnp.ones((GMLP_BLOCK, GMLP_BLOCK), dtype=bool))
    wsm = jnp.where(mask[None], ws.astype(jnp.float32), 0.0)
    sv = jnp.einsum('gij,bnjgc->bnigc', wsm, vb) + bs.astype(jnp.float32).T[None, None, :, :, None]
    return u * sv.reshape(bsz, s, GMLP_WIDTH)


def moe(h, router_w, router_bias, w1, w3, w2, sw1, sw3, sw2):
    bsz, s, d = h.shape
    t = bsz * s
    hf = h.reshape(t, d)
    scores = jax.nn.sigmoid(hf.astype(jnp.float32) @ router_w.astype(jnp.float32))
    sel = scores + router_bias.astype(jnp.float32)
    grp = sel.reshape(t, N_EXPERT_GROUPS, N_EXPERTS // N_EXPERT_GROUPS)
    grp_score = jnp.sum(lax.top_k(grp, 2)[0], axis=-1)
    _, gidx = lax.top_k(grp_score, TOPK_GROUPS)
    gmask = jnp.any(gidx[..., None] == jnp.arange(N_EXPERT_GROUPS)[None, None, :], axis=1)
    emask = jnp.repeat(gmask, N_EXPERTS // N_EXPERT_GROUPS, axis=-1)
    _, eidx = lax.top_k(jnp.where(emask, sel, -jnp.inf), TOP_K)
    wts = jnp.take_along_axis(scores, eidx, axis=-1)
    wts = wts / jnp.sum(wts, axis=-1, keepdims=True) * ROUTED_SCALE

    n_assign = t * TOP_K
    flat_e = eidx.reshape(n_assign)
    flat_tok = jnp.arange(n_assign, dtype=jnp.int32) // TOP_K
    flat_w = wts.reshape(n_assign)
    order = jnp.argsort(flat_e)
    se, stok, sw = flat_e[order], flat_tok[order], flat_w[order]
    counts = jnp.bincount(flat_e, length=N_EXPERTS)
    starts = jnp.cumsum(counts) - counts
    padded = (counts + DISPATCH_BLOCK - 1) // DISPATCH_BLOCK * DISPATCH_BLOCK
    pends = jnp.cumsum(padded)
    pstarts = pends - padded
    dest = pstarts[se] + jnp.arange(n_assign, dtype=jnp.int32) - starts[se]
    n_rows = n_assign + N_EXPERTS * DISPATCH_BLOCK
    n_blocks = n_rows // DISPATCH_BLOCK
    row_tok = jnp.zeros((n_rows,), jnp.int32).at[dest].set(stok)
    row_w = jnp.zeros((n_rows,), jnp.float32).at[dest].set(sw)
    block_start = jnp.arange(n_blocks, dtype=jnp.int32) * DISPATCH_BLOCK
    block_e = jnp.clip(jnp.searchsorted(pends, block_start, side='right'), 0, N_EXPERTS - 1)

    def block_step(acc, inp):
        tok, wt, e = inp
        xb = hf[tok]
        hid = jax.nn.silu(xb @ w1[e]) * (xb @ w3[e])
        yb = hid @ w2[e]
        acc = acc.at[tok].add((yb * wt[:, None]).astype(jnp.float32))
        return acc, None

    acc0 = jnp.zeros((t, d), jnp.float32)
    routed, _ = lax.scan(block_step, acc0,
                         (row_tok.reshape(n_blocks, DISPATCH_BLOCK),
                          row_w.reshape(n_blocks, DISPATCH_BLOCK), block_e))
    shared = (jax.nn.silu(hf @ sw1) * (hf @ sw3)) @ sw2
    return (routed + shared.astype(jnp.float32)).reshape(bsz, s, d)


def setup_inputs(seed: int = 0) -> dict:
    key = jax.random.key(seed)
    ks = jax.random.split(key, 40)
    L, D = DEPTH, D_MODEL
    nrm = lambda k, shape, scale: jax.random.normal(k, shape, jnp.float32) * scale
    gain = lambda k, shape: 1.0 + nrm(k, shape, 0.02)
    return {
        "x": nrm(ks[0], (BATCH, SEQ, D), 1.0),
        "c": nrm(ks[1], (BATCH, D), 1.0),
        "ada_w": nrm(ks[2], (L, D, 6 * D), 0.3 * D ** -0.5),
        "ada_b": nrm(ks[3], (L, 6 * D), 0.02),
        "norm1_g": gain(ks[4], (L, D)),
        "norm2_g": gain(ks[5], (L, D)),
        "w_in": nrm(ks[6], (L, D, IN_COLS), D ** -0.5),
        "tshift_mu": jax.random.uniform(ks[7], (L, RWKV_COLS), jnp.float32),
        "rwkv_w0": jax.random.uniform(ks[8], (L, RWKV_WIDTH), jnp.float32, -6.0, -1.0),
        "rwkv_w_up": nrm(ks[9], (L, DECAY_LORA, RWKV_WIDTH), 0.3 * DECAY_LORA ** -0.5),
        "rwkv_a0": nrm(ks[10], (L, RWKV_WIDTH), 0.1),
        "rwkv_a_up": nrm(ks[11], (L, AAA_LORA, RWKV_WIDTH), 0.5 * AAA_LORA ** -0.5),
        "rwkv_g_up": nrm(ks[12], (L, GATE_LORA, RWKV_WIDTH), GATE_LORA ** -0.5),
        "rwkv_k_k": 0.85 + nrm(ks[13], (L, RWKV_WIDTH), 0.02),
        "rwkv_k_a": gain(ks[14], (L, RWKV_WIDTH)),
        "rwkv_r_k": nrm(ks[15], (L, RWKV_HEADS, RWKV_HEAD), 0.1),
        "rwkv_ln_g": gain(ks[16], (L, RWKV_WIDTH)),
        "rwkv_ln_b": nrm(ks[17], (L, RWKV_WIDTH), 0.02),
        "gmlp_ln_g": gain(ks[18], (L, GMLP_WIDTH)),
        "gmlp_ln_b": nrm(ks[19], (L, GMLP_WIDTH), 0.02),
        "gmlp_ws": nrm(ks[20], (L, GMLP_GROUPS, GMLP_BLOCK, GMLP_BLOCK), GMLP_BLOCK ** -0.5),
        "gmlp_bs": 1.0 + nrm(ks[21], (L, GMLP_GROUPS, GMLP_BLOCK), 0.1),
        "w_out_a": nrm(ks[22], (L, RWKV_WIDTH, D), RWKV_WIDTH ** -0.5),
        "w_out_b": nrm(ks[23], (L, GMLP_WIDTH, D), GMLP_WIDTH ** -0.5),
        "w_out": nrm(ks[24], (L, D, D), D ** -0.5),
        "router_w": nrm(ks[25], (L, D, N_EXPERTS), D ** -0.5),
        "router_bias": nrm(ks[26], (L, N_EXPERTS), 0.01),
        "exp_w1": nrm(ks[27], (L, N_EXPERTS, D, EXPERT_FF), D ** -0.5),
        "exp_w3": nrm(ks[28], (L, N_EXPERTS, D, EXPERT_FF), D ** -0.5),
        "exp_w2": nrm(ks[29], (L, N_EXPERTS, EXPERT_FF, D), EXPERT_FF ** -0.5),
        "shared_w1": nrm(ks[30], (L, D, SHARED_FF), D ** -0.5),
        "shared_w3": nrm(ks[31], (L, D, SHARED_FF), D ** -0.5),
        "shared_w2": nrm(ks[32], (L, SHARED_FF, D), SHARED_FF ** -0.5),
        "normf_g": gain(ks[33], (D,)),
    }


def reference(x, c, ada_w, ada_b, norm1_g, norm2_g, w_in, tshift_mu,
              rwkv_w0, rwkv_w_up, rwkv_a0, rwkv_a_up, rwkv_g_up, rwkv_k_k, rwkv_k_a, rwkv_r_k,
              rwkv_ln_g, rwkv_ln_b, gmlp_ln_g, gmlp_ln_b, gmlp_ws, gmlp_bs,
              w_out_a, w_out_b, w_out, router_w, router_bias,
              exp_w1, exp_w3, exp_w2, shared_w1, shared_w3, shared_w2, normf_g):
    for l in range(DEPTH):
        mod = jax.nn.silu(c) @ ada_w[l] + ada_b[l]
        sh1, sc1, g1, sh2, sc2, g2 = jnp.split(mod[:, None, :], 6, axis=-1)

        h = rmsnorm(x, norm1_g[l]) * (1.0 + sc1) + sh1
        p = h @ w_in[l]
        p_rwkv, p_u, p_v, p_gate = jnp.split(
            p, [RWKV_COLS, RWKV_COLS + GMLP_WIDTH, RWKV_COLS + 2 * GMLP_WIDTH], axis=-1)
        p_rwkv = token_shift(p_rwkv, tshift_mu[l])
        y_a = rwkv7_mixer(p_rwkv, rwkv_w0[l], rwkv_w_up[l], rwkv_a0[l], rwkv_a_up[l], rwkv_g_up[l],
                          rwkv_k_k[l], rwkv_k_a[l], rwkv_r_k[l], rwkv_ln_g[l], rwkv_ln_b[l])
        y_b = gmlp_mixer(p_u, p_v, gmlp_ln_g[l], gmlp_ln_b[l], gmlp_ws[l], gmlp_bs[l])
        gate_a, gate_b = jnp.split(jax.nn.sigmoid(p_gate.astype(jnp.float32)), 2, axis=-1)
        merged = gate_a * (y_a @ w_out_a[l]) + gate_b * (y_b @ w_out_b[l])
        x = x + (g1 * (merged @ w_out[l])).astype(x.dtype)

        h = rmsnorm(x, norm2_g[l]) * (1.0 + sc2) + sh2
        y = moe(h, router_w[l], router_bias[l], exp_w1[l], exp_w3[l], exp_w2[l],
                shared_w1[l], shared_w3[l], shared_w2[l])
        x = x + (g2 * y).astype(x.dtype)
    return rmsnorm(x, normf_g)
```

```python
import numpy as np
import concourse.bass as bass
import concourse.mybir as mybir
from concourse.bass_utils import run_bass_kernel_spmd

F32 = mybir.dt.float32
BF16 = mybir.dt.bfloat16
U32 = mybir.dt.uint32
I32 = mybir.dt.int32
AF = mybir.ActivationFunctionType
ALU = mybir.AluOpType
AX = mybir.AxisListType


class Buf:
    __slots__ = ("name", "w", "rs")

    def __init__(self, name):
        self.name = name
        self.w = None
        self.rs = []


class Tl:
    def __init__(self, t, name):
        self.t = t
        self.b = Buf(name)

    def __getitem__(self, k):
        return self.t[k]


class Eng:
    def __init__(self, P, name, h, sem):
        self.P = P
        self.name = name
        self.h = h
        self.sem = sem
        self.cnt = 0
        self.waited = {}


class Lane:
    def __init__(self, sem):
        self.sem = sem
        self.val = 0


class Prog:
    def __init__(self, nc, stack):
        self.nc = nc
        self.stack = stack
        mk = lambda n: stack.enter_context(nc.semaphore(n))
        self.pe = Eng(self, "pe", nc.tensor, mk("s_pe"))
        self.act = Eng(self, "act", nc.scalar, mk("s_act"))
        self.dve = Eng(self, "dve", nc.vector, mk("s_dve"))
        self.pool = Eng(self, "pool", nc.gpsimd, mk("s_pool"))
        self.sp = Eng(self, "sp", nc.sync, mk("s_sp"))
        self.engs = [self.pe, self.act, self.dve, self.pool, self.sp]
        self.nlanes = 0
        self.all_lanes = []
        self.out_toks = []
        self.nins = 0

    def tile(self, name, shape, dt):
        return Tl(self.sb(name, shape, dt), name)

    def ptile(self, name, shape, dt=F32):
        return Tl(self.ps(name, shape, dt), name)

    def barrier(self):
        toks = [(e.sem, e.cnt) for e in self.engs if e.cnt] + [(l.sem, l.val) for l in self.all_lanes if l.val]
        for e in self.engs:
            for t in toks:
                self._wait(e, t)

    def sb(self, name, shape, dt):
        return self.stack.enter_context(self.nc.sbuf_tensor(name, shape, dt))

    def ps(self, name, shape, dt=F32):
        return self.stack.enter_context(self.nc.psum_tensor(name, shape, dt))

    def lanes(self, n, name="ln"):
        out = []
        for i in range(n):
            out.append(Lane(self.stack.enter_context(self.nc.semaphore(f"{name}{self.nlanes}"))))
            self.nlanes += 1
        self.all_lanes += out
        return out

    def _wait(self, e, tok):
        if tok is None:
            return
        sem, val = tok
        k = id(sem)
        if e.waited.get(k, 0) >= val:
            return
        if sem is e.sem and e is self.pe:
            return
        e.h.wait_ge(sem, val)
        e.waited[k] = val
        self.nins += 1

    def _deps(self, e, r, w):
        r = [getattr(b, "b", b) for b in r]
        w = [getattr(b, "b", b) for b in w]
        for b in r:
            self._wait(e, b.w)
        for b in w:
            self._wait(e, b.w)
            for t in b.rs:
                self._wait(e, t)

    def _commit(self, tok, r, w):
        r = [getattr(b, "b", b) for b in r]
        w = [getattr(b, "b", b) for b in w]
        for b in r:
            b.rs.append(tok)
            if len(b.rs) > 24:
                d = {}
                for s, v in b.rs:
                    if id(s) not in d or d[id(s)][1] < v:
                        d[id(s)] = (s, v)
                b.rs = list(d.values())
        for b in w:
            b.w = tok
            b.rs = []

    def op(self, e, fn, r=(), w=()):
        self._deps(e, r, w)
        ins = fn(e.h)
        e.cnt += 1
        ins.then_inc(e.sem, 1)
        tok = (e.sem, e.cnt)
        self._commit(tok, r, w)
        self.nins += 1
        return tok

    def dma(self, e, lane, fn, r=(), w=(), is_out=False):
        self._wait(e, (lane.sem, lane.val) if lane.val else None)
        self._deps(e, r, w)
        ins = fn(e.h)
        lane.val += 16
        ins.then_inc(lane.sem, 16)
        tok = (lane.sem, lane.val)
        self._commit(tok, r, w)
        if is_out:
            self.out_toks.append(tok)
        self.nins += 1
        return tok

    def finish(self):
        for tok in self.out_toks:
            self._wait(self.sp, tok)


from contextlib import ExitStack

T = 8192
D = 1024
ST = 512
NST = T // ST
SDEC = -0.6065306597126334
RW = 1792
CAP = 512
BLK = 256
NBLK = 512
NE = 256
NSLOT = NE * CAP
ROW = 1024 + 64


def sl(h_):
    return (h_ % 2) * 4 + h_ // 2


class StopBuild(Exception):
    pass


def build(dbg=None, nst=NST, nblk_run=NBLK):
    nc = bass.Bass("TRN2", target_bir_lowering=False)
    holder = {}
    try:
        return _build(nc, dbg, nst, holder, nblk_run)
    except StopBuild:
        return nc, holder["P"]


def _build(nc, dbg, nst, holder, nblk_run):

    def din(name, shape, dt=F32):
        return nc.dram_tensor(name, shape, dt, kind="ExternalInput").ap()

    x = din("x", [T, D]); c = din("c", [1, D])
    ada_w = din("ada_w", [D, 6 * D]); ada_b = din("ada_b", [6 * D])
    norm1_g = din("norm1_g", [D]); norm2_g = din("norm2_g", [D])
    w_in = din("w_in", [D, 4864]); tshift_mu = din("tshift_mu", [RW])
    rwkv_w0 = din("rwkv_w0", [512]); rwkv_w_up = din("rwkv_w_up", [64, 512])
    rwkv_a0 = din("rwkv_a0", [512]); rwkv_a_up = din("rwkv_a_up", [64, 512])
    rwkv_g_up = din("rwkv_g_up", [128, 512]); rwkv_k_k = din("rwkv_k_k", [512])
    rwkv_k_a = din("rwkv_k_a", [512]); rwkv_r_k = din("rwkv_r_k", [512])
    rwkv_ln_g = din("rwkv_ln_g", [512]); rwkv_ln_b = din("rwkv_ln_b", [512])
    gmlp_ln_g = din("gmlp_ln_g", [512]); gmlp_ln_b = din("gmlp_ln_b", [512])
    gmlp_ws = din("gmlp_ws", [8, 128, 128]); gmlp_bs = din("gmlp_bs", [8, 128])
    w_out_a = din("w_out_a", [512, D]); w_out_b = din("w_out_b", [512, D]); w_out = din("w_out", [D, D])
    router_w = din("router_w", [D, NE]); router_bias = din("router_bias", [NE])
    if True:
        exp_w1 = din("exp_w1", [NE, D, 256]); exp_w3 = din("exp_w3", [NE, D, 256]); exp_w2 = din("exp_w2", [NE, 256, D])
    shared_w1 = din("shared_w1", [D, 256]); shared_w3 = din("shared_w3", [D, 256]); shared_w2 = din("shared_w2", [256, D])
    normf_g = din("normf_g", [D])
    out = nc.dram_tensor("out", [T, D], F32, kind="ExternalOutput").ap()

    def dscr(name, shape, dt):
        k = "ExternalOutput" if dbg == name else "Internal"
        return Tl(nc.dram_tensor(name, shape, dt, kind=k).ap(), name)

    yaT_d = dscr("yaT_d", [512, T], BF16)
    x1_d = dscr("x1_d", [T, D], F32)
    xg_d = dscr("xg_d", [NSLOT, D], BF16)
    yg_d = dscr("yg_d", [NSLOT, D], BF16)

    with ExitStack() as gs:
        P = Prog(nc, gs)
        holder["P"] = P

        def ck(n, tl, ap=None):
            if dbg == f"ck{n}":
                o_ = nc.dram_tensor("o_ck", list((ap if ap is not None else tl[:]).shape), (ap if ap is not None else tl[:]).dtype, kind="ExternalOutput").ap()
                stq(o_, ap if ap is not None else tl[:], [tl], is_out=True)
                P.finish()
                raise StopBuild()
        LD = P.lanes(6, "ld")
        LP = P.lanes(4, "lp")
        LS = P.lanes(4, "lst")
        ldi = [0]; lpi = [0]; lsi = [0]

        def ld(out_ap, in_ap, w, r=()):
            l = LD[ldi[0] % len(LD)]; ldi[0] += 1
            return P.dma(P.sp, l, lambda h: h.dma_start(out=out_ap, in_=in_ap), r=r, w=w)

        def ldc(out_ap, in_ap, w, r=()):
            l = LP[lpi[0] % len(LP)]; lpi[0] += 1
            return P.dma(P.pool, l, lambda h: h.dma_start(out=out_ap, in_=in_ap), r=r, w=w)

        LSQ = {"sp": LS, "pool": P.lanes(3, "lsp"), "act": P.lanes(3, "lsa")}

        def stq(out_ap, in_ap, r, w=(), is_out=False, q=None):
            q = q or P.sp
            ll = LSQ[q.name]
            l = ll[lsi[0] % len(ll)]; lsi[0] += 1
            return P.dma(q, l, lambda h: h.dma_start(out=out_ap, in_=in_ap), r=r, w=w, is_out=is_out)

        pbanks = [P.ptile(f"pb{i}", [128, 512], F32) for i in range(6)]
        bbanks = [P.ptile(f"bb{i}", [128, 1024], BF16) for i in range(2)]
        pbi = [0]; bbi = [0]

        def bank():
            b = pbanks[pbi[0] % 6]; pbi[0] += 1
            return b

        def bbank():
            b = bbanks[bbi[0] % 2]; bbi[0] += 1
            return b

        def mm(o, lhsT, rhs, start, stop, r, w):
            return P.op(P.pe, lambda h: h.matmul(o, lhsT=lhsT, rhs=rhs, start=start, stop=stop), r=r, w=w)

        def tr(o, in_, ident, r, w):
            return P.op(P.pe, lambda h: h.transpose(out=o, in_=in_, identity=ident), r=r, w=w)

        def act(o, in_, func, r, w, **kw):
            return P.op(P.act, lambda h: h.activation(out=o, in_=in_, func=func, **kw), r=r, w=w)

        def tt(e, o, a, b, op, r, w):
            return P.op(e, lambda h: h.tensor_tensor(out=o, in0=a, in1=b, op=op), r=r, w=w)

        def ts(e, o, a, s1, s2, op0, op1, r, w):
            if s2 is None:
                return P.op(e, lambda h: h.tensor_scalar(out=o, in0=a, scalar1=s1, scalar2=None, op0=op0), r=r, w=w)
            return P.op(e, lambda h: h.tensor_scalar(out=o, in0=a, scalar1=s1, scalar2=s2, op0=op0, op1=op1), r=r, w=w)

        def stt(e, o, a, s, b, op0, op1, r, w, **kw):
            return P.op(e, lambda h: h.scalar_tensor_tensor(out=o, in0=a, scalar=s, in1=b, op0=op0, op1=op1, **kw), r=r, w=w)

        def cp(e, o, a, r, w):
            return P.op(e, lambda h: h.tensor_copy(out=o, in_=a), r=r, w=w)

        def ms(e, o, v, w, r=()):
            return P.op(e, lambda h: h.memset(o, v), r=r, w=w)

        identf = P.tile("identf", [128, 128], F32)
        identb = P.tile("identb", [128, 128], BF16)
        ms(P.pool, identf[:], 0.0, [identf])
        P.op(P.pool, lambda h: h.affine_select(out=identf[:], in_=identf[:], pattern=[[-1, 128]], compare_op=ALU.not_equal, fill=1.0, base=0, channel_multiplier=1), r=[identf], w=[identf])
        cp(P.dve, identb[:], identf[:], [identf], [identb])
        maskCM = P.tile("maskCM", [128, 512], F32)
        mAT = P.tile("mAT", [128, 128], F32)
        ms(P.pool, maskCM[:], 1.0, [maskCM])
        P.op(P.pool, lambda h: h.affine_select(out=maskCM[:, 0:128], in_=maskCM[:, 0:128], pattern=[[1, 128]], compare_op=ALU.is_gt, fill=0.0, base=0, channel_multiplier=-1), r=[maskCM], w=[maskCM])
        P.op(P.pool, lambda h: h.affine_select(out=maskCM[:, 128:256], in_=maskCM[:, 128:256], pattern=[[1, 128]], compare_op=ALU.is_ge, fill=0.0, base=0, channel_multiplier=-1), r=[maskCM], w=[maskCM])
        ms(P.pool, maskCM[0:64, 64:128], 0.0, [maskCM], [maskCM])
        ms(P.pool, maskCM[0:64, 192:256], 0.0, [maskCM], [maskCM])
        cp(P.dve, maskCM[:, 256:512], maskCM[:, 0:256], [maskCM], [maskCM])
        ms(P.pool, mAT[:], 1.0, [mAT])
        P.op(P.pool, lambda h: h.affine_select(out=mAT[:], in_=mAT[:], pattern=[[-1, 128]], compare_op=ALU.is_gt, fill=0.0, base=0, channel_multiplier=1), r=[mAT], w=[mAT])
        ms(P.pool, mAT[64:128, 0:64], 0.0, [mAT], [mAT])
        bd4 = P.tile("bd4", [128, 512], F32)
        bdb = P.tile("bdb", [128, 128], BF16)
        ms(P.pool, bd4[:], 0.0, [bd4])
        for q in range(4):
            ms(P.pool, bd4[0:64, q * 128:q * 128 + 64], 1.0, [bd4], [bd4])
            ms(P.pool, bd4[64:128, q * 128 + 64:q * 128 + 128], 1.0, [bd4], [bd4])
        cp(P.dve, bdb[:], bd4[:, 0:128], [bd4], [bdb])
        reset = P.tile("reset", [128, 512], F32)
        ms(P.pool, reset[:], 1.0, [reset])
        ms(P.pool, reset[:].rearrange("p (c t) -> p c t", t=64)[:, :, 0:1], 0.0, [reset], [reset])

        s1 = P.tile("s1", [128, 8], F32); sh1 = P.tile("sh1", [128, 8], F32)
        s2 = P.tile("s2", [128, 8], F32); sh2 = P.tile("sh2", [128, 8], F32)
        g1bc = P.tile("g1bc", [128, D], F32); g2bc = P.tile("g2bc", [128, D], F32)
        with ExitStack() as ph, nc.allow_non_contiguous_dma(reason="tiny per-channel vectors"):
            P.stack = ph
            ccol = P.tile("ccol", [128, 8], F32)
            ld(ccol[:], c[0, :].rearrange("(n p) -> p n", p=128), [ccol])
            scol2 = P.tile("scol2", [128, 8, 2], F32)
            act(scol2[:, :, 0], ccol[:], AF.Silu, [ccol], [scol2])
            act(scol2[:, :, 1], ccol[:], AF.Silu, [ccol], [scol2])
            sbc = P.tile("sbc", [128, 8, 128], F32)
            for kc in range(8):
                cp(P.dve, sbc[:, kc, :], scol2[:, kc, 0:1].to_broadcast([128, 128]), [scol2], [sbc])
            adabT = P.tile("adabT", [128, 48], F32)
            ld(adabT[:], ada_b.rearrange("(n p) -> p n", p=128), [adabT])
            n1g = P.tile("n1g", [128, 8], F32); n2g = P.tile("n2g", [128, 8], F32)
            ld(n1g[:], norm1_g.rearrange("(n p) -> p n", p=128), [n1g])
            ld(n2g[:], norm2_g.rearrange("(n p) -> p n", p=128), [n2g])
            modT = P.tile("modT", [128, 48], F32)
            awb = [P.tile(f"awb{i}", [128, 8, 1024], F32) for i in range(2)]
            adab_bc = P.tile("adab_bc", [128, 1024], F32)
            for vi in range(6):
                aw = awb[vi % 2]
                for kc in range(8):
                    ld(aw[:, kc, :], ada_w[kc * 128:(kc + 1) * 128, vi * 1024:(vi + 1) * 1024], [aw])
                if vi in (2, 5):
                    gbc = g1bc if vi == 2 else g2bc
                    ld(adab_bc[:], ada_b[vi * 1024:(vi + 1) * 1024].partition_broadcast(128), [adab_bc])
                    for hf in range(2):
                        pb = bank()
                        for kc in range(8):
                            mm(pb[:, :], sbc[:, kc, :], aw[:, kc, hf * 512:(hf + 1) * 512], kc == 0, kc == 7, [sbc, aw], [pb])
                        tt(P.dve, gbc[:, hf * 512:(hf + 1) * 512], pb[:, :], adab_bc[:, hf * 512:(hf + 1) * 512], ALU.add, [pb, adab_bc], [gbc])
                else:
                    pb = bank()
                    for oc in range(8):
                        for kc in range(8):
                            mm(pb[:, oc * 2:oc * 2 + 2], aw[:, kc, oc * 128:(oc + 1) * 128], scol2[:, kc, :], kc == 0, kc == 7, [aw, scol2], [pb])
                    tt(P.dve, modT[:, vi * 8:(vi + 1) * 8], pb[:, 0:16].rearrange("p (o t) -> p o t", t=2)[:, :, 0], adabT[:, vi * 8:(vi + 1) * 8], ALU.add, [pb, adabT], [modT])
            stt(P.dve, s1[:], modT[:, 8:16], 1.0, n1g[:], ALU.add, ALU.mult, [modT, n1g], [s1])
            cp(P.dve, sh1[:], modT[:, 0:8], [modT], [sh1])
            stt(P.dve, s2[:], modT[:, 32:40], 1.0, n2g[:], ALU.add, ALU.mult, [modT, n2g], [s2])
            cp(P.dve, sh2[:], modT[:, 24:32], [modT], [sh2])
            P.barrier()
            if dbg == "p0":
                o0 = nc.dram_tensor("o_p0", [128, 32], F32, kind="ExternalOutput").ap()
                o1 = nc.dram_tensor("o_g", [128, 2048], F32, kind="ExternalOutput").ap()
                stq(o0[:, 0:8], s1[:], [s1], is_out=True); stq(o0[:, 8:16], sh1[:], [sh1], is_out=True)
                stq(o0[:, 16:24], s2[:], [s2], is_out=True); stq(o0[:, 24:32], sh2[:], [sh2], is_out=True)
                stq(o1[:, 0:1024], g1bc[:], [g1bc], is_out=True); stq(o1[:, 1024:2048], g2bc[:], [g2bc], is_out=True)
                P.finish()
                return nc, P
        P.stack = gs

        def norm_T(xts, hT, src, sidx, sc, shf, tmp):
            t0 = sidx * ST
            ssq, rstd, xn, junk = tmp
            for ti in range(4):
                xt = xts[ti % 2]
                ld(xt[:], src[t0 + ti * 128:t0 + (ti + 1) * 128, :], [xt])
                act(xn[:, ti, :], xt[:], AF.Square, [xt], [xn, ssq], accum_out=ssq[:, ti:ti + 1])
                ts(P.dve, rstd[:, ti:ti + 1], ssq[:, ti:ti + 1], 1.0 / D, 1e-6, ALU.mult, ALU.add, [ssq], [rstd])
                act(rstd[:, ti:ti + 1], rstd[:, ti:ti + 1], AF.Sqrt, [rstd], [rstd])
                P.op(P.dve, lambda h: h.reciprocal(out=rstd[:, ti:ti + 1], in_=rstd[:, ti:ti + 1]), r=[rstd], w=[rstd])
                act(xn[:, ti, :], xt[:], AF.Identity, [xt, rstd], [xn], scale=rstd[:, ti:ti + 1])
            if dbg == "a1n1":
                o0 = nc.dram_tensor("o_xn", [128, 4, D], BF16, kind="ExternalOutput").ap()
                stq(o0, xn[:], [xn], is_out=True)
                o1 = nc.dram_tensor("o_rstd", [128, 4], F32, kind="ExternalOutput").ap()
                stq(o1, rstd[:], [rstd], is_out=True)
                P.finish()
                return
            for dc in range(8):
                bb = bbank()
                for ti in range(4):
                    tr(bb[:, ti * 128:(ti + 1) * 128], xn[:, ti, dc * 128:(dc + 1) * 128], identb[:], [xn, identb], [bb])
                act(hT[:, dc, :], bb[:, 0:512], AF.Identity, [bb, sc, shf], [hT], scale=sc[:, dc:dc + 1], bias=shf[:, dc:dc + 1])

        with ExitStack() as ph, nc.allow_non_contiguous_dma(reason="tiny per-channel vectors"):
            P.stack = ph
            winr = P.tile("winr", [128, 8, RW], BF16)
            for dc in range(8):
                ldc(winr[:, dc, :], w_in[dc * 128:(dc + 1) * 128, 0:RW], [winr])
            Wlw = P.tile("Wlw", [128, 512], BF16); Wla = P.tile("Wla", [128, 512], BF16)
            gup = P.tile("gup", [128, 512], BF16)
            ms(P.dve, Wlw[:], 0.0, [Wlw]); ms(P.dve, Wla[:], 0.0, [Wla])
            ldc(Wlw[0:64, :], rwkv_w_up, [Wlw]); ldc(Wla[64:128, :], rwkv_a_up, [Wla]); ldc(gup[:], rwkv_g_up, [gup])

            def colvec(name, src, n):
                t = P.tile(name, [128, n], F32)
                ld(t[:], src.rearrange("(n p) -> p n", p=128), [t])
                return t
            w0 = colvec("w0", rwkv_w0, 4); a0 = colvec("a0", rwkv_a0, 4); kkv = colvec("kkv", rwkv_k_k, 4)
            kav = colvec("kav", rwkv_k_a, 4); rkv = colvec("rkv", rwkv_r_k, 4)
            lng = colvec("lng", rwkv_ln_g, 4); lnb = colvec("lnb", rwkv_ln_b, 4)
            mu = colvec("mu", tshift_mu, 14)
            omu = P.tile("omu", [128, 14], F32); omka = P.tile("omka", [128, 4], F32)
            ts(P.dve, omu[:], mu[:], -1.0, 1.0, ALU.mult, ALU.add, [mu], [omu])
            ts(P.dve, omka[:], kav[:], -1.0, 1.0, ALU.mult, ALU.add, [kav], [omka])

            if dbg == "a1w":
                o1 = nc.dram_tensor("o_omu", [128, 14], F32, kind="ExternalOutput").ap()
                stq(o1, omu[:], [omu], is_out=True)
                o2 = nc.dram_tensor("o_wla", [128, 512], BF16, kind="ExternalOutput").ap()
                stq(o2, Wla[:], [Wla], is_out=True)
                o3 = nc.dram_tensor("o_winr", [128, 8, RW], BF16, kind="ExternalOutput").ap()
                stq(o3, winr[:], [winr], is_out=True)
                P.finish()
                return nc, P
            xts = [P.tile(f"xt{i}", [128, D], F32) for i in range(2)]
            hT = P.tile("hT", [128, 8, 512], BF16)
            ssq = P.tile("ssq", [128, 4], F32); rstd = P.tile("rstd", [128, 4], F32)
            xn = P.tile("xn", [128, 4, D], BF16); junk = None
            ntmp = (ssq, rstd, xn, junk)
            sh = [P.tile(f"shq{q}", [128, 512], F32) for q in range(14)]
            pmus = [P.tile("pmu0", [128, 513], F32)] * 2
            carry = P.tile("carry", [128, 14], F32)
            ms(P.pool, carry[:], 0.0, [carry])
            lo_in = P.tile("lo_in", [128, 512], BF16); sxg = P.tile("sxg", [128, 512], BF16)
            gT = [P.tile(f"gT{j}", [128, 512], BF16) for j in range(4)]
            bonT = [P.tile(f"bonT{j}", [128, 512], BF16) for j in range(4)]
            RA = P.tile("RA", [128, 4, 4, 256], BF16)
            BT = [P.tile(f"BT{j}", [128, 512], BF16) for j in range(4)]
            KT = [P.tile(f"KT{j}", [128, 512], BF16) for j in range(4)]
            gC = P.tile("gC", [128, 4, 8], F32)
            TOKB2 = P.tile("TOKB2", [128, 4, 512], BF16)
            TOKK2 = P.tile("TOKK2", [128, 4, 512], BF16)
            TOKV = P.tile("TOKV", [128, 4, 512], BF16)
            sgw = P.tile("sgw", [128, 512], F32); asig = P.tile("asig", [128, 512], F32)
            kksq = P.tile("kksq", [128, 512], BF16); rn = P.tile("rn", [128, 512], F32)
            kkn = P.tile("kkn", [128, 512], F32); ff = P.tile("ff", [128, 512], F32)
            kp = P.tile("kp", [128, 512], F32); bp = P.tile("bp", [128, 512], F32)
            cum = P.tile("cum", [128, 512], F32); cme = ff
            cdf = rn
            e1 = sgw; e2 = P.tile("e2", [128, 512], F32)
            e3 = e1; e4 = e2
            rk = P.tile("rk", [128, 512], BF16)
            tb3 = P.tile("tb3", [128, 3, 512], BF16)
            class _View:
                def __init__(self, ap, b):
                    self.ap = ap; self.b = b

                def __getitem__(self, k):
                    return self.ap[k]
            CM = [P.tile("CM0", [128, 8, 512], BF16), _View(xn[:].rearrange("p a (b c) -> p (a b) c", c=512), xn.b)]
            Xs = [P.tile(f"Xs{i}", [128, 8, 128], BF16) for i in range(2)]
            XTs = [P.tile(f"XTs{i}", [128, 8, 128], BF16) for i in range(2)]
            Ps = [P.tile(f"Ps{i}", [128, 8, 128], BF16) for i in range(2)]
            TT = [P.tile(f"TT{i}", [128, 8, 128], BF16) for i in range(2)]
            Hf = [P.tile(f"Hf{i}", [128, 512], F32) for i in range(2)]
            Hb = [P.tile(f"Hb{i}", [128, 512], BF16) for i in range(2)]
            ms(P.pool, Hf[0][:], 0.0, [Hf[0]]); ms(P.pool, Hb[0][:], 0.0, [Hb[0]])
            X1sb = P.tile("X1sb", [128, 512], BF16); Usb = P.tile("Usb", [128, 512], BF16)
            ms(P.pool, X1sb[:], 0.0, [X1sb]); ms(P.pool, Usb[:], 0.0, [Usb])
            htmp = kp
            Ysb = P.tile("Ysb", [128, 512], F32)
            gmean = P.tile("gmean", [128, 8], F32); gvar = P.tile("gvar", [128, 8], F32)
            yc = e2; ysq = e1
            ynb = P.tile("ynb", [128, 4, 512], BF16)
            yaT = P.tile("yaT", [128, 4, 512], BF16)
            yt1 = kkn
            hcur = [0]
            chunk_g = 0

            for si in range(nst):
                t0 = si * ST
                norm_T(xts, hT, x, si, s1, sh1, ntmp)
                if dbg == "a1n1":
                    return nc, P
                if dbg == "a1n":
                    o0 = nc.dram_tensor("o_hT", [128, 8, 512], BF16, kind="ExternalOutput").ap()
                    stq(o0, hT[:], [hT], is_out=True)
                    o1 = nc.dram_tensor("o_omu", [128, 14], F32, kind="ExternalOutput").ap()
                    stq(o1, omu[:], [omu], is_out=True)
                    P.finish()
                    return nc, P
                for q in range(14):
                    pb = bank()
                    for dc in range(8):
                        mm(pb[:, :], winr[:, dc, q * 128:(q + 1) * 128], hT[:, dc, :], dc == 0, dc == 7, [winr, hT], [pb])
                    pmu = pmus[q % 2]
                    act(sh[q][:], pb[:, :], AF.Identity, [pb, omu], [sh[q]], scale=omu[:, q:q + 1])
                    act(pmu[:, 0:1], carry[:, q:q + 1], AF.Copy, [carry], [pmu])
                    ts(P.dve, pmu[:, 1:513], pb[:, :], mu[:, q:q + 1], None, ALU.mult, None, [pb, mu], [pmu])
                    tt(P.dve, sh[q][:], sh[q][:], pmu[:, 0:512], ALU.add, [sh[q], pmu], [sh[q]])
                    act(carry[:, q:q + 1], pmu[:, 512:513], AF.Copy, [pmu], [carry])
                if dbg == "a1a":
                    o0 = nc.dram_tensor("o_sh", [14, 128, 512], F32, kind="ExternalOutput").ap()
                    for q in range(14):
                        stq(o0[q], sh[q][:], [sh[q]], is_out=True)
                    P.finish()
                    return nc, P
                act(lo_in[0:64, :], sh[12][0:64, :], AF.Tanh, [sh[12]], [lo_in])
                act(lo_in[64:128, :], sh[12][64:128, :], AF.Copy, [sh[12]], [lo_in])
                act(sxg[:], sh[13][:], AF.Sigmoid, [sh[13]], [sxg])
                for j in range(4):
                    r_, k_, v_ = sh[j], sh[4 + j], sh[8 + j]
                    js = slice(j * 128, (j + 1) * 128)
                    pb = bank()
                    mm(pb[:, :], Wlw[:, js], lo_in[:], True, True, [Wlw, lo_in], [pb])
                    act(sgw[:], pb[:, :], AF.Sigmoid, [pb, w0], [sgw], bias=w0[:, j:j + 1])
                    pb = bank()
                    mm(pb[:, :], Wla[:, js], lo_in[:], True, True, [Wla, lo_in], [pb])
                    act(asig[:], pb[:, :], AF.Sigmoid, [pb, a0], [asig], bias=a0[:, j:j + 1])
                    pb = bank()
                    mm(pb[:, :], gup[:, js], sxg[:], True, True, [gup, sxg], [pb])
                    act(gT[j][:], pb[:, :], AF.Copy, [pb], [gT[j]])
                    ck(1, asig)
                    act(kksq[:], k_[:], AF.Square, [k_, kkv], [kksq], scale=kkv[:, j:j + 1])
                    pb = bank()
                    mm(pb[:, :], bdb[:], kksq[:], True, True, [bdb, kksq], [pb])
                    act(rn[:], pb[:, :], AF.Sqrt, [pb], [rn], bias=1e-24)
                    P.op(P.dve, lambda h: h.reciprocal(out=rn[:], in_=rn[:]), r=[rn], w=[rn])
                    stt(P.dve, kkn[:], k_[:], kkv[:, j:j + 1], rn[:], ALU.mult, ALU.mult, [k_, kkv, rn], [kkn])
                    ck(2, kkn)
                    ts(P.dve, ff[:], asig[:], kav[:, j:j + 1], omka[:, j:j + 1], ALU.mult, ALU.add, [asig, kav, omka], [ff])
                    tt(P.pool, kp[:], k_[:], ff[:], ALU.mult, [k_, ff], [kp])
                    tt(P.pool, bp[:], kkn[:], asig[:], ALU.mult, [kkn, asig], [bp])
                    P.op(P.dve, lambda h: h.tensor_tensor_scan(out=cum[:], data0=reset[:], data1=sgw[:], initial=0.0, op0=ALU.mult, op1=ALU.add), r=[reset, sgw], w=[cum])
                    tt(P.pool, cme[:], cum[:], sgw[:], ALU.subtract, [cum, sgw], [cme])
                    ck(3, cum)
                    cum3 = cum[:].rearrange("p (c t) -> p c t", t=64)
                    tt(P.dve, cdf[:].rearrange("p (c t) -> p c t", t=64), cum3[:, :, 63:64].to_broadcast([128, 8, 64]), cum3, ALU.subtract, [cum], [cdf])
                    ck(4, cdf)
                    act(e1[:], cum[:], AF.Exp, [cum], [e1], scale=SDEC)
                    act(e2[:], cme[:], AF.Exp, [cme], [e2], scale=SDEC)
                    act(gC[:, j, :], cum3[:, :, 63], AF.Exp, [cum], [gC], scale=SDEC)
                    tt(P.dve, RA[:, j, :, 128:256], r_[:].rearrange("p (a t) -> p a t", t=128), e1[:].rearrange("p (a t) -> p a t", t=128), ALU.mult, [r_, e1], [RA])
                    stt(P.dve, RA[:, j, :, 0:128], kkn[:].rearrange("p (a t) -> p a t", t=128), -1.0, e2[:].rearrange("p (a t) -> p a t", t=128), ALU.mult, ALU.mult, [kkn, e2], [RA])
                    ck(5, RA)
                    act(e3[:], cum[:], AF.Exp, [cum], [e3], scale=-SDEC)
                    act(e4[:], cdf[:], AF.Exp, [cdf], [e4], scale=SDEC)
                    tt(P.pool, BT[j][:], bp[:], e3[:], ALU.mult, [bp, e3], [BT[j]])
                    tt(P.pool, KT[j][:], kp[:], e3[:], ALU.mult, [kp, e3], [KT[j]])
                    tt(P.dve, tb3[:, 0, :], bp[:], e4[:], ALU.mult, [bp, e4], [tb3])
                    tt(P.pool, tb3[:, 1, :], kp[:], e4[:], ALU.mult, [kp, e4], [tb3])
                    act(tb3[:, 2, :], v_[:], AF.Copy, [v_], [tb3])
                    stt(P.dve, rk[:], r_[:], rkv[:, j:j + 1], kp[:], ALU.mult, ALU.mult, [r_, rkv, kp], [rk])
                    pb = bank()
                    mm(pb[:, :], bdb[:], rk[:], True, True, [bdb, rk], [pb])
                    tt(P.dve, bonT[j][:], pb[:, :], v_[:], ALU.mult, [pb, v_], [bonT[j]])
                    ck(6, bonT[j])
                    for ti in range(4):
                        bb = bbank()
                        for kind in range(3):
                            tr(bb[:, kind * 128:(kind + 1) * 128], tb3[:, kind, ti * 128:(ti + 1) * 128], identb[:], [tb3, identb], [bb])
                        cp(P.dve, TOKB2[:, ti, js], bb[:, 0:128], [bb], [TOKB2])
                        cp(P.dve, TOKK2[:, ti, js], bb[:, 128:256], [bb], [TOKK2])
                        cp(P.dve, TOKV[:, ti, js], bb[:, 256:384], [bb], [TOKV])
                    ck(7, TOKV, TOKV[:, 0, 0:128])

                if dbg == "a1b":
                    o0 = nc.dram_tensor("o_ra", [128, 4, 4, 256], BF16, kind="ExternalOutput").ap()
                    o1 = nc.dram_tensor("o_tokv", [128, 4, 512], BF16, kind="ExternalOutput").ap()
                    o2 = nc.dram_tensor("o_gc", [128, 4, 8], F32, kind="ExternalOutput").ap()
                    stq(o0, RA[:], [RA], is_out=True); stq(o1, TOKV[:], [TOKV], is_out=True); stq(o2, gC[:], [gC], is_out=True)
                    P.finish()
                    return nc, P
                def gen_D(ti):
                    cm = CM[ti % 2]; tts = TT[ti % 2]
                    tsl = slice(ti * 128, (ti + 1) * 128)
                    for hb4 in range(2):
                        pbn = bank()
                        for hh in range(4):
                            h_ = 2 * hh + hb4; j = hh; ps_ = slice(hb4 * 64, hb4 * 64 + 64)
                            mm(pbn[:, hh * 128:(hh + 1) * 128], RA[ps_, j, ti, 0:128], BT[j][ps_, tsl], True, True, [RA, BT[j]], [pbn])
                        tt(P.dve, XTs[0][:, hb4 * 4:(hb4 + 1) * 4, :], pbn[:, :].rearrange("p (a t) -> p a t", t=128), mAT[:, :].unsqueeze(1).to_broadcast([128, 4, 128]), ALU.mult, [pbn, mAT], [XTs[0]])
                    for h_ in range(8):
                        j = h_ // 2; ps_ = slice((h_ % 2) * 64, (h_ % 2) * 64 + 64)
                        pb = bank()
                        mm(pb[:, 0:256], KT[j][ps_, tsl], RA[ps_, j, ti, :], True, True, [KT[j], RA], [pb])
                        mm(pb[:, 256:512], BT[j][ps_, tsl], RA[ps_, j, ti, :], True, True, [BT[j], RA], [pb])
                        tt(P.dve, cm[:, sl(h_), :], pb[:, :], maskCM[:], ALU.mult, [pb, maskCM], [cm])
                        if h_ % 2 == 1:
                            yield
                    ck(8, cm, cm[:, 0, :])
                    cp(P.pool, Xs[0][:], cm[:, :, 256:384], [cm], [Xs[0]])
                    tt(P.pool, Ps[0][:], cm[:, :, 256:384], identf[:, :].unsqueeze(1).to_broadcast([128, 8, 128]), ALU.add, [cm, identf], [Ps[0]])
                    cur = 0
                    for it in range(1, 6):
                        nxt = 1 - cur
                        last = (it == 5)
                        for hb4 in range(2):
                            hs4 = slice(hb4 * 4, hb4 * 4 + 4)
                            pbx = bank() if not last else None
                            pbt = bank()
                            for hh in range(4):
                                h_ = hb4 * 4 + hh
                                cs = slice(hh * 128, (hh + 1) * 128)
                                if not last:
                                    mm(pbx[:, cs], XTs[cur][:, h_, :], Xs[cur][:, h_, :], True, True, [XTs[cur], Xs[cur]], [pbx])
                                mm(pbt[:, cs], Xs[cur][:, h_, :], XTs[cur][:, h_, :], True, True, [XTs[cur], Xs[cur]], [pbt])
                            if not last:
                                act(Xs[nxt][:, hs4, :], pbx[:, :].rearrange("p (a t) -> p a t", t=128), AF.Copy, [pbx], [Xs[nxt]])
                            cp(P.dve, XTs[nxt][:, hs4, :], pbt[:, :].rearrange("p (a t) -> p a t", t=128), [pbt], [XTs[nxt]])
                            pbp = bank()
                            for hh in range(4):
                                h_ = hb4 * 4 + hh
                                cs = slice(hh * 128, (hh + 1) * 128)
                                mm(pbp[:, cs], identb[:], Ps[cur][:, h_, :], True, False, [identb, Ps[cur]], [pbp])
                                mm(pbp[:, cs], XTs[nxt][:, h_, :], Ps[cur][:, h_, :], False, True, [XTs[nxt], Ps[cur]], [pbp])
                            dst = tts if last else Ps[nxt]
                            act(dst[:, hs4, :], pbp[:, :].rearrange("p (a t) -> p a t", t=128), AF.Copy, [pbp], [dst])
                            yield
                        cur = nxt

                def gen_S(ti):
                    cm = CM[ti % 2]; tts = TT[ti % 2]
                    for p in range(2):
                        rows = slice(64 * p, 64 * p + 64)
                        cc = slice(64 * p, 64 * p + 64)
                        hold_f, hold_b = Hf[hcur[0]], Hb[hcur[0]]
                        hnew_f, hnew_b = Hf[1 - hcur[0]], Hb[1 - hcur[0]]
                        ch = ti * 2 + p
                        tt(P.pool, hnew_f[:].rearrange("p (j v) -> p j v", v=128), hold_f[:].rearrange("p (j v) -> p j v", v=128), gC[:, :, ch:ch + 1].to_broadcast([128, 4, 128]), ALU.mult, [hold_f, gC], [hnew_f])
                        ps1 = bank()
                        for h_ in range(8):
                            j = h_ // 2; hb = h_ % 2
                            o = ps1[rows, h_ * 64:(h_ + 1) * 64]
                            mm(o, RA[:, j, ti, 64 * p:64 * p + 64], hold_b[:, j * 128 + hb * 64:j * 128 + hb * 64 + 64], True, False, [RA, hold_b], [ps1])
                            mm(o, cm[:, sl(h_), 64 * p:64 * p + 64], TOKV[:, ti, h_ * 64:(h_ + 1) * 64], False, True, [cm, TOKV], [ps1])
                        act(X1sb[rows, :], ps1[rows, :], AF.Copy, [ps1], [X1sb])
                        yield
                        ps2 = bank()
                        for h_ in range(8):
                            mm(ps2[rows, h_ * 64:(h_ + 1) * 64], tts[:, sl(h_), 64 * p:64 * p + 64], X1sb[:, h_ * 64:(h_ + 1) * 64], True, True, [tts, X1sb], [ps2])
                        cp(P.dve, Usb[rows, :], ps2[rows, :], [ps2], [Usb])
                        yield
                        ps4 = bank()
                        for h_ in range(8):
                            j = h_ // 2; hb = h_ % 2
                            o = ps4[rows, h_ * 64:(h_ + 1) * 64]
                            mm(o, RA[:, j, ti, 128 + 64 * p:128 + 64 * p + 64], hold_b[:, j * 128 + hb * 64:j * 128 + hb * 64 + 64], True, False, [RA, hold_b], [ps4])
                            mm(o, cm[:, sl(h_), 384 + 64 * p:384 + 64 * p + 64], Usb[:, h_ * 64:(h_ + 1) * 64], False, False, [cm, Usb], [ps4])
                            mm(o, cm[:, sl(h_), 128 + 64 * p:128 + 64 * p + 64], TOKV[:, ti, h_ * 64:(h_ + 1) * 64], False, True, [cm, TOKV], [ps4])
                        act(Ysb[rows, :], ps4[rows, :], AF.Copy, [ps4], [Ysb])
                        yield
                        ps3 = bank()
                        for j in range(4):
                            js = slice(j * 128, (j + 1) * 128)
                            mm(ps3[:, js], TOKB2[rows, ti, js], Usb[rows, js], True, False, [TOKB2, Usb], [ps3])
                            mm(ps3[:, js], TOKK2[rows, ti, js], TOKV[rows, ti, js], False, True, [TOKK2, TOKV], [ps3])
                        tt(P.dve, htmp[:], ps3[:, :], bd4[:], ALU.mult, [ps3, bd4], [htmp])
                        tt(P.dve, hnew_f[:], hnew_f[:], htmp[:], ALU.add, [hnew_f, htmp], [hnew_f])
                        act(hnew_b[:], hnew_f[:], AF.Copy, [hnew_f], [hnew_b])
                        hcur[0] = 1 - hcur[0]
                        yield
                    y3 = Ysb[:, :].rearrange("p (h v) -> p h v", v=64)
                    P.op(P.dve, lambda h: h.tensor_reduce(out=gmean[:], in_=y3, axis=AX.X, op=ALU.add), r=[Ysb], w=[gmean])
                    ts(P.dve, gmean[:], gmean[:], 1.0 / 64, None, ALU.mult, None, [gmean], [gmean])
                    tt(P.dve, yc[:].rearrange("p (h v) -> p h v", v=64), y3, gmean[:, :].unsqueeze(2).to_broadcast([128, 8, 64]), ALU.subtract, [Ysb, gmean], [yc])
                    tt(P.pool, ysq[:], yc[:], yc[:], ALU.mult, [yc], [ysq])
                    P.op(P.dve, lambda h: h.tensor_reduce(out=gvar[:], in_=ysq[:].rearrange("p (h v) -> p h v", v=64), axis=AX.X, op=ALU.add), r=[ysq], w=[gvar])
                    ts(P.dve, gvar[:], gvar[:], 1.0 / 64, 64e-5, ALU.mult, ALU.add, [gvar], [gvar])
                    act(gvar[:], gvar[:], AF.Sqrt, [gvar], [gvar])
                    P.op(P.dve, lambda h: h.reciprocal(out=gvar[:], in_=gvar[:]), r=[gvar], w=[gvar])
                    tt(P.dve, ynb[:, ti, :].rearrange("p (h v) -> p h v", v=64), yc[:].rearrange("p (h v) -> p h v", v=64), gvar[:, :].unsqueeze(2).to_broadcast([128, 8, 64]), ALU.mult, [yc, gvar], [ynb])

                def drive(*gens):
                    gens = [g for g in gens if g is not None]
                    while gens:
                        for g in list(gens):
                            try:
                                next(g)
                            except StopIteration:
                                gens.remove(g)
                drive(gen_D(0))
                for ti in range(4):
                    drive(gen_S(ti), gen_D(ti + 1) if ti < 3 else None)
                for j in range(4):
                    bb = bbank()
                    for ti in range(4):
                        tr(bb[:, ti * 128:(ti + 1) * 128], ynb[:, ti, j * 128:(j + 1) * 128], identb[:], [ynb, identb], [bb])
                    act(yt1[:], bb[:, 0:512], AF.Identity, [bb, lng, lnb], [yt1], scale=lng[:, j:j + 1], bias=lnb[:, j:j + 1])
                    tt(P.dve, yt1[:], yt1[:], bonT[j][:], ALU.add, [yt1, bonT[j]], [yt1])
                    tt(P.dve, yaT[:, j, :], yt1[:], gT[j][:], ALU.mult, [yt1, gT[j]], [yaT])
                    stq(yaT_d[j * 128:(j + 1) * 128, t0:t0 + ST], yaT[:, j, :], [yaT], [yaT_d], is_out=(dbg == "yaT_d"), q=P.pool)
            P.barrier()
        P.stack = gs
        if dbg == "yaT_d":
            P.finish()
            return nc, P

        with ExitStack() as ph, nc.allow_non_contiguous_dma(reason="tiny per-channel vectors"):
            P.stack = ph
            C0 = RW
            winu = P.tile("winu", [128, 8, 512], BF16); winv = P.tile("winv", [128, 8, 512], BF16)
            wing = P.tile("wing", [128, 8, 2048], BF16)
            woa = P.tile("woa", [128, 4, D], BF16); wob = P.tile("wob", [128, 4, D], BF16); wo = P.tile("wo", [128, 8, D], BF16)
            for dc in range(8):
                ldc(winu[:, dc, :], w_in[dc * 128:(dc + 1) * 128, C0:C0 + 512], [winu])
                ldc(winv[:, dc, :], w_in[dc * 128:(dc + 1) * 128, C0 + 512:C0 + 1024], [winv])
                ldc(wing[:, dc, 0:1024], w_in[dc * 128:(dc + 1) * 128, C0 + 1024:C0 + 2048], [wing])
                ldc(wing[:, dc, 1024:2048], w_in[dc * 128:(dc + 1) * 128, C0 + 2048:C0 + 3072], [wing])
                ldc(wo[:, dc, :], w_out[dc * 128:(dc + 1) * 128, :], [wo])
            for q in range(4):
                ldc(woa[:, q, :], w_out_a[q * 128:(q + 1) * 128, :], [woa])
                ldc(wob[:, q, :], w_out_b[q * 128:(q + 1) * 128, :], [wob])
            mU = P.tile("mU", [128, 128], F32)
            ms(P.pool, mU[:], 1.0, [mU])
            P.op(P.pool, lambda h: h.affine_select(out=mU[:], in_=mU[:], pattern=[[1, 128]], compare_op=ALU.is_ge, fill=0.0, base=0, channel_multiplier=-1), r=[mU], w=[mU])
            wsf = P.tile("wsf", [128, 8, 128], F32)
            for g_ in range(8):
                ld(wsf[:, g_, :], gmlp_ws[g_], [wsf])
            wsmT = P.tile("wsmT", [128, 8, 128], BF16)
            for g4 in range(2):
                pb = bank()
                for gg in range(4):
                    tr(pb[:, gg * 128:(gg + 1) * 128], wsf[:, g4 * 4 + gg, :], identf[:], [wsf, identf], [pb])
                tt(P.dve, wsmT[:, g4 * 4:(g4 + 1) * 4, :], pb[:, :].rearrange("p (a t) -> p a t", t=128), mU[:, :].unsqueeze(1).to_broadcast([128, 4, 128]), ALU.mult, [pb, mU], [wsmT])
            bsT = P.tile("bsT", [128, 4, 128], F32)
            for g_ in range(8):
                ld(bsT[(g_ % 2) * 64:(g_ % 2) * 64 + 64, g_ // 2, :], gmlp_bs[g_].partition_broadcast(64), [bsT])
            lngbc = P.tile("lngbc", [128, 512], F32); lnbbc = P.tile("lnbbc", [128, 512], F32)
            ld(lngbc[:], gmlp_ln_g.partition_broadcast(128), [lngbc]); ld(lnbbc[:], gmlp_ln_b.partition_broadcast(128), [lnbbc])

            xts = [P.tile(f"bxt{i}", [128, D], F32) for i in range(2)]
            hT = P.tile("bhT", [128, 8, 512], BF16)
            ssq = P.tile("bssq", [128, 4], F32); rstd = P.tile("brstd", [128, 4], F32)
            xn = P.tile("bxn", [128, 4, D], BF16)
            ntmp = (ssq, rstd, xn, None)
            uT = P.tile("uT", [128, 4, 512], BF16)
            vg = P.tile("vg", [128, 512], F32); vc = P.tile("vc", [128, 512], F32)
            vst = P.tile("vst", [128, 4], F32)
            vln = P.tile("vln", [128, 4, 512], BF16)
            ybT = P.tile("ybT", [128, 4, 512], BF16)
            gts = P.tile("gts", [128, 16, 512], BF16)
            yaTs = P.tile("yaTs", [128, 4, 512], BF16)
            mgT = P.tile("mgT", [128, 8, 512], BF16)
            t1 = P.tile("t1", [128, 512], F32); t2_ = P.tile("t2_", [128, 512], F32)
            xo = [P.tile(f"xo{i}", [128, D], F32) for i in range(2)]
            for si in range(nst):
                t0 = si * ST
                norm_T(xts, hT, x, si, s1, sh1, ntmp)
                for q in range(4):
                    ld(yaTs[:, q, :], yaT_d[q * 128:(q + 1) * 128, t0:t0 + ST], [yaTs], r=[yaT_d])
                for q in range(4):
                    pb = bank()
                    for dc in range(8):
                        mm(pb[:, :], winu[:, dc, q * 128:(q + 1) * 128], hT[:, dc, :], dc == 0, dc == 7, [winu, hT], [pb])
                    act(uT[:, q, :], pb[:, :], AF.Gelu, [pb], [uT])
                for ti in range(4):
                    pb = bank()
                    for dc in range(8):
                        mm(pb[:, :], hT[:, dc, ti * 128:(ti + 1) * 128], winv[:, dc, :], dc == 0, dc == 7, [winv, hT], [pb])
                    act(vg[:], pb[:, :], AF.Gelu, [pb], [vg, vst], accum_out=vst[:, 0:1])
                    ts(P.dve, vst[:, 1:2], vst[:, 0:1], 1.0 / 512, None, ALU.mult, None, [vst], [vst])
                    ts(P.dve, vc[:], vg[:], vst[:, 1:2], None, ALU.subtract, None, [vg, vst], [vc])
                    act(vg[:], vc[:], AF.Square, [vc], [vg, vst], accum_out=vst[:, 2:3])
                    ts(P.dve, vst[:, 3:4], vst[:, 2:3], 1.0 / 512, 1e-5, ALU.mult, ALU.add, [vst], [vst])
                    act(vst[:, 3:4], vst[:, 3:4], AF.Sqrt, [vst], [vst])
                    P.op(P.dve, lambda h: h.reciprocal(out=vst[:, 3:4], in_=vst[:, 3:4]), r=[vst], w=[vst])
                    stt(P.dve, vc[:], vc[:], vst[:, 3:4], lngbc[:], ALU.mult, ALU.mult, [vc, vst, lngbc], [vc])
                    tt(P.dve, vln[:, ti, :], vc[:], lnbbc[:], ALU.add, [vc, lnbbc], [vln])
                for q in range(4):
                    pb = bank()
                    for ti in range(4):
                        for gg in range(2):
                            g_ = 2 * q + gg
                            mm(pb[gg * 64:(gg + 1) * 64, ti * 128:(ti + 1) * 128], vln[:, ti, g_ * 64:(g_ + 1) * 64], wsmT[:, g_, :], True, True, [vln, wsmT], [pb])
                    tt(P.dve, t1[:].rearrange("p (a t) -> p a t", t=128), pb[:, :].rearrange("p (a t) -> p a t", t=128), bsT[:, q, :].unsqueeze(1).to_broadcast([128, 4, 128]), ALU.add, [pb, bsT], [t1])
                    tt(P.dve, ybT[:, q, :], t1[:], uT[:, q, :], ALU.mult, [t1, uT], [ybT])
                for q in range(16):
                    pb = bank()
                    for dc in range(8):
                        mm(pb[:, :], wing[:, dc, q * 128:(q + 1) * 128], hT[:, dc, :], dc == 0, dc == 7, [wing, hT], [pb])
                    act(gts[:, q, :], pb[:, :], AF.Sigmoid, [pb], [gts])
                for m in range(8):
                    pa = bank()
                    for q in range(4):
                        mm(pa[:, :], woa[:, q, m * 128:(m + 1) * 128], yaTs[:, q, :], q == 0, q == 3, [woa, yaTs], [pa])
                    pb = bank()
                    for q in range(4):
                        mm(pb[:, :], wob[:, q, m * 128:(m + 1) * 128], ybT[:, q, :], q == 0, q == 3, [wob, ybT], [pb])
                    tt(P.dve, t1[:], pa[:, :], gts[:, m, :], ALU.mult, [pa, gts], [t1])
                    tt(P.dve, t2_[:], pb[:, :], gts[:, 8 + m, :], ALU.mult, [pb, gts], [t2_])
                    tt(P.dve, mgT[:, m, :], t1[:], t2_[:], ALU.add, [t1, t2_], [mgT])
                for ti in range(4):
                    xt = xts[ti % 2]; xo_ = xo[ti % 2]
                    ld(xt[:], x[t0 + ti * 128:t0 + (ti + 1) * 128, :], [xt])
                    for hf in range(2):
                        pb = bank()
                        for m in range(8):
                            mm(pb[:, :], mgT[:, m, ti * 128:(ti + 1) * 128], wo[:, m, hf * 512:(hf + 1) * 512], m == 0, m == 7, [mgT, wo], [pb])
                        tt(P.dve, t1[:], pb[:, :], g1bc[:, hf * 512:(hf + 1) * 512], ALU.mult, [pb, g1bc], [t1])
                        tt(P.dve, xo_[:, hf * 512:(hf + 1) * 512], t1[:], xt[:, hf * 512:(hf + 1) * 512], ALU.add, [t1, xt], [xo_])
                    stq(x1_d[t0 + ti * 128:t0 + (ti + 1) * 128, :], xo_[:], [xo_], [x1_d], is_out=(dbg == "x1_d"), q=P.pool)
            P.barrier()
        P.stack = gs
        if dbg == "x1_d":
            P.finish()
            return nc, P

        NT = nst * 4
        dest8 = P.tile("dest8", [128, 64, 8], I32)
        w8 = P.tile("w8", [128, 64, 8], F32)
        idxw = P.tile("idxw", [128, NBLK], I32)
        w8b = [Buf(f"w8b{i}") for i in range(64)]; d8b = [Buf(f"d8b{i}") for i in range(64)]
        with ExitStack() as ph, nc.allow_non_contiguous_dma(reason="tiny per-channel vectors"):
            P.stack = ph
            zt = P.tile("zt", [128, 8192], BF16)
            ms(P.pool, zt[:], 0.0, [zt])
            nzb = NSLOT // 1024
            for i in range(nzb):
                stq(xg_d[i * 1024:(i + 1) * 1024, :].rearrange("(p r) d -> p (r d)", p=128), zt[:], [zt], [xg_d])
            rw = P.tile("rw", [128, 8, NE], BF16)
            sw1 = P.tile("sw1", [128, 8, 256], BF16); sw3 = P.tile("sw3", [128, 8, 256], BF16); sw2 = P.tile("sw2", [128, 2, D], BF16)
            for dc in range(8):
                ldc(rw[:, dc, :], router_w[dc * 128:(dc + 1) * 128, :], [rw])
                ldc(sw1[:, dc, :], shared_w1[dc * 128:(dc + 1) * 128, :], [sw1])
                ldc(sw3[:, dc, :], shared_w3[dc * 128:(dc + 1) * 128, :], [sw3])
            for fc in range(2):
                ldc(sw2[:, fc, :], shared_w2[fc * 128:(fc + 1) * 128, :], [sw2])
            rbias = P.tile("rbias", [128, NE], F32)
            ld(rbias[:], router_bias.partition_broadcast(128), [rbias])
            eoff = P.tile("eoff", [128, NE], F32)
            ustr = P.tile("ustr", [128, 128], BF16)
            onesb = P.tile("onesb", [128, 128], BF16)
            cp(P.dve, ustr[:], mU[:], [mU], [ustr]) if False else None
            uf = P.tile("uf", [128, 128], F32)
            ms(P.pool, uf[:], 1.0, [uf])
            P.op(P.pool, lambda h: h.affine_select(out=uf[:], in_=uf[:], pattern=[[1, 128]], compare_op=ALU.is_gt, fill=0.0, base=0, channel_multiplier=-1), r=[uf], w=[uf])
            cp(P.dve, ustr[:], uf[:], [uf], [ustr])
            ms(P.dve, onesb[:], 1.0, [onesb])
            basec = P.tile("basec", [128, NE], F32)
            ms(P.dve, basec[:], 0.0, [basec])

            xts = [P.tile(f"cxt{i}", [128, D], F32) for i in range(2)]
            hT = P.tile("chT", [128, 8, 512], BF16)
            ssq = P.tile("cssq", [128, 4], F32); rstd = P.tile("crstd", [128, 4], F32)
            xn = P.tile("cxn", [128, 4, D], BF16)
            ntmp = (ssq, rstd, xn, None)
            h2row = [P.tile(f"h2row{i}", [128, D], BF16) for i in range(2)]
            class _S:
                pass

            def mkset(n):
                S = _S()
                S.sc_ = P.tile(f"sc_{n}", [128, NE], F32); S.sel = P.tile(f"sel{n}", [128, NE], F32)
                S.m88 = P.tile(f"m88{n}", [128, 8, 8], F32); S.gs_ = P.tile(f"gs_{n}", [128, 8], F32)
                S.g8 = P.tile(f"g8{n}", [128, 8], F32); S.gmask = P.tile(f"gmask{n}", [128, 8], F32)
                S.selm = P.tile(f"selm{n}", [128, NE], F32); S.smask = P.tile(f"smask{n}", [128, NE], F32)
                S.smb = P.tile(f"smb{n}", [128, NE], BF16)
                S.wd = P.tile(f"wd{n}", [128, NE], F32); S.wsum = P.tile(f"wsum{n}", [128, 2], F32)
                S.key = P.tile(f"key{n}", [128, NE], F32); S.k8 = P.tile(f"k8{n}", [128, 8], F32)
                S.kz = P.tile(f"kz{n}", [128, 8], F32); S.jk = P.tile(f"jk{n}", [128, NE], F32)
                S.t2_ = P.tile(f"ct2{n}", [128, 512], F32)
                return S
            SS = [mkset(0), mkset(1)]
            hsT = P.tile("hsT", [128, 2, 512], BF16)
            t1 = P.tile("ct1", [128, 512], F32)
            xo = [P.tile(f"cxo{i}", [128, D], F32) for i in range(2)]

            def route(tsl, S):
                pb = bank()
                for dc in range(8):
                    mm(pb[:, 0:NE], hT[:, dc, tsl], rw[:, dc, :], dc == 0, dc == 7, [hT, rw], [pb])
                act(S.sc_[:], pb[:, 0:NE], AF.Sigmoid, [pb], [S.sc_])
                yield
                tt(P.pool, S.sel[:], S.sc_[:], rbias[:], ALU.add, [S.sc_, rbias], [S.sel])
                yield
                for g_ in range(8):
                    P.op(P.dve, lambda h: h.max(out=S.m88[:, g_, :], in_=S.sel[:, g_ * 32:(g_ + 1) * 32]), r=[S.sel], w=[S.m88])
                yield
                tt(P.dve, S.gs_[:], S.m88[:, :, 0], S.m88[:, :, 1], ALU.add, [S.m88], [S.gs_])
                yield
                P.op(P.dve, lambda h: h.max(out=S.g8[:], in_=S.gs_[:]), r=[S.gs_], w=[S.g8])
                yield
                ts(P.dve, S.gmask[:], S.gs_[:], S.g8[:, 3:4], None, ALU.is_ge, None, [S.gs_, S.g8], [S.gmask])
                yield
                stt(P.dve, S.selm[:].rearrange("p (g e) -> p g e", e=32), S.sel[:].rearrange("p (g e) -> p g e", e=32), 2.0, S.gmask[:, :].unsqueeze(2).to_broadcast([128, 8, 32]), ALU.add, ALU.mult, [S.sel, S.gmask], [S.selm])
                yield
                P.op(P.dve, lambda h: h.max(out=S.g8[:], in_=S.selm[:]), r=[S.selm], w=[S.g8])
                yield
                ts(P.dve, S.smask[:], S.selm[:], S.g8[:, 7:8], None, ALU.is_ge, None, [S.selm, S.g8], [S.smask])
                yield
                cp(P.pool, S.smb[:], S.smask[:], [S.smask], [S.smb])
                yield

            def drive(*gens):
                gens = [g for g in gens if g is not None]
                while gens:
                    for g in list(gens):
                        try:
                            next(g)
                        except StopIteration:
                            gens.remove(g)

            def gen_p1(ti, S):
                yield from route(slice(ti * 128, (ti + 1) * 128), S)
                pp = bank()
                mm(pp[:, 0:NE], onesb[:], S.smb[:], True, True, [onesb, S.smb], [pp])
                tt(P.dve, basec[:], basec[:], pp[:, 0:NE], ALU.add, [pp, basec], [basec])
                yield

            for si in range(nst):
                norm_T(xts, hT, x1_d.t, si, s2, sh2, ntmp)
                drive(gen_p1(0, SS[0]), gen_p1(1, SS[1]))
                drive(gen_p1(2, SS[0]), gen_p1(3, SS[1]))
            nblk = P.tile("nblk", [128, NE], F32); pends = P.tile("pends", [128, NE], F32)
            ones256 = P.tile("ones256", [128, NE], F32)
            ms(P.dve, nblk[:], 0.0, [nblk]); ms(P.dve, ones256[:], 1.0, [ones256])
            for m_ in range(T // BLK):
                stt(P.dve, nblk[:], basec[:], float(BLK * m_), nblk[:], ALU.is_gt, ALU.add, [basec, nblk], [nblk])
            ts(P.dve, nblk[:], nblk[:], float(BLK), None, ALU.mult, None, [nblk], [nblk])
            P.op(P.dve, lambda h: h.tensor_tensor_scan(out=pends[:], data0=ones256[:], data1=nblk[:], initial=0.0, op0=ALU.mult, op1=ALU.add), r=[ones256, nblk], w=[pends])
            tt(P.dve, eoff[:], pends[:], nblk[:], ALU.subtract, [pends, nblk], [eoff])
            ts(P.dve, eoff[:], eoff[:], 1.0, None, ALU.add, None, [eoff], [eoff])
            pcol = P.tile("pcol", [128, 2], F32)
            for c_ in range(2):
                pb = bank()
                tr(pb[:, 0:128], pends[:, c_ * 128:(c_ + 1) * 128], identf[:], [pends, identf], [pb])
                cp(P.dve, pcol[:, c_:c_ + 1], pb[:, 0:1], [pb], [pcol])
            iotab = P.tile("iotab", [128, NBLK], F32)
            P.op(P.pool, lambda h: h.iota(iotab[:], pattern=[[BLK, NBLK]], base=0, channel_multiplier=0, allow_small_or_imprecise_dtypes=True), w=[iotab])
            cmpb = P.tile("cmpb", [128, 2, NBLK], BF16)
            for c_ in range(2):
                ts(P.dve, cmpb[:, c_, :], iotab[:], pcol[:, c_:c_ + 1], None, ALU.is_ge, None, [iotab, pcol], [cmpb])
            pb = bank()
            for c_ in range(2):
                mm(pb[:, :], onesb[:], cmpb[:, c_, :], c_ == 0, c_ == 1, [onesb, cmpb], [pb])
            pidx = P.tile("pidx", [128, NBLK], F32)
            P.op(P.pool, lambda h: h.iota(pidx[:], pattern=[[0, NBLK]], base=0, channel_multiplier=1, allow_small_or_imprecise_dtypes=True), w=[pidx])
            ts(P.dve, iotab[:], pb[:, :], 255.0, 128.0, ALU.min, ALU.mult, [pb], [iotab])
            tt(P.dve, idxw[:], iotab[:], pidx[:], ALU.add, [iotab, pidx], [idxw])
            ms(P.dve, basec[:], 0.0, [basec])

            def gen_p2(si, ti, S):
                t0 = si * ST
                tg = si * 4 + ti
                tsl = slice(ti * 128, (ti + 1) * 128)
                hr = h2row[ti % 2]
                bb = bbank()
                for dc in range(8):
                    tr(bb[:, dc * 128:(dc + 1) * 128], hT[:, dc, tsl], identb[:], [hT, identb], [bb])
                cp(P.dve, hr[:], bb[:, :], [bb], [hr])
                yield
                yield from route(tsl, S)
                stt(P.dve, S.wd[:], S.smask[:], 1.0, S.sc_[:], ALU.mult, ALU.mult, [S.smask, S.sc_], [S.wd, S.wsum], accum_out=S.wsum[:, 0:1])
                yield
                P.op(P.dve, lambda h: h.reciprocal(out=S.wsum[:, 1:2], in_=S.wsum[:, 0:1]), r=[S.wsum], w=[S.wsum])
                yield
                ts(P.dve, S.wd[:], S.wd[:], S.wsum[:, 1:2], 2.5, ALU.mult, ALU.mult, [S.wd, S.wsum], [S.wd])
                pp = bank()
                mm(pp[:, 0:NE], ustr[:], S.smb[:], True, True, [ustr, S.smb], [pp])
                mm(pp[:, NE:2 * NE], onesb[:], S.smb[:], True, True, [onesb, S.smb], [pp])
                tt(P.dve, S.key[:], pp[:, 0:NE], basec[:], ALU.add, [pp, basec], [S.key])
                tt(P.dve, basec[:], basec[:], pp[:, NE:2 * NE], ALU.add, [pp, basec], [basec])
                yield
                tt(P.pool, S.key[:], S.key[:], eoff[:], ALU.add, [S.key, eoff], [S.key])
                yield
                tt(P.pool, S.key[:], S.key[:], S.smask[:], ALU.mult, [S.key, S.smask], [S.key])
                yield
                P.op(P.dve, lambda h: h.max(out=S.k8[:], in_=S.key[:]), r=[S.key], w=[S.k8])
                yield
                for k in range(8):
                    stt(P.dve, S.jk[:], S.key[:], S.k8[:, k:k + 1], S.wd[:], ALU.is_equal, ALU.mult, [S.key, S.k8, S.wd], [S.jk, w8b[tg]], accum_out=w8[:, tg, k:k + 1])
                    yield
                ts(P.dve, S.kz[:], S.k8[:], 0.0, float(NSLOT), ALU.is_equal, ALU.mult, [S.k8], [S.kz])
                yield
                stt(P.dve, dest8[:, tg, :], S.k8[:], -1.0, S.kz[:], ALU.add, ALU.add, [S.k8, S.kz], [d8b[tg]])
                yield
                for k in range(8):
                    l = LP[lpi[0] % len(LP)]; lpi[0] += 1
                    P.dma(P.pool, l, lambda h: h.indirect_dma_start(out=xg_d.t, out_offset=bass.IndirectOffsetOnAxis(ap=dest8[:, tg, k:k + 1], axis=0), in_=hr[:], in_offset=None), r=[hr, d8b[tg]], w=[xg_d])
                yield
                xt = xts[ti % 2]; xo_ = xo[ti % 2]
                ld(xt[:], x1_d[t0 + ti * 128:t0 + (ti + 1) * 128, :], [xt])
                for hf in range(2):
                    pb = bank()
                    for fc in range(2):
                        mm(pb[:, :], hsT[:, fc, tsl], sw2[:, fc, hf * 512:(hf + 1) * 512], fc == 0, fc == 1, [hsT, sw2], [pb])
                    tt(P.dve, S.t2_[:], pb[:, :], g2bc[:, hf * 512:(hf + 1) * 512], ALU.mult, [pb, g2bc], [S.t2_])
                    yield
                    tt(P.dve, xo_[:, hf * 512:(hf + 1) * 512], S.t2_[:], xt[:, hf * 512:(hf + 1) * 512], ALU.add, [S.t2_, xt], [xo_])
                    yield
                stq(x1_d[t0 + ti * 128:t0 + (ti + 1) * 128, :], xo_[:], [xo_], [x1_d], q=P.act)
                yield

            for si in range(nst):
                norm_T(xts, hT, x1_d.t, si, s2, sh2, ntmp)
                for fc in range(2):
                    p1 = bank()
                    for dc in range(8):
                        mm(p1[:, :], sw1[:, dc, fc * 128:(fc + 1) * 128], hT[:, dc, :], dc == 0, dc == 7, [sw1, hT], [p1])
                    p3 = bank()
                    for dc in range(8):
                        mm(p3[:, :], sw3[:, dc, fc * 128:(fc + 1) * 128], hT[:, dc, :], dc == 0, dc == 7, [sw3, hT], [p3])
                    act(t1[:], p1[:, :], AF.Silu, [p1], [t1])
                    tt(P.dve, hsT[:, fc, :], t1[:], p3[:, :], ALU.mult, [t1, p3], [hsT])
                drive(gen_p2(si, 0, SS[0]), gen_p2(si, 1, SS[1]))
                drive(gen_p2(si, 2, SS[0]), gen_p2(si, 3, SS[1]))
            P.barrier()
            if dbg == "pB":
                P.finish()
                raise StopBuild()
        P.stack = gs

        with ExitStack() as ph:
            P.stack = ph
            w1v = exp_w1.rearrange("e (p c) f -> (e p) (c f)", c=8)
            w3v = exp_w3.rearrange("e (p c) f -> (e p) (c f)", c=8)
            w2v = exp_w2.rearrange("e (p c) d -> (e p) (c d)", c=2)
            xgt = [P.tile(f"xgt{i}", [128, 2, D], BF16) for i in range(3)]
            xgT = [P.tile(f"xgT{i}", [128, 8, BLK], BF16) for i in range(2)]
            w1b = [P.tile(f"w1b{i}", [128, 2048], BF16) for i in range(3)]
            w3b = [P.tile(f"w3b{i}", [128, 2048], BF16) for i in range(3)]
            w2b = [P.tile(f"w2b{i}", [128, 2048], BF16) for i in range(3)]
            hid = [P.tile(f"hid{i}", [128, 2, BLK], BF16) for i in range(2)]
            st1 = [P.tile(f"st1{i}", [128, BLK], F32) for i in range(2)]
            yrow = [P.tile(f"yrow{i}", [128, 2, D], BF16) for i in range(2)]
            LY = P.lanes(2, "ly")

            def wgather(dst, src, i_):
                l = LP[lpi[0] % len(LP)]; lpi[0] += 1
                P.dma(P.pool, l, lambda h: h.indirect_dma_start(out=dst[:], out_offset=None, in_=src, in_offset=bass.IndirectOffsetOnAxis(ap=idxw[:, i_:i_ + 1], axis=0)), r=[idxw], w=[dst])

            def c_loads(i_):
                i3 = i_ % 3
                ld(xgt[i3][:], xg_d[i_ * BLK:(i_ + 1) * BLK, :].rearrange("(b p) d -> p b d", p=128), [xgt[i3]], r=[xg_d])
                wgather(w1b[i3], w1v, i_); wgather(w3b[i3], w3v, i_); wgather(w2b[i3], w2v, i_)

            def c_T(i_):
                i3 = i_ % 3; i2 = i_ % 2
                xv = xgt[i3][:].rearrange("p b (q c) -> p b c q", c=8)
                for dc in range(8):
                    bb = bbank()
                    for b_ in range(2):
                        tr(bb[:, b_ * 128:(b_ + 1) * 128], xv[:, b_, dc, :], identb[:], [xgt[i3], identb], [bb])
                    cp(P.dve, xgT[i2][:, dc, :], bb[:, 0:BLK], [bb], [xgT[i2]])

            def c_H(i_):
                i3 = i_ % 3; i2 = i_ % 2
                w1r = w1b[i3][:].rearrange("p (c m two) -> p c two m", c=8, two=2)
                w3r = w3b[i3][:].rearrange("p (c m two) -> p c two m", c=8, two=2)
                for fc in range(2):
                    p1 = bank()
                    for dc in range(8):
                        mm(p1[:, 0:BLK], w1r[:, dc, fc, :], xgT[i2][:, dc, :], dc == 0, dc == 7, [w1b[i3], xgT[i2]], [p1])
                    p3 = bank()
                    for dc in range(8):
                        mm(p3[:, 0:BLK], w3r[:, dc, fc, :], xgT[i2][:, dc, :], dc == 0, dc == 7, [w3b[i3], xgT[i2]], [p3])
                    act(st1[fc][:], p1[:, 0:BLK], AF.Silu, [p1], [st1[fc]])
                    tt(P.dve, hid[i2][:, fc, :], st1[fc][:], p3[:, 0:BLK], ALU.mult, [st1[fc], p3], [hid[i2]])

            def c_Y(i_):
                i3 = i_ % 3; i2 = i_ % 2
                w2r = w2b[i3][:].rearrange("p (c d) -> p c d", c=2)
                for b_ in range(2):
                    for hf in range(2):
                        pb = bank()
                        for fc in range(2):
                            mm(pb[:, :], hid[i2][:, fc, b_ * 128:(b_ + 1) * 128], w2r[:, fc, hf * 512:(hf + 1) * 512], fc == 0, fc == 1, [hid[i2], w2b[i3]], [pb])
                        act(yrow[i2][:, b_, hf * 512:(hf + 1) * 512], pb[:, :], AF.Copy, [pb], [yrow[i2]])
                P.dma(P.act, LY[i2], lambda h: h.dma_start(out=yg_d[i_ * BLK:(i_ + 1) * BLK, :].rearrange("(b p) d -> p b d", p=128), in_=yrow[i2][:]), r=[yrow[i2]], w=[yg_d])

            nb_ = nblk_run
            c_loads(0)
            if nb_ > 1:
                c_loads(1)
            c_T(0)
            for i_ in range(nb_):
                if i_ + 1 < nb_:
                    c_T(i_ + 1)
                c_H(i_)
                if i_ >= 1:
                    c_Y(i_ - 1)
                if i_ + 2 < nb_:
                    c_loads(i_ + 2)
            c_Y(nb_ - 1)
            P.barrier()
            if dbg == "pC":
                P.finish()
                raise StopBuild()
        P.stack = gs

        with ExitStack() as ph:
            P.stack = ph
            nfbc = P.tile("nfbc", [128, D], F32)
            ld(nfbc[:], normf_g.partition_broadcast(128), [nfbc])
            xts = [P.tile(f"dxt{i}", [128, D], F32) for i in range(2)]
            gat = [P.tile(f"gat{i}", [128, D], BF16) for i in range(4)]
            acc = P.tile("acc", [128, D], F32)
            ot = [P.tile(f"ot{i}", [128, D], F32) for i in range(2)]
            fs = P.tile("fs", [128, 2], F32)
            jk2 = P.tile("jk2", [128, D], BF16)
            for tg in range(NT):
                xt = xts[tg % 2]; o_ = ot[tg % 2]
                ld(xt[:], x1_d[tg * 128:(tg + 1) * 128, :], [xt])
                for k in range(8):
                    gt = gat[k % 4]
                    l = LP[lpi[0] % len(LP)]; lpi[0] += 1
                    P.dma(P.pool, l, lambda h: h.indirect_dma_start(out=gt[:], out_offset=None, in_=yg_d.t, in_offset=bass.IndirectOffsetOnAxis(ap=dest8[:, tg, k:k + 1], axis=0)), r=[yg_d, d8b[tg]], w=[gt])
                    if k == 0:
                        ts(P.dve, acc[:], gt[:], w8[:, tg, 0:1], None, ALU.mult, None, [gt, w8b[tg]], [acc])
                    else:
                        stt(P.dve, acc[:], gt[:], w8[:, tg, k:k + 1], acc[:], ALU.mult, ALU.add, [gt, w8b[tg], acc], [acc])
                tt(P.dve, acc[:], acc[:], g2bc[:], ALU.mult, [acc, g2bc], [acc])
                tt(P.dve, acc[:], acc[:], xt[:], ALU.add, [acc, xt], [acc])
                act(jk2[:], acc[:], AF.Square, [acc], [jk2, fs], accum_out=fs[:, 0:1])
                ts(P.dve, fs[:, 1:2], fs[:, 0:1], 1.0 / D, 1e-6, ALU.mult, ALU.add, [fs], [fs])
                act(fs[:, 1:2], fs[:, 1:2], AF.Sqrt, [fs], [fs])
                P.op(P.dve, lambda h: h.reciprocal(out=fs[:, 1:2], in_=fs[:, 1:2]), r=[fs], w=[fs])
                stt(P.dve, o_[:], acc[:], fs[:, 1:2], nfbc[:], ALU.mult, ALU.mult, [acc, fs, nfbc], [o_])
                stq(out[tg * 128:(tg + 1) * 128, :], o_[:], [o_], is_out=True, q=P.act)
            P.finish()
        P.stack = gs
        return nc, P


_NAMES = ["ada_w", "ada_b", "norm1_g", "norm2_g", "w_in", "tshift_mu", "rwkv_w0", "rwkv_w_up", "rwkv_a0", "rwkv_a_up",
          "rwkv_g_up", "rwkv_k_k", "rwkv_k_a", "rwkv_r_k", "rwkv_ln_g", "rwkv_ln_b", "gmlp_ln_g", "gmlp_ln_b", "gmlp_ws",
          "gmlp_bs", "w_out_a", "w_out_b", "w_out", "router_w", "router_bias", "exp_w1", "exp_w3", "exp_w2",
          "shared_w1", "shared_w3", "shared_w2"]


def kernel(**inputs):
    nc, _ = build()
    shared = {}
    for k in _NAMES:
        a = np.asarray(inputs[k], dtype=np.float32)[0]
        if k == "rwkv_r_k":
            a = a.reshape(512)
        shared[k] = np.ascontiguousarray(a)
    shared["normf_g"] = np.ascontiguousarray(np.asarray(inputs["normf_g"], dtype=np.float32))
    x = np.asarray(inputs["x"], dtype=np.float32)
    c = np.asarray(inputs["c"], dtype=np.float32)
    in_maps = []
    for b in range(8):
        m = dict(shared)
        m["x"] = np.ascontiguousarray(x[b])
        m["c"] = np.ascontiguousarray(c[b:b + 1])
        in_maps.append(m)
    res = run_bass_kernel_spmd(nc, in_maps, core_ids=list(range(8)))
    return np.stack([np.asarray(r["out"], dtype=np.float32) for r in res.results], axis=0)
```

```python
import numpy as np
import concourse.bass as bass
import concourse.mybir as mybir
from concourse.bass_utils import run_bass_kernel_spmd

F32 = mybir.dt.float32
BF16 = mybir.dt.bfloat16
U32 = mybir.dt.uint32
I32 = mybir.dt.int32
AF = mybir.ActivationFunctionType
ALU = mybir.AluOpType
AX = mybir.AxisListType


class Buf:
    __slots__ = ("name", "w", "rs")

    def __init__(self, name):
        self.name = name
        self.w = None
        self.rs = []


class Tl:
    def __init__(self, t, name):
        self.t = t
        self.b = Buf(name)

    def __getitem__(self, k):
        return self.t[k]


class Eng:
    def __init__(self, P, name, h, sem):
        self.P = P
        self.name = name
        self.h = h
        self.sem = sem
        self.cnt = 0
        self.waited = {}


class Lane:
    def __init__(self, sem):
        self.sem = sem
        self.val = 0


class Prog:
    def __init__(self, nc, stack):
        self.nc = nc
        self.stack = stack
        mk = lambda n: stack.enter_context(nc.semaphore(n))
        self.pe = Eng(self, "pe", nc.tensor, mk("s_pe"))
        self.act = Eng(self, "act", nc.scalar, mk("s_act"))
        self.dve = Eng(self, "dve", nc.vector, mk("s_dve"))
        self.pool = Eng(self, "pool", nc.gpsimd, mk("s_pool"))
        self.sp = Eng(self, "sp", nc.sync, mk("s_sp"))
        self.engs = [self.pe, self.act, self.dve, self.pool, self.sp]
        self.nlanes = 0
        self.all_lanes = []
        self.out_toks = []
        self.nins = 0

    def tile(self, name, shape, dt):
        return Tl(self.sb(name, shape, dt), name)

    def ptile(self, name, shape, dt=F32):
        return Tl(self.ps(name, shape, dt), name)

    def barrier(self):
        toks = [(e.sem, e.cnt) for e in self.engs if e.cnt] + [(l.sem, l.val) for l in self.all_lanes if l.val]
        for e in self.engs:
            for t in toks:
                self._wait(e, t)

    def sb(self, name, shape, dt):
        return self.stack.enter_context(self.nc.sbuf_tensor(name, shape, dt))

    def ps(self, name, shape, dt=F32):
        return self.stack.enter_context(self.nc.psum_tensor(name, shape, dt))

    def lanes(self, n, name="ln"):
        out = []
        for i in range(n):
            out.append(Lane(self.stack.enter_context(self.nc.semaphore(f"{name}{self.nlanes}"))))
            self.nlanes += 1
        self.all_lanes += out
        return out

    def _wait(self, e, tok):
        if tok is None:
            return
        sem, val = tok
        k = id(sem)
        if e.waited.get(k, 0) >= val:
            return
        if sem is e.sem and e is self.pe:
            return
        e.h.wait_ge(sem, val)
        e.waited[k] = val
        self.nins += 1

    def _deps(self, e, r, w):
        r = [getattr(b, "b", b) for b in r]
        w = [getattr(b, "b", b) for b in w]
        for b in r:
            self._wait(e, b.w)
        for b in w:
            self._wait(e, b.w)
            for t in b.rs:
                self._wait(e, t)

    def _commit(self, tok, r, w):
        r = [getattr(b, "b", b) for b in r]
        w = [getattr(b, "b", b) for b in w]
        for b in r:
            b.rs.append(tok)
            if len(b.rs) > 24:
                d = {}
                for s, v in b.rs:
                    if id(s) not in d or d[id(s)][1] < v:
                        d[id(s)] = (s, v)
                b.rs = list(d.values())
        for b in w:
            b.w = tok
            b.rs = []

    def op(self, e, fn, r=(), w=()):
        self._deps(e, r, w)
        ins = fn(e.h)
        e.cnt += 1
        ins.then_inc(e.sem, 1)
        tok = (e.sem, e.cnt)
        self._commit(tok, r, w)
        self.nins += 1
        return tok

    def dma(self, e, lane, fn, r=(), w=(), is_out=False):
        self._wait(e, (lane.sem, lane.val) if lane.val else None)
        self._deps(e, r, w)
        ins = fn(e.h)
        lane.val += 16
        ins.then_inc(lane.sem, 16)
        tok = (lane.sem, lane.val)
        self._commit(tok, r, w)
        if is_out:
            self.out_toks.append(tok)
        self.nins += 1
        return tok

    def finish(self):
        for tok in self.out_toks:
            self._wait(self.sp, tok)


from contextlib import ExitStack

T = 8192
D = 1024
ST = 512
NST = T // ST
SDEC = -0.6065306597126334
RW = 1792
CAP = 512
BLK = 256
NBLK = 512
NE = 256
NSLOT = NE * CAP
ROW = 1024 + 64


def sl(h_):
    return (h_ % 2) * 4 + h_ // 2


class StopBuild(Exception):
    pass


def build(dbg=None, nst=NST, nblk_run=NBLK):
    nc = bass.Bass("TRN2", target_bir_lowering=False)
    holder = {}
    try:
        return _build(nc, dbg, nst, holder, nblk_run)
    except StopBuild:
        return nc, holder["P"]


def _build(nc, dbg, nst, holder, nblk_run):

    def din(name, shape, dt=F32):
        return nc.dram_tensor(name, shape, dt, kind="ExternalInput").ap()

    x = din("x", [T, D]); c = din("c", [1, D])
    ada_w = din("ada_w", [D, 6 * D]); ada_b = din("ada_b", [6 * D])
    norm1_g = din("norm1_g", [D]); norm2_g = din("norm2_g", [D])
    w_in = din("w_in", [D, 4864]); tshift_mu = din("tshift_mu", [RW])
    rwkv_w0 = din("rwkv_w0", [512]); rwkv_w_up = din("rwkv_w_up", [64, 512])
    rwkv_a0 = din("rwkv_a0", [512]); rwkv_a_up = din("rwkv_a_up", [64, 512])
    rwkv_g_up = din("rwkv_g_up", [128, 512]); rwkv_k_k = din("rwkv_k_k", [512])
    rwkv_k_a = din("rwkv_k_a", [512]); rwkv_r_k = din("rwkv_r_k", [512])
    rwkv_ln_g = din("rwkv_ln_g", [512]); rwkv_ln_b = din("rwkv_ln_b", [512])
    gmlp_ln_g = din("gmlp_ln_g", [512]); gmlp_ln_b = din("gmlp_ln_b", [512])
    gmlp_ws = din("gmlp_ws", [8, 128, 128]); gmlp_bs = din("gmlp_bs", [8, 128])
    w_out_a = din("w_out_a", [512, D]); w_out_b = din("w_out_b", [512, D]); w_out = din("w_out", [D, D])
    router_w = din("router_w", [D, NE]); router_bias = din("router_bias", [NE])
    if True:
        exp_w1 = din("exp_w1", [NE, D, 256]); exp_w3 = din("exp_w3", [NE, D, 256]); exp_w2 = din("exp_w2", [NE, 256, D])
    shared_w1 = din("shared_w1", [D, 256]); shared_w3 = din("shared_w3", [D, 256]); shared_w2 = din("shared_w2", [256, D])
    normf_g = din("normf_g", [D])
    out = nc.dram_tensor("out", [T, D], F32, kind="ExternalOutput").ap()

    def dscr(name, shape, dt):
        k = "ExternalOutput" if dbg == name else "Internal"
        return Tl(nc.dram_tensor(name, shape, dt, kind=k).ap(), name)

    yaT_d = dscr("yaT_d", [512, T], BF16)
    x1_d = dscr("x1_d", [T, D], F32)
    xg_d = dscr("xg_d", [NSLOT, D], BF16)
    yg_d = dscr("yg_d", [NSLOT, D], BF16)

    with ExitStack() as gs:
        P = Prog(nc, gs)
        holder["P"] = P

        def ck(n, tl, ap=None):
            if dbg == f"ck{n}":
                o_ = nc.dram_tensor("o_ck", list((ap if ap is not None else tl[:]).shape), (ap if ap is not None else tl[:]).dtype, kind="ExternalOutput").ap()
                stq(o_, ap if ap is not None else tl[:], [tl], is_out=True)
                P.finish()
                raise StopBuild()
        LD = P.lanes(6, "ld")
        LP = P.lanes(4, "lp")
        LS = P.lanes(4, "lst")
        ldi = [0]; lpi = [0]; lsi = [0]

        def ld(out_ap, in_ap, w, r=()):
            l = LD[ldi[0] % len(LD)]; ldi[0] += 1
            return P.dma(P.sp, l, lambda h: h.dma_start(out=out_ap, in_=in_ap), r=r, w=w)

        def ldc(out_ap, in_ap, w, r=()):
            l = LP[lpi[0] % len(LP)]; lpi[0] += 1
            return P.dma(P.pool, l, lambda h: h.dma_start(out=out_ap, in_=in_ap), r=r, w=w)

        LSQ = {"sp": LS, "pool": P.lanes(3, "lsp"), "act": P.lanes(3, "lsa")}

        def stq(out_ap, in_ap, r, w=(), is_out=False, q=None):
            q = q or P.sp
            ll = LSQ[q.name]
            l = ll[lsi[0] % len(ll)]; lsi[0] += 1
            return P.dma(q, l, lambda h: h.dma_start(out=out_ap, in_=in_ap), r=r, w=w, is_out=is_out)

        pbanks = [P.ptile(f"pb{i}", [128, 512], F32) for i in range(6)]
        bbanks = [P.ptile(f"bb{i}", [128, 1024], BF16) for i in range(2)]
        pbi = [0]; bbi = [0]

        def bank():
            b = pbanks[pbi[0] % 6]; pbi[0] += 1
            return b

        def bbank():
            b = bbanks[bbi[0] % 2]; bbi[0] += 1
            return b

        def mm(o, lhsT, rhs, start, stop, r, w):
            return P.op(P.pe, lambda h: h.matmul(o, lhsT=lhsT, rhs=rhs, start=start, stop=stop), r=r, w=w)

        def tr(o, in_, ident, r, w):
            return P.op(P.pe, lambda h: h.transpose(out=o, in_=in_, identity=ident), r=r, w=w)

        def act(o, in_, func, r, w, **kw):
            return P.op(P.act, lambda h: h.activation(out=o, in_=in_, func=func, **kw), r=r, w=w)

        def tt(e, o, a, b, op, r, w):
            return P.op(e, lambda h: h.tensor_tensor(out=o, in0=a, in1=b, op=op), r=r, w=w)

        def ts(e, o, a, s1, s2, op0, op1, r, w):
            if s2 is None:
                return P.op(e, lambda h: h.tensor_scalar(out=o, in0=a, scalar1=s1, scalar2=None, op0=op0), r=r, w=w)
            return P.op(e, lambda h: h.tensor_scalar(out=o, in0=a, scalar1=s1, scalar2=s2, op0=op0, op1=op1), r=r, w=w)

        def stt(e, o, a, s, b, op0, op1, r, w, **kw):
            return P.op(e, lambda h: h.scalar_tensor_tensor(out=o, in0=a, scalar=s, in1=b, op0=op0, op1=op1, **kw), r=r, w=w)

        def cp(e, o, a, r, w):
            return P.op(e, lambda h: h.tensor_copy(out=o, in_=a), r=r, w=w)

        def ms(e, o, v, w, r=()):
            return P.op(e, lambda h: h.memset(o, v), r=r, w=w)

        identf = P.tile("identf", [128, 128], F32)
        identb = P.tile("identb", [128, 128], BF16)
        ms(P.pool, identf[:], 0.0, [identf])
        P.op(P.pool, lambda h: h.affine_select(out=identf[:], in_=identf[:], pattern=[[-1, 128]], compare_op=ALU.not_equal, fill=1.0, base=0, channel_multiplier=1), r=[identf], w=[identf])
        cp(P.dve, identb[:], identf[:], [identf], [identb])
        maskCM = P.tile("maskCM", [128, 512], F32)
        mAT = P.tile("mAT", [128, 128], F32)
        ms(P.pool, maskCM[:], 1.0, [maskCM])
        P.op(P.pool, lambda h: h.affine_select(out=maskCM[:, 0:128], in_=maskCM[:, 0:128], pattern=[[1, 128]], compare_op=ALU.is_gt, fill=0.0, base=0, channel_multiplier=-1), r=[maskCM], w=[maskCM])
        P.op(P.pool, lambda h: h.affine_select(out=maskCM[:, 128:256], in_=maskCM[:, 128:256], pattern=[[1, 128]], compare_op=ALU.is_ge, fill=0.0, base=0, channel_multiplier=-1), r=[maskCM], w=[maskCM])
        ms(P.pool, maskCM[0:64, 64:128], 0.0, [maskCM], [maskCM])
        ms(P.pool, maskCM[0:64, 192:256], 0.0, [maskCM], [maskCM])
        cp(P.dve, maskCM[:, 256:512], maskCM[:, 0:256], [maskCM], [maskCM])
        ms(P.pool, mAT[:], 1.0, [mAT])
        P.op(P.pool, lambda h: h.affine_select(out=mAT[:], in_=mAT[:], pattern=[[-1, 128]], compare_op=ALU.is_gt, fill=0.0, base=0, channel_multiplier=1), r=[mAT], w=[mAT])
        ms(P.pool, mAT[64:128, 0:64], 0.0, [mAT], [mAT])
        bd4 = P.tile("bd4", [128, 512], F32)
        bdb = P.tile("bdb", [128, 128], BF16)
        ms(P.pool, bd4[:], 0.0, [bd4])
        for q in range(4):
            ms(P.pool, bd4[0:64, q * 128:q * 128 + 64], 1.0, [bd4], [bd4])
            ms(P.pool, bd4[64:128, q * 128 + 64:q * 128 + 128], 1.0, [bd4], [bd4])
        cp(P.dve, bdb[:], bd4[:, 0:128], [bd4], [bdb])
        reset = P.tile("reset", [128, 512], F32)
        ms(P.pool, reset[:], 1.0, [reset])
        ms(P.pool, reset[:].rearrange("p (c t) -> p c t", t=64)[:, :, 0:1], 0.0, [reset], [reset])

        s1 = P.tile("s1", [128, 8], F32); sh1 = P.tile("sh1", [128, 8], F32)
        s2 = P.tile("s2", [128, 8], F32); sh2 = P.tile("sh2", [128, 8], F32)
        g1bc = P.tile("g1bc", [128, D], F32); g2bc = P.tile("g2bc", [128, D], F32)
        with ExitStack() as ph, nc.allow_non_contiguous_dma(reason="tiny per-channel vectors"):
            P.stack = ph
            ccol = P.tile("ccol", [128, 8], F32)
            ld(ccol[:], c[0, :].rearrange("(n p) -> p n", p=128), [ccol])
            scol2 = P.tile("scol2", [128, 8, 2], F32)
            act(scol2[:, :, 0], ccol[:], AF.Silu, [ccol], [scol2])
            act(scol2[:, :, 1], ccol[:], AF.Silu, [ccol], [scol2])
            sbc = P.tile("sbc", [128, 8, 128], F32)
            for kc in range(8):
                cp(P.dve, sbc[:, kc, :], scol2[:, kc, 0:1].to_broadcast([128, 128]), [scol2], [sbc])
            adabT = P.tile("adabT", [128, 48], F32)
            ld(adabT[:], ada_b.rearrange("(n p) -> p n", p=128), [adabT])
            n1g = P.tile("n1g", [128, 8], F32); n2g = P.tile("n2g", [128, 8], F32)
            ld(n1g[:], norm1_g.rearrange("(n p) -> p n", p=128), [n1g])
            ld(n2g[:], norm2_g.rearrange("(n p) -> p n", p=128), [n2g])
            modT = P.tile("modT", [128, 48], F32)
            awb = [P.tile(f"awb{i}", [128, 8, 1024], F32) for i in range(2)]
            adab_bc = P.tile("adab_bc", [128, 1024], F32)
            for vi in range(6):
                aw = awb[vi % 2]
                for kc in range(8):
                    ld(aw[:, kc, :], ada_w[kc * 128:(kc + 1) * 128, vi * 1024:(vi + 1) * 1024], [aw])
                if vi in (2, 5):
                    gbc = g1bc if vi == 2 else g2bc
                    ld(adab_bc[:], ada_b[vi * 1024:(vi + 1) * 1024].partition_broadcast(128), [adab_bc])
                    for hf in range(2):
                        pb = bank()
                        for kc in range(8):
                            mm(pb[:, :], sbc[:, kc, :], aw[:, kc, hf * 512:(hf + 1) * 512], kc == 0, kc == 7, [sbc, aw], [pb])
                        tt(P.dve, gbc[:, hf * 512:(hf + 1) * 512], pb[:, :], adab_bc[:, hf * 512:(hf + 1) * 512], ALU.add, [pb, adab_bc], [gbc])
                else:
                    pb = bank()
                    for oc in range(8):
                        for kc in range(8):
                            mm(pb[:, oc * 2:oc * 2 + 2], aw[:, kc, oc * 128:(oc + 1) * 128], scol2[:, kc, :], kc == 0, kc == 7, [aw, scol2], [pb])
                    tt(P.dve, modT[:, vi * 8:(vi + 1) * 8], pb[:, 0:16].rearrange("p (o t) -> p o t", t=2)[:, :, 0], adabT[:, vi * 8:(vi + 1) * 8], ALU.add, [pb, adabT], [modT])
            stt(P.dve, s1[:], modT[:, 8:16], 1.0, n1g[:], ALU.add, ALU.mult, [modT, n1g], [s1])
            cp(P.dve, sh1[:], modT[:, 0:8], [modT], [sh1])
            stt(P.dve, s2[:], modT[:, 32:40], 1.0, n2g[:], ALU.add, ALU.mult, [modT, n2g], [s2])
            cp(P.dve, sh2[:], modT[:, 24:32], [modT], [sh2])
            P.barrier()
            if dbg == "p0":
                o0 = nc.dram_tensor("o_p0", [128, 32], F32, kind="ExternalOutput").ap()
                o1 = nc.dram_tensor("o_g", [128, 2048], F32, kind="ExternalOutput").ap()
                stq(o0[:, 0:8], s1[:], [s1], is_out=True); stq(o0[:, 8:16], sh1[:], [sh1], is_out=True)
                stq(o0[:, 16:24], s2[:], [s2], is_out=True); stq(o0[:, 24:32], sh2[:], [sh2], is_out=True)
                stq(o1[:, 0:1024], g1bc[:], [g1bc], is_out=True); stq(o1[:, 1024:2048], g2bc[:], [g2bc], is_out=True)
                P.finish()
                return nc, P
        P.stack = gs

        def norm_T(xts, hT, src, sidx, sc, shf, tmp):
            t0 = sidx * ST
            ssq, rstd, xn, junk = tmp
            for ti in range(4):
                xt = xts[ti % 2]
                ld(xt[:], src[t0 + ti * 128:t0 + (ti + 1) * 128, :], [xt])
                act(xn[:, ti, :], xt[:], AF.Square, [xt], [xn, ssq], accum_out=ssq[:, ti:ti + 1])
                ts(P.dve, rstd[:, ti:ti + 1], ssq[:, ti:ti + 1], 1.0 / D, 1e-6, ALU.mult, ALU.add, [ssq], [rstd])
                act(rstd[:, ti:ti + 1], rstd[:, ti:ti + 1], AF.Sqrt, [rstd], [rstd])
                P.op(P.dve, lambda h: h.reciprocal(out=rstd[:, ti:ti + 1], in_=rstd[:, ti:ti + 1]), r=[rstd], w=[rstd])
                act(xn[:, ti, :], xt[:], AF.Identity, [xt, rstd], [xn], scale=rstd[:, ti:ti + 1])
            if dbg == "a1n1":
                o0 = nc.dram_tensor("o_xn", [128, 4, D], BF16, kind="ExternalOutput").ap()
                stq(o0, xn[:], [xn], is_out=True)
                o1 = nc.dram_tensor("o_rstd", [128, 4], F32, kind="ExternalOutput").ap()
                stq(o1, rstd[:], [rstd], is_out=True)
                P.finish()
                return
            for dc in range(8):
                bb = bbank()
                for ti in range(4):
                    tr(bb[:, ti * 128:(ti + 1) * 128], xn[:, ti, dc * 128:(dc + 1) * 128], identb[:], [xn, identb], [bb])
                act(hT[:, dc, :], bb[:, 0:512], AF.Identity, [bb, sc, shf], [hT], scale=sc[:, dc:dc + 1], bias=shf[:, dc:dc + 1])

        with ExitStack() as ph, nc.allow_non_contiguous_dma(reason="tiny per-channel vectors"):
            P.stack = ph
            winr = P.tile("winr", [128, 8, RW], BF16)
            for dc in range(8):
                ldc(winr[:, dc, :], w_in[dc * 128:(dc + 1) * 128, 0:RW], [winr])
            Wlw = P.tile("Wlw", [128, 512], BF16); Wla = P.tile("Wla", [128, 512], BF16)
            gup = P.tile("gup", [128, 512], BF16)
            ms(P.dve, Wlw[:], 0.0, [Wlw]); ms(P.dve, Wla[:], 0.0, [Wla])
            ldc(Wlw[0:64, :], rwkv_w_up, [Wlw]); ldc(Wla[64:128, :], rwkv_a_up, [Wla]); ldc(gup[:], rwkv_g_up, [gup])

            def colvec(name, src, n):
                t = P.tile(name, [128, n], F32)
                ld(t[:], src.rearrange("(n p) -> p n", p=128), [t])
                return t
            w0 = colvec("w0", rwkv_w0, 4); a0 = colvec("a0", rwkv_a0, 4); kkv = colvec("kkv", rwkv_k_k, 4)
            kav = colvec("kav", rwkv_k_a, 4); rkv = colvec("rkv", rwkv_r_k, 4)
            lng = colvec("lng", rwkv_ln_g, 4); lnb = colvec("lnb", rwkv_ln_b, 4)
            mu = colvec("mu", tshift_mu, 14)
            omu = P.tile("omu", [128, 14], F32); omka = P.tile("omka", [128, 4], F32)
            ts(P.dve, omu[:], mu[:], -1.0, 1.0, ALU.mult, ALU.add, [mu], [omu])
            ts(P.dve, omka[:], kav[:], -1.0, 1.0, ALU.mult, ALU.add, [kav], [omka])

            if dbg == "a1w":
                o1 = nc.dram_tensor("o_omu", [128, 14], F32, kind="ExternalOutput").ap()
                stq(o1, omu[:], [omu], is_out=True)
                o2 = nc.dram_tensor("o_wla", [128, 512], BF16, kind="ExternalOutput").ap()
                stq(o2, Wla[:], [Wla], is_out=True)
                o3 = nc.dram_tensor("o_winr", [128, 8, RW], BF16, kind="ExternalOutput").ap()
                stq(o3, winr[:], [winr], is_out=True)
                P.finish()
                return nc, P
            xts = [P.tile(f"xt{i}", [128, D], F32) for i in range(2)]
            hT = P.tile("hT", [128, 8, 512], BF16)
            ssq = P.tile("ssq", [128, 4], F32); rstd = P.tile("rstd", [128, 4], F32)
            xn = P.tile("xn", [128, 4, D], BF16); junk = None
            ntmp = (ssq, rstd, xn, junk)
            sh = [P.tile(f"shq{q}", [128, 512], F32) for q in range(14)]
            pmus = [P.tile("pmu0", [128, 513], F32)] * 2
            carry = P.tile("carry", [128, 14], F32)
            ms(P.pool, carry[:], 0.0, [carry])
            lo_in = P.tile("lo_in", [128, 512], BF16); sxg = P.tile("sxg", [128, 512], BF16)
            gT = [P.tile(f"gT{j}", [128, 512], BF16) for j in range(4)]
            bonT = [P.tile(f"bonT{j}", [128, 512], BF16) for j in range(4)]
            RA = P.tile("RA", [128, 4, 4, 256], BF16)
            BT = [P.tile(f"BT{j}", [128, 512], BF16) for j in range(4)]
            KT = [P.tile(f"KT{j}", [128, 512], BF16) for j in range(4)]
            gC = P.tile("gC", [128, 4, 8], F32)
            TOKB2 = P.tile("TOKB2", [128, 4, 512], BF16)
            TOKK2 = P.tile("TOKK2", [128, 4, 512], BF16)
            TOKV = P.tile("TOKV", [128, 4, 512], BF16)
            sgw = P.tile("sgw", [128, 512], F32); asig = P.tile("asig", [128, 512], F32)
            kksq = P.tile("kksq", [128, 512], BF16); rn = P.tile("rn", [128, 512], F32)
            kkn = P.tile("kkn", [128, 512], F32); ff = P.tile("ff", [128, 512], F32)
            kp = P.tile("kp", [128, 512], F32); bp = P.tile("bp", [128, 512], F32)
            cum = P.tile("cum", [128, 512], F32); cme = ff
            cdf = rn
            e1 = sgw; e2 = P.tile("e2", [128, 512], F32)
            e3 = e1; e4 = e2
            rk = P.tile("rk", [128, 512], BF16)
            tb3 = P.tile("tb3", [128, 3, 512], BF16)
            class _View:
                def __init__(self, ap, b):
                    self.ap = ap; self.b = b

                def __getitem__(self, k):
                    return self.ap[k]
            CM = [P.tile("CM0", [128, 8, 512], BF16), _View(xn[:].rearrange("p a (b c) -> p (a b) c", c=512), xn.b)]
            Xs = [P.tile(f"Xs{i}", [128, 8, 128], BF16) for i in range(2)]
            XTs = [P.tile(f"XTs{i}", [128, 8, 128], BF16) for i in range(2)]
            Ps = [P.tile(f"Ps{i}", [128, 8, 128], BF16) for i in range(2)]
            TT = [P.tile(f"TT{i}", [128, 8, 128], BF16) for i in range(2)]
            Hf = [P.tile(f"Hf{i}", [128, 512], F32) for i in range(2)]
            Hb = [P.tile(f"Hb{i}", [128, 512], BF16) for i in range(2)]
            ms(P.pool, Hf[0][:], 0.0, [Hf[0]]); ms(P.pool, Hb[0][:], 0.0, [Hb[0]])
            X1sb = P.tile("X1sb", [128, 512], BF16); Usb = P.tile("Usb", [128, 512], BF16)
            ms(P.pool, X1sb[:], 0.0, [X1sb]); ms(P.pool, Usb[:], 0.0, [Usb])
            htmp = kp
            Ysb = P.tile("Ysb", [128, 512], F32)
            gmean = P.tile("gmean", [128, 8], F32); gvar = P.tile("gvar", [128, 8], F32)
            yc = e2; ysq = e1
            ynb = P.tile("ynb", [128, 4, 512], BF16)
            yaT = P.tile("yaT", [128, 4, 512], BF16)
            yt1 = kkn
            hcur = [0]
            chunk_g = 0

            for si in range(nst):
                t0 = si * ST
                norm_T(xts, hT, x, si, s1, sh1, ntmp)
                if dbg == "a1n1":
                    return nc, P
                if dbg == "a1n":
                    o0 = nc.dram_tensor("o_hT", [128, 8, 512], BF16, kind="ExternalOutput").ap()
                    stq(o0, hT[:], [hT], is_out=True)
                    o1 = nc.dram_tensor("o_omu", [128, 14], F32, kind="ExternalOutput").ap()
                    stq(o1, omu[:], [omu], is_out=True)
                    P.finish()
                    return nc, P
                for q in range(14):
                    pb = bank()
                    for dc in range(8):
                        mm(pb[:, :], winr[:, dc, q * 128:(q + 1) * 128], hT[:, dc, :], dc == 0, dc == 7, [winr, hT], [pb])
                    pmu = pmus[q % 2]
                    act(sh[q][:], pb[:, :], AF.Identity, [pb, omu], [sh[q]], scale=omu[:, q:q + 1])
                    act(pmu[:, 0:1], carry[:, q:q + 1], AF.Copy, [carry], [pmu])
                    ts(P.dve, pmu[:, 1:513], pb[:, :], mu[:, q:q + 1], None, ALU.mult, None, [pb, mu], [pmu])
                    tt(P.dve, sh[q][:], sh[q][:], pmu[:, 0:512], ALU.add, [sh[q], pmu], [sh[q]])
                    act(carry[:, q:q + 1], pmu[:, 512:513], AF.Copy, [pmu], [carry])
                if dbg == "a1a":
                    o0 = nc.dram_tensor("o_sh", [14, 128, 512], F32, kind="ExternalOutput").ap()
                    for q in range(14):
                        stq(o0[q], sh[q][:], [sh[q]], is_out=True)
                    P.finish()
                    return nc, P
                act(lo_in[0:64, :], sh[12][0:64, :], AF.Tanh, [sh[12]], [lo_in])
                act(lo_in[64:128, :], sh[12][64:128, :], AF.Copy, [sh[12]], [lo_in])
                act(sxg[:], sh[13][:], AF.Sigmoid, [sh[13]], [sxg])
                for j in range(4):
                    r_, k_, v_ = sh[j], sh[4 + j], sh[8 + j]
                    js = slice(j * 128, (j + 1) * 128)
                    pb = bank()
                    mm(pb[:, :], Wlw[:, js], lo_in[:], True, True, [Wlw, lo_in], [pb])
                    act(sgw[:], pb[:, :], AF.Sigmoid, [pb, w0], [sgw], bias=w0[:, j:j + 1])
                    pb = bank()
                    mm(pb[:, :], Wla[:, js], lo_in[:], True, True, [Wla, lo_in], [pb])
                    act(asig[:], pb[:, :], AF.Sigmoid, [pb, a0], [asig], bias=a0[:, j:j + 1])
                    pb = bank()
                    mm(pb[:, :], gup[:, js], sxg[:], True, True, [gup, sxg], [pb])
                    act(gT[j][:], pb[:, :], AF.Copy, [pb], [gT[j]])
                    ck(1, asig)
                    act(kksq[:], k_[:], AF.Square, [k_, kkv], [kksq], scale=kkv[:, j:j + 1])
                    pb = bank()
                    mm(pb[:, :], bdb[:], kksq[:], True, True, [bdb, kksq], [pb])
                    act(rn[:], pb[:, :], AF.Sqrt, [pb], [rn], bias=1e-24)
                    P.op(P.dve, lambda h: h.reciprocal(out=rn[:], in_=rn[:]), r=[rn], w=[rn])
                    stt(P.dve, kkn[:], k_[:], kkv[:, j:j + 1], rn[:], ALU.mult, ALU.mult, [k_, kkv, rn], [kkn])
                    ck(2, kkn)
                    ts(P.dve, ff[:], asig[:], kav[:, j:j + 1], omka[:, j:j + 1], ALU.mult, ALU.add, [asig, kav, omka], [ff])
                    tt(P.dve, kp[:], k_[:], ff[:], ALU.mult, [k_, ff], [kp])
                    tt(P.dve, bp[:], kkn[:], asig[:], ALU.mult, [kkn, asig], [bp])
                    P.op(P.dve, lambda h: h.tensor_tensor_scan(out=cum[:], data0=reset[:], data1=sgw[:], initial=0.0, op0=ALU.mult, op1=ALU.add), r=[reset, sgw], w=[cum])
                    tt(P.dve, cme[:], cum[:], sgw[:], ALU.subtract, [cum, sgw], [cme])
                    ck(3, cum)
                    cum3 = cum[:].rearrange("p (c t) -> p c t", t=64)
                    tt(P.dve, cdf[:].rearrange("p (c t) -> p c t", t=64), cum3[:, :, 63:64].to_broadcast([128, 8, 64]), cum3, ALU.subtract, [cum], [cdf])
                    ck(4, cdf)
                    act(e1[:], cum[:], AF.Exp, [cum], [e1], scale=SDEC)
                    act(e2[:], cme[:], AF.Exp, [cme], [e2], scale=SDEC)
                    act(gC[:, j, :], cum3[:, :, 63], AF.Exp, [cum], [gC], scale=SDEC)
                    tt(P.dve, RA[:, j, :, 128:256], r_[:].rearrange("p (a t) -> p a t", t=128), e1[:].rearrange("p (a t) -> p a t", t=128), ALU.mult, [r_, e1], [RA])
                    stt(P.dve, RA[:, j, :, 0:128], kkn[:].rearrange("p (a t) -> p a t", t=128), -1.0, e2[:].rearrange("p (a t) -> p a t", t=128), ALU.mult, ALU.mult, [kkn, e2], [RA])
                    ck(5, RA)
                    act(e3[:], cum[:], AF.Exp, [cum], [e3], scale=-SDEC)
                    act(e4[:], cdf[:], AF.Exp, [cdf], [e4], scale=SDEC)
                    tt(P.dve, BT[j][:], bp[:], e3[:], ALU.mult, [bp, e3], [BT[j]])
                    tt(P.dve, KT[j][:], kp[:], e3[:], ALU.mult, [kp, e3], [KT[j]])
                    tt(P.dve, tb3[:, 0, :], bp[:], e4[:], ALU.mult, [bp, e4], [tb3])
                    tt(P.dve, tb3[:, 1, :], kp[:], e4[:], ALU.mult, [kp, e4], [tb3])
                    act(tb3[:, 2, :], v_[:], AF.Copy, [v_], [tb3])
                    stt(P.dve, rk[:], r_[:], rkv[:, j:j + 1], kp[:], ALU.mult, ALU.mult, [r_, rkv, kp], [rk])
                    pb = bank()
                    mm(pb[:, :], bdb[:], rk[:], True, True, [bdb, rk], [pb])
                    tt(P.dve, bonT[j][:], pb[:, :], v_[:], ALU.mult, [pb, v_], [bonT[j]])
                    ck(6, bonT[j])
                    for ti in range(4):
                        bb = bbank()
                        for kind in range(3):
                            tr(bb[:, kind * 128:(kind + 1) * 128], tb3[:, kind, ti * 128:(ti + 1) * 128], identb[:], [tb3, identb], [bb])
                        cp(P.dve, TOKB2[:, ti, js], bb[:, 0:128], [bb], [TOKB2])
                        cp(P.dve, TOKK2[:, ti, js], bb[:, 128:256], [bb], [TOKK2])
                        cp(P.dve, TOKV[:, ti, js], bb[:, 256:384], [bb], [TOKV])
                    ck(7, TOKV, TOKV[:, 0, 0:128])

                if dbg == "a1b":
                    o0 = nc.dram_tensor("o_ra", [128, 4, 4, 256], BF16, kind="ExternalOutput").ap()
                    o1 = nc.dram_tensor("o_tokv", [128, 4, 512], BF16, kind="ExternalOutput").ap()
                    o2 = nc.dram_tensor("o_gc", [128, 4, 8], F32, kind="ExternalOutput").ap()
                    stq(o0, RA[:], [RA], is_out=True); stq(o1, TOKV[:], [TOKV], is_out=True); stq(o2, gC[:], [gC], is_out=True)
                    P.finish()
                    return nc, P
                def gen_D(ti):
                    cm = CM[ti % 2]; tts = TT[ti % 2]
                    tsl = slice(ti * 128, (ti + 1) * 128)
                    for hb4 in range(2):
                        pbn = bank()
                        for hh in range(4):
                            h_ = 2 * hh + hb4; j = hh; ps_ = slice(hb4 * 64, hb4 * 64 + 64)
                            mm(pbn[:, hh * 128:(hh + 1) * 128], RA[ps_, j, ti, 0:128], BT[j][ps_, tsl], True, True, [RA, BT[j]], [pbn])
                        tt(P.dve, XTs[0][:, hb4 * 4:(hb4 + 1) * 4, :], pbn[:, :].rearrange("p (a t) -> p a t", t=128), mAT[:, :].unsqueeze(1).to_broadcast([128, 4, 128]), ALU.mult, [pbn, mAT], [XTs[0]])
                    for h_ in range(8):
                        j = h_ // 2; ps_ = slice((h_ % 2) * 64, (h_ % 2) * 64 + 64)
                        pb = bank()
                        mm(pb[:, 0:256], KT[j][ps_, tsl], RA[ps_, j, ti, :], True, True, [KT[j], RA], [pb])
                        mm(pb[:, 256:512], BT[j][ps_, tsl], RA[ps_, j, ti, :], True, True, [BT[j], RA], [pb])
                        tt(P.dve, cm[:, sl(h_), :], pb[:, :], maskCM[:], ALU.mult, [pb, maskCM], [cm])
                        if h_ % 2 == 1:
                            yield
                    ck(8, cm, cm[:, 0, :])
                    cp(P.dve, Xs[0][:], cm[:, :, 256:384], [cm], [Xs[0]])
                    tt(P.dve, Ps[0][:], cm[:, :, 256:384], identf[:, :].unsqueeze(1).to_broadcast([128, 8, 128]), ALU.add, [cm, identf], [Ps[0]])
                    cur = 0
                    for it in range(1, 6):
                        nxt = 1 - cur
                        last = (it == 5)
                        for hb4 in range(2):
                            hs4 = slice(hb4 * 4, hb4 * 4 + 4)
                            pbx = bank() if not last else None
                            pbt = bank()
                            for hh in range(4):
                                h_ = hb4 * 4 + hh
                                cs = slice(hh * 128, (hh + 1) * 128)
                                if not last:
                                    mm(pbx[:, cs], XTs[cur][:, h_, :], Xs[cur][:, h_, :], True, True, [XTs[cur], Xs[cur]], [pbx])
                                mm(pbt[:, cs], Xs[cur][:, h_, :], XTs[cur][:, h_, :], True, True, [XTs[cur], Xs[cur]], [pbt])
                            if not last:
                                act(Xs[nxt][:, hs4, :], pbx[:, :].rearrange("p (a t) -> p a t", t=128), AF.Copy, [pbx], [Xs[nxt]])
                            cp(P.dve, XTs[nxt][:, hs4, :], pbt[:, :].rearrange("p (a t) -> p a t", t=128), [pbt], [XTs[nxt]])
                            pbp = bank()
                            for hh in range(4):
                                h_ = hb4 * 4 + hh
                                cs = slice(hh * 128, (hh + 1) * 128)
                                mm(pbp[:, cs], identb[:], Ps[cur][:, h_, :], True, False, [identb, Ps[cur]], [pbp])
                                mm(pbp[:, cs], XTs[nxt][:, h_, :], Ps[cur][:, h_, :], False, True, [XTs[nxt], Ps[cur]], [pbp])
                            dst = tts if last else Ps[nxt]
                            act(dst[:, hs4, :], pbp[:, :].rearrange("p (a t) -> p a t", t=128), AF.Copy, [pbp], [dst])
                            yield
                        cur = nxt

                def gen_S(ti):
                    cm = CM[ti % 2]; tts = TT[ti % 2]
                    for p in range(2):
                        rows = slice(64 * p, 64 * p + 64)
                        cc = slice(64 * p, 64 * p + 64)
                        hold_f, hold_b = Hf[hcur[0]], Hb[hcur[0]]
                        hnew_f, hnew_b = Hf[1 - hcur[0]], Hb[1 - hcur[0]]
                        ch = ti * 2 + p
                        tt(P.dve, hnew_f[:].rearrange("p (j v) -> p j v", v=128), hold_f[:].rearrange("p (j v) -> p j v", v=128), gC[:, :, ch:ch + 1].to_broadcast([128, 4, 128]), ALU.mult, [hold_f, gC], [hnew_f])
                        ps1 = bank()
                        for h_ in range(8):
                            j = h_ // 2; hb = h_ % 2
                            o = ps1[rows, h_ * 64:(h_ + 1) * 64]
                            mm(o, RA[:, j, ti, 64 * p:64 * p + 64], hold_b[:, j * 128 + hb * 64:j * 128 + hb * 64 + 64], True, False, [RA, hold_b], [ps1])
                            mm(o, cm[:, sl(h_), 64 * p:64 * p + 64], TOKV[:, ti, h_ * 64:(h_ + 1) * 64], False, True, [cm, TOKV], [ps1])
                        act(X1sb[rows, :], ps1[rows, :], AF.Copy, [ps1], [X1sb])
                        yield
                        ps2 = bank()
                        for h_ in range(8):
                            mm(ps2[rows, h_ * 64:(h_ + 1) * 64], tts[:, sl(h_), 64 * p:64 * p + 64], X1sb[:, h_ * 64:(h_ + 1) * 64], True, True, [tts, X1sb], [ps2])
                        cp(P.dve, Usb[rows, :], ps2[rows, :], [ps2], [Usb])
                        yield
                        ps4 = bank()
                        for h_ in range(8):
                            j = h_ // 2; hb = h_ % 2
                            o = ps4[rows, h_ * 64:(h_ + 1) * 64]
                            mm(o, RA[:, j, ti, 128 + 64 * p:128 + 64 * p + 64], hold_b[:, j * 128 + hb * 64:j * 128 + hb * 64 + 64], True, False, [RA, hold_b], [ps4])
                            mm(o, cm[:, sl(h_), 384 + 64 * p:384 + 64 * p + 64], Usb[:, h_ * 64:(h_ + 1) * 64], False, False, [cm, Usb], [ps4])
                            mm(o, cm[:, sl(h_), 128 + 64 * p:128 + 64 * p + 64], TOKV[:, ti, h_ * 64:(h_ + 1) * 64], False, True, [cm, TOKV], [ps4])
                        act(Ysb[rows, :], ps4[rows, :], AF.Copy, [ps4], [Ysb])
                        yield
                        ps3 = bank()
                        for j in range(4):
                            js = slice(j * 128, (j + 1) * 128)
                            mm(ps3[:, js], TOKB2[rows, ti, js], Usb[rows, js], True, False, [TOKB2, Usb], [ps3])
                            mm(ps3[:, js], TOKK2[rows, ti, js], TOKV[rows, ti, js], False, True, [TOKK2, TOKV], [ps3])
                        tt(P.dve, htmp[:], ps3[:, :], bd4[:], ALU.mult, [ps3, bd4], [htmp])
                        tt(P.dve, hnew_f[:], hnew_f[:], htmp[:], ALU.add, [hnew_f, htmp], [hnew_f])
                        act(hnew_b[:], hnew_f[:], AF.Copy, [hnew_f], [hnew_b])
                        hcur[0] = 1 - hcur[0]
                        yield
                    y3 = Ysb[:, :].rearrange("p (h v) -> p h v", v=64)
                    P.op(P.dve, lambda h: h.tensor_reduce(out=gmean[:], in_=y3, axis=AX.X, op=ALU.add), r=[Ysb], w=[gmean])
                    ts(P.dve, gmean[:], gmean[:], 1.0 / 64, None, ALU.mult, None, [gmean], [gmean])
                    tt(P.dve, yc[:].rearrange("p (h v) -> p h v", v=64), y3, gmean[:, :].unsqueeze(2).to_broadcast([128, 8, 64]), ALU.subtract, [Ysb, gmean], [yc])
                    tt(P.dve, ysq[:], yc[:], yc[:], ALU.mult, [yc], [ysq])
                    P.op(P.dve, lambda h: h.tensor_reduce(out=gvar[:], in_=ysq[:].rearrange("p (h v) -> p h v", v=64), axis=AX.X, op=ALU.add), r=[ysq], w=[gvar])
                    ts(P.dve, gvar[:], gvar[:], 1.0 / 64, 64e-5, ALU.mult, ALU.add, [gvar], [gvar])
                    act(gvar[:], gvar[:], AF.Sqrt, [gvar], [gvar])
                    P.op(P.dve, lambda h: h.reciprocal(out=gvar[:], in_=gvar[:]), r=[gvar], w=[gvar])
                    tt(P.dve, ynb[:, ti, :].rearrange("p (h v) -> p h v", v=64), yc[:].rearrange("p (h v) -> p h v", v=64), gvar[:, :].unsqueeze(2).to_broadcast([128, 8, 64]), ALU.mult, [yc, gvar], [ynb])

                def drive(*gens):
                    gens = [g for g in gens if g is not None]
                    while gens:
                        for g in list(gens):
                            try:
                                next(g)
                            except StopIteration:
                                gens.remove(g)
                drive(gen_D(0))
                for ti in range(4):
                    drive(gen_S(ti), gen_D(ti + 1) if ti < 3 else None)
                for j in range(4):
                    bb = bbank()
                    for ti in range(4):
                        tr(bb[:, ti * 128:(ti + 1) * 128], ynb[:, ti, j * 128:(j + 1) * 128], identb[:], [ynb, identb], [bb])
                    act(yt1[:], bb[:, 0:512], AF.Identity, [bb, lng, lnb], [yt1], scale=lng[:, j:j + 1], bias=lnb[:, j:j + 1])
                    tt(P.dve, yt1[:], yt1[:], bonT[j][:], ALU.add, [yt1, bonT[j]], [yt1])
                    tt(P.dve, yaT[:, j, :], yt1[:], gT[j][:], ALU.mult, [yt1, gT[j]], [yaT])
                    stq(yaT_d[j * 128:(j + 1) * 128, t0:t0 + ST], yaT[:, j, :], [yaT], [yaT_d], is_out=(dbg == "yaT_d"), q=P.pool)
            P.barrier()
        P.stack = gs
        if dbg == "yaT_d":
            P.finish()
            return nc, P

        with ExitStack() as ph, nc.allow_non_contiguous_dma(reason="tiny per-channel vectors"):
            P.stack = ph
            C0 = RW
            winu = P.tile("winu", [128, 8, 512], BF16); winv = P.tile("winv", [128, 8, 512], BF16)
            wing = P.tile("wing", [128, 8, 2048], BF16)
            woa = P.tile("woa", [128, 4, D], BF16); wob = P.tile("wob", [128, 4, D], BF16); wo = P.tile("wo", [128, 8, D], BF16)
            for dc in range(8):
                ldc(winu[:, dc, :], w_in[dc * 128:(dc + 1) * 128, C0:C0 + 512], [winu])
                ldc(winv[:, dc, :], w_in[dc * 128:(dc + 1) * 128, C0 + 512:C0 + 1024], [winv])
                ldc(wing[:, dc, 0:1024], w_in[dc * 128:(dc + 1) * 128, C0 + 1024:C0 + 2048], [wing])
                ldc(wing[:, dc, 1024:2048], w_in[dc * 128:(dc + 1) * 128, C0 + 2048:C0 + 3072], [wing])
                ldc(wo[:, dc, :], w_out[dc * 128:(dc + 1) * 128, :], [wo])
            for q in range(4):
                ldc(woa[:, q, :], w_out_a[q * 128:(q + 1) * 128, :], [woa])
                ldc(wob[:, q, :], w_out_b[q * 128:(q + 1) * 128, :], [wob])
            mU = P.tile("mU", [128, 128], F32)
            ms(P.pool, mU[:], 1.0, [mU])
            P.op(P.pool, lambda h: h.affine_select(out=mU[:], in_=mU[:], pattern=[[1, 128]], compare_op=ALU.is_ge, fill=0.0, base=0, channel_multiplier=-1), r=[mU], w=[mU])
            wsf = P.tile("wsf", [128, 8, 128], F32)
            for g_ in range(8):
                ld(wsf[:, g_, :], gmlp_ws[g_], [wsf])
            wsmT = P.tile("wsmT", [128, 8, 128], BF16)
            for g4 in range(2):
                pb = bank()
                for gg in range(4):
                    tr(pb[:, gg * 128:(gg + 1) * 128], wsf[:, g4 * 4 + gg, :], identf[:], [wsf, identf], [pb])
                tt(P.dve, wsmT[:, g4 * 4:(g4 + 1) * 4, :], pb[:, :].rearrange("p (a t) -> p a t", t=128), mU[:, :].unsqueeze(1).to_broadcast([128, 4, 128]), ALU.mult, [pb, mU], [wsmT])
            bsT = P.tile("bsT", [128, 4, 128], F32)
            for g_ in range(8):
                ld(bsT[(g_ % 2) * 64:(g_ % 2) * 64 + 64, g_ // 2, :], gmlp_bs[g_].partition_broadcast(64), [bsT])
            lngbc = P.tile("lngbc", [128, 512], F32); lnbbc = P.tile("lnbbc", [128, 512], F32)
            ld(lngbc[:], gmlp_ln_g.partition_broadcast(128), [lngbc]); ld(lnbbc[:], gmlp_ln_b.partition_broadcast(128), [lnbbc])

            xts = [P.tile(f"bxt{i}", [128, D], F32) for i in range(2)]
            hT = P.tile("bhT", [128, 8, 512], BF16)
            ssq = P.tile("bssq", [128, 4], F32); rstd = P.tile("brstd", [128, 4], F32)
            xn = P.tile("bxn", [128, 4, D], BF16)
            ntmp = (ssq, rstd, xn, None)
            uT = P.tile("uT", [128, 4, 512], BF16)
            vg = P.tile("vg", [128, 512], F32); vc = P.tile("vc", [128, 512], F32)
            vst = P.tile("vst", [128, 4], F32)
            vln = P.tile("vln", [128, 4, 512], BF16)
            ybT = P.tile("ybT", [128, 4, 512], BF16)
            gts = P.tile("gts", [128, 16, 512], BF16)
            yaTs = P.tile("yaTs", [128, 4, 512], BF16)
            mgT = P.tile("mgT", [128, 8, 512], BF16)
            t1 = P.tile("t1", [128, 512], F32); t2_ = P.tile("t2_", [128, 512], F32)
            xo = [P.tile(f"xo{i}", [128, D], F32) for i in range(2)]
            for si in range(nst):
                t0 = si * ST
                norm_T(xts, hT, x, si, s1, sh1, ntmp)
                for q in range(4):
                    ld(yaTs[:, q, :], yaT_d[q * 128:(q + 1) * 128, t0:t0 + ST], [yaTs], r=[yaT_d])
                for q in range(4):
                    pb = bank()
                    for dc in range(8):
                        mm(pb[:, :], winu[:, dc, q * 128:(q + 1) * 128], hT[:, dc, :], dc == 0, dc == 7, [winu, hT], [pb])
                    act(uT[:, q, :], pb[:, :], AF.Gelu, [pb], [uT])
                for ti in range(4):
                    pb = bank()
                    for dc in range(8):
                        mm(pb[:, :], hT[:, dc, ti * 128:(ti + 1) * 128], winv[:, dc, :], dc == 0, dc == 7, [winv, hT], [pb])
                    act(vg[:], pb[:, :], AF.Gelu, [pb], [vg, vst], accum_out=vst[:, 0:1])
                    ts(P.dve, vst[:, 1:2], vst[:, 0:1], 1.0 / 512, None, ALU.mult, None, [vst], [vst])
                    ts(P.dve, vc[:], vg[:], vst[:, 1:2], None, ALU.subtract, None, [vg, vst], [vc])
                    act(vg[:], vc[:], AF.Square, [vc], [vg, vst], accum_out=vst[:, 2:3])
                    ts(P.dve, vst[:, 3:4], vst[:, 2:3], 1.0 / 512, 1e-5, ALU.mult, ALU.add, [vst], [vst])
                    act(vst[:, 3:4], vst[:, 3:4], AF.Sqrt, [vst], [vst])
                    P.op(P.dve, lambda h: h.reciprocal(out=vst[:, 3:4], in_=vst[:, 3:4]), r=[vst], w=[vst])
                    stt(P.dve, vc[:], vc[:], vst[:, 3:4], lngbc[:], ALU.mult, ALU.mult, [vc, vst, lngbc], [vc])
                    tt(P.dve, vln[:, ti, :], vc[:], lnbbc[:], ALU.add, [vc, lnbbc], [vln])
                for q in range(4):
                    pb = bank()
                    for ti in range(4):
                        for gg in range(2):
                            g_ = 2 * q + gg
                            mm(pb[gg * 64:(gg + 1) * 64, ti * 128:(ti + 1) * 128], vln[:, ti, g_ * 64:(g_ + 1) * 64], wsmT[:, g_, :], True, True, [vln, wsmT], [pb])
                    tt(P.dve, t1[:].rearrange("p (a t) -> p a t", t=128), pb[:, :].rearrange("p (a t) -> p a t", t=128), bsT[:, q, :].unsqueeze(1).to_broadcast([128, 4, 128]), ALU.add, [pb, bsT], [t1])
                    tt(P.dve, ybT[:, q, :], t1[:], uT[:, q, :], ALU.mult, [t1, uT], [ybT])
                for q in range(16):
                    pb = bank()
                    for dc in range(8):
                        mm(pb[:, :], wing[:, dc, q * 128:(q + 1) * 128], hT[:, dc, :], dc == 0, dc == 7, [wing, hT], [pb])
                    act(gts[:, q, :], pb[:, :], AF.Sigmoid, [pb], [gts])
                for m in range(8):
                    pa = bank()
                    for q in range(4):
                        mm(pa[:, :], woa[:, q, m * 128:(m + 1) * 128], yaTs[:, q, :], q == 0, q == 3, [woa, yaTs], [pa])
                    pb = bank()
                    for q in range(4):
                        mm(pb[:, :], wob[:, q, m * 128:(m + 1) * 128], ybT[:, q, :], q == 0, q == 3, [wob, ybT], [pb])
                    tt(P.dve, t1[:], pa[:, :], gts[:, m, :], ALU.mult, [pa, gts], [t1])
                    tt(P.dve, t2_[:], pb[:, :], gts[:, 8 + m, :], ALU.mult, [pb, gts], [t2_])
                    tt(P.dve, mgT[:, m, :], t1[:], t2_[:], ALU.add, [t1, t2_], [mgT])
                for ti in range(4):
                    xt = xts[ti % 2]; xo_ = xo[ti % 2]
                    ld(xt[:], x[t0 + ti * 128:t0 + (ti + 1) * 128, :], [xt])
                    for hf in range(2):
                        pb = bank()
                        for m in range(8):
                            mm(pb[:, :], mgT[:, m, ti * 128:(ti + 1) * 128], wo[:, m, hf * 512:(hf + 1) * 512], m == 0, m == 7, [mgT, wo], [pb])
                        tt(P.dve, t1[:], pb[:, :], g1bc[:, hf * 512:(hf + 1) * 512], ALU.mult, [pb, g1bc], [t1])
                        tt(P.dve, xo_[:, hf * 512:(hf + 1) * 512], t1[:], xt[:, hf * 512:(hf + 1) * 512], ALU.add, [t1, xt], [xo_])
                    stq(x1_d[t0 + ti * 128:t0 + (ti + 1) * 128, :], xo_[:], [xo_], [x1_d], is_out=(dbg == "x1_d"), q=P.pool)
            P.barrier()
        P.stack = gs
        if dbg == "x1_d":
            P.finish()
            return nc, P

        NT = nst * 4
        dest8 = P.tile("dest8", [128, 64, 8], I32)
        w8 = P.tile("w8", [128, 64, 8], F32)
        idxw = P.tile("idxw", [128, NBLK], I32)
        w8b = [Buf(f"w8b{i}") for i in range(64)]; d8b = [Buf(f"d8b{i}") for i in range(64)]
        with ExitStack() as ph, nc.allow_non_contiguous_dma(reason="tiny per-channel vectors"):
            P.stack = ph
            zt = P.tile("zt", [128, 8192], BF16)
            ms(P.pool, zt[:], 0.0, [zt])
            nzb = NSLOT // 1024
            for i in range(nzb):
                stq(xg_d[i * 1024:(i + 1) * 1024, :].rearrange("(p r) d -> p (r d)", p=128), zt[:], [zt], [xg_d])
            rw = P.tile("rw", [128, 8, NE], BF16)
            sw1 = P.tile("sw1", [128, 8, 256], BF16); sw3 = P.tile("sw3", [128, 8, 256], BF16); sw2 = P.tile("sw2", [128, 2, D], BF16)
            for dc in range(8):
                ldc(rw[:, dc, :], router_w[dc * 128:(dc + 1) * 128, :], [rw])
                ldc(sw1[:, dc, :], shared_w1[dc * 128:(dc + 1) * 128, :], [sw1])
                ldc(sw3[:, dc, :], shared_w3[dc * 128:(dc + 1) * 128, :], [sw3])
            for fc in range(2):
                ldc(sw2[:, fc, :], shared_w2[fc * 128:(fc + 1) * 128, :], [sw2])
            rbias = P.tile("rbias", [128, NE], F32)
            ld(rbias[:], router_bias.partition_broadcast(128), [rbias])
            eoff = P.tile("eoff", [128, NE], F32)
            ustr = P.tile("ustr", [128, 128], BF16)
            onesb = P.tile("onesb", [128, 128], BF16)
            cp(P.dve, ustr[:], mU[:], [mU], [ustr]) if False else None
            uf = P.tile("uf", [128, 128], F32)
            ms(P.pool, uf[:], 1.0, [uf])
            P.op(P.pool, lambda h: h.affine_select(out=uf[:], in_=uf[:], pattern=[[1, 128]], compare_op=ALU.is_gt, fill=0.0, base=0, channel_multiplier=-1), r=[uf], w=[uf])
            cp(P.dve, ustr[:], uf[:], [uf], [ustr])
            ms(P.dve, onesb[:], 1.0, [onesb])
            basec = P.tile("basec", [128, NE], F32)
            ms(P.dve, basec[:], 0.0, [basec])

            xts = [P.tile(f"cxt{i}", [128, D], F32) for i in range(2)]
            hT = P.tile("chT", [128, 8, 512], BF16)
            ssq = P.tile("cssq", [128, 4], F32); rstd = P.tile("crstd", [128, 4], F32)
            xn = P.tile("cxn", [128, 4, D], BF16)
            ntmp = (ssq, rstd, xn, None)
            h2row = [P.tile(f"h2row{i}", [128, D], BF16) for i in range(2)]
            class _S:
                pass

            def mkset(n):
                S = _S()
                S.sc_ = P.tile(f"sc_{n}", [128, NE], F32); S.sel = P.tile(f"sel{n}", [128, NE], F32)
                S.m88 = P.tile(f"m88{n}", [128, 8, 8], F32); S.gs_ = P.tile(f"gs_{n}", [128, 8], F32)
                S.g8 = P.tile(f"g8{n}", [128, 8], F32); S.gmask = P.tile(f"gmask{n}", [128, 8], F32)
                S.selm = P.tile(f"selm{n}", [128, NE], F32); S.smask = P.tile(f"smask{n}", [128, NE], F32)
                S.smb = P.tile(f"smb{n}", [128, NE], BF16)
                S.wd = P.tile(f"wd{n}", [128, NE], F32); S.wsum = P.tile(f"wsum{n}", [128, 2], F32)
                S.key = P.tile(f"key{n}", [128, NE], F32); S.k8 = P.tile(f"k8{n}", [128, 8], F32)
                S.kz = P.tile(f"kz{n}", [128, 8], F32); S.jk = P.tile(f"jk{n}", [128, NE], F32)
                S.t2_ = P.tile(f"ct2{n}", [128, 512], F32)
                return S
            SS = [mkset(0), mkset(1)]
            hsT = P.tile("hsT", [128, 2, 512], BF16)
            t1 = P.tile("ct1", [128, 512], F32)
            xo = [P.tile(f"cxo{i}", [128, D], F32) for i in range(2)]

            def route(tsl, S):
                pb = bank()
                for dc in range(8):
                    mm(pb[:, 0:NE], hT[:, dc, tsl], rw[:, dc, :], dc == 0, dc == 7, [hT, rw], [pb])
                act(S.sc_[:], pb[:, 0:NE], AF.Sigmoid, [pb], [S.sc_])
                yield
                tt(P.dve, S.sel[:], S.sc_[:], rbias[:], ALU.add, [S.sc_, rbias], [S.sel])
                yield
                for g_ in range(8):
                    P.op(P.dve, lambda h: h.max(out=S.m88[:, g_, :], in_=S.sel[:, g_ * 32:(g_ + 1) * 32]), r=[S.sel], w=[S.m88])
                yield
                tt(P.dve, S.gs_[:], S.m88[:, :, 0], S.m88[:, :, 1], ALU.add, [S.m88], [S.gs_])
                yield
                P.op(P.dve, lambda h: h.max(out=S.g8[:], in_=S.gs_[:]), r=[S.gs_], w=[S.g8])
                yield
                ts(P.dve, S.gmask[:], S.gs_[:], S.g8[:, 3:4], None, ALU.is_ge, None, [S.gs_, S.g8], [S.gmask])
                yield
                stt(P.dve, S.selm[:].rearrange("p (g e) -> p g e", e=32), S.sel[:].rearrange("p (g e) -> p g e", e=32), 2.0, S.gmask[:, :].unsqueeze(2).to_broadcast([128, 8, 32]), ALU.add, ALU.mult, [S.sel, S.gmask], [S.selm])
                yield
                P.op(P.dve, lambda h: h.max(out=S.g8[:], in_=S.selm[:]), r=[S.selm], w=[S.g8])
                yield
                ts(P.dve, S.smask[:], S.selm[:], S.g8[:, 7:8], None, ALU.is_ge, None, [S.selm, S.g8], [S.smask])
                yield
                cp(P.dve, S.smb[:], S.smask[:], [S.smask], [S.smb])
                yield

            def drive(*gens):
                gens = [g for g in gens if g is not None]
                while gens:
                    for g in list(gens):
                        try:
                            next(g)
                        except StopIteration:
                            gens.remove(g)

            def gen_p1(ti, S):
                yield from route(slice(ti * 128, (ti + 1) * 128), S)
                pp = bank()
                mm(pp[:, 0:NE], onesb[:], S.smb[:], True, True, [onesb, S.smb], [pp])
                tt(P.dve, basec[:], basec[:], pp[:, 0:NE], ALU.add, [pp, basec], [basec])
                yield

            for si in range(nst):
                norm_T(xts, hT, x1_d.t, si, s2, sh2, ntmp)
                drive(gen_p1(0, SS[0]), gen_p1(1, SS[1]))
                drive(gen_p1(2, SS[0]), gen_p1(3, SS[1]))
            nblk = P.tile("nblk", [128, NE], F32); pends = P.tile("pends", [128, NE], F32)
            ones256 = P.tile("ones256", [128, NE], F32)
            ms(P.dve, nblk[:], 0.0, [nblk]); ms(P.dve, ones256[:], 1.0, [ones256])
            for m_ in range(T // BLK):
                stt(P.dve, nblk[:], basec[:], float(BLK * m_), nblk[:], ALU.is_gt, ALU.add, [basec, nblk], [nblk])
            ts(P.dve, nblk[:], nblk[:], float(BLK), None, ALU.mult, None, [nblk], [nblk])
            P.op(P.dve, lambda h: h.tensor_tensor_scan(out=pends[:], data0=ones256[:], data1=nblk[:], initial=0.0, op0=ALU.mult, op1=ALU.add), r=[ones256, nblk], w=[pends])
            tt(P.dve, eoff[:], pends[:], nblk[:], ALU.subtract, [pends, nblk], [eoff])
            ts(P.dve, eoff[:], eoff[:], 1.0, None, ALU.add, None, [eoff], [eoff])
            pcol = P.tile("pcol", [128, 2], F32)
            for c_ in range(2):
                pb = bank()
                tr(pb[:, 0:128], pends[:, c_ * 128:(c_ + 1) * 128], identf[:], [pends, identf], [pb])
                cp(P.dve, pcol[:, c_:c_ + 1], pb[:, 0:1], [pb], [pcol])
            iotab = P.tile("iotab", [128, NBLK], F32)
            P.op(P.pool, lambda h: h.iota(iotab[:], pattern=[[BLK, NBLK]], base=0, channel_multiplier=0, allow_small_or_imprecise_dtypes=True), w=[iotab])
            cmpb = P.tile("cmpb", [128, 2, NBLK], BF16)
            for c_ in range(2):
                ts(P.dve, cmpb[:, c_, :], iotab[:], pcol[:, c_:c_ + 1], None, ALU.is_ge, None, [iotab, pcol], [cmpb])
            pb = bank()
            for c_ in range(2):
                mm(pb[:, :], onesb[:], cmpb[:, c_, :], c_ == 0, c_ == 1, [onesb, cmpb], [pb])
            pidx = P.tile("pidx", [128, NBLK], F32)
            P.op(P.pool, lambda h: h.iota(pidx[:], pattern=[[0, NBLK]], base=0, channel_multiplier=1, allow_small_or_imprecise_dtypes=True), w=[pidx])
            ts(P.dve, iotab[:], pb[:, :], 255.0, 128.0, ALU.min, ALU.mult, [pb], [iotab])
            tt(P.dve, idxw[:], iotab[:], pidx[:], ALU.add, [iotab, pidx], [idxw])
            ms(P.dve, basec[:], 0.0, [basec])

            def gen_p2(si, ti, S):
                t0 = si * ST
                tg = si * 4 + ti
                tsl = slice(ti * 128, (ti + 1) * 128)
                hr = h2row[ti % 2]
                bb = bbank()
                for dc in range(8):
                    tr(bb[:, dc * 128:(dc + 1) * 128], hT[:, dc, tsl], identb[:], [hT, identb], [bb])
                cp(P.dve, hr[:], bb[:, :], [bb], [hr])
                yield
                yield from route(tsl, S)
                stt(P.dve, S.wd[:], S.smask[:], 1.0, S.sc_[:], ALU.mult, ALU.mult, [S.smask, S.sc_], [S.wd, S.wsum], accum_out=S.wsum[:, 0:1])
                yield
                P.op(P.dve, lambda h: h.reciprocal(out=S.wsum[:, 1:2], in_=S.wsum[:, 0:1]), r=[S.wsum], w=[S.wsum])
                yield
                ts(P.dve, S.wd[:], S.wd[:], S.wsum[:, 1:2], 2.5, ALU.mult, ALU.mult, [S.wd, S.wsum], [S.wd])
                pp = bank()
                mm(pp[:, 0:NE], ustr[:], S.smb[:], True, True, [ustr, S.smb], [pp])
                mm(pp[:, NE:2 * NE], onesb[:], S.smb[:], True, True, [onesb, S.smb], [pp])
                tt(P.dve, S.key[:], pp[:, 0:NE], basec[:], ALU.add, [pp, basec], [S.key])
                tt(P.dve, basec[:], basec[:], pp[:, NE:2 * NE], ALU.add, [pp, basec], [basec])
                yield
                tt(P.dve, S.key[:], S.key[:], eoff[:], ALU.add, [S.key, eoff], [S.key])
                yield
                tt(P.dve, S.key[:], S.key[:], S.smask[:], ALU.mult, [S.key, S.smask], [S.key])
                yield
                P.op(P.dve, lambda h: h.max(out=S.k8[:], in_=S.key[:]), r=[S.key], w=[S.k8])
                yield
                for k in range(8):
                    stt(P.dve, S.jk[:], S.key[:], S.k8[:, k:k + 1], S.wd[:], ALU.is_equal, ALU.mult, [S.key, S.k8, S.wd], [S.jk, w8b[tg]], accum_out=w8[:, tg, k:k + 1])
                    yield
                ts(P.dve, S.kz[:], S.k8[:], 0.0, float(NSLOT), ALU.is_equal, ALU.mult, [S.k8], [S.kz])
                yield
                stt(P.dve, dest8[:, tg, :], S.k8[:], -1.0, S.kz[:], ALU.add, ALU.add, [S.k8, S.kz], [d8b[tg]])
                yield
                for k in range(8):
                    l = LP[lpi[0] % len(LP)]; lpi[0] += 1
                    P.dma(P.pool, l, lambda h: h.indirect_dma_start(out=xg_d.t, out_offset=bass.IndirectOffsetOnAxis(ap=dest8[:, tg, k:k + 1], axis=0), in_=hr[:], in_offset=None), r=[hr, d8b[tg]], w=[xg_d])
                yield
                xt = xts[ti % 2]; xo_ = xo[ti % 2]
                ld(xt[:], x1_d[t0 + ti * 128:t0 + (ti + 1) * 128, :], [xt])
                for hf in range(2):
                    pb = bank()
                    for fc in range(2):
                        mm(pb[:, :], hsT[:, fc, tsl], sw2[:, fc, hf * 512:(hf + 1) * 512], fc == 0, fc == 1, [hsT, sw2], [pb])
                    tt(P.dve, S.t2_[:], pb[:, :], g2bc[:, hf * 512:(hf + 1) * 512], ALU.mult, [pb, g2bc], [S.t2_])
                    yield
                    tt(P.dve, xo_[:, hf * 512:(hf + 1) * 512], S.t2_[:], xt[:, hf * 512:(hf + 1) * 512], ALU.add, [S.t2_, xt], [xo_])
                    yield
                stq(x1_d[t0 + ti * 128:t0 + (ti + 1) * 128, :], xo_[:], [xo_], [x1_d], q=P.act)
                yield

            for si in range(nst):
                norm_T(xts, hT, x1_d.t, si, s2, sh2, ntmp)
                for fc in range(2):
                    p1 = bank()
                    for dc in range(8):
                        mm(p1[:, :], sw1[:, dc, fc * 128:(fc + 1) * 128], hT[:, dc, :], dc == 0, dc == 7, [sw1, hT], [p1])
                    p3 = bank()
                    for dc in range(8):
                        mm(p3[:, :], sw3[:, dc, fc * 128:(fc + 1) * 128], hT[:, dc, :], dc == 0, dc == 7, [sw3, hT], [p3])
                    act(t1[:], p1[:, :], AF.Silu, [p1], [t1])
                    tt(P.dve, hsT[:, fc, :], t1[:], p3[:, :], ALU.mult, [t1, p3], [hsT])
                drive(gen_p2(si, 0, SS[0]), gen_p2(si, 1, SS[1]))
                drive(gen_p2(si, 2, SS[0]), gen_p2(si, 3, SS[1]))
            P.barrier()
            if dbg == "pB":
                P.finish()
                raise StopBuild()
        P.stack = gs

        with ExitStack() as ph:
            P.stack = ph
            w1v = exp_w1.rearrange("e (p c) f -> (e p) (c f)", c=8)
            w3v = exp_w3.rearrange("e (p c) f -> (e p) (c f)", c=8)
            w2v = exp_w2.rearrange("e (p c) d -> (e p) (c d)", c=2)
            xgt = [P.tile(f"xgt{i}", [128, 2, D], BF16) for i in range(3)]
            xgT = [P.tile(f"xgT{i}", [128, 8, BLK], BF16) for i in range(2)]
            w1b = [P.tile(f"w1b{i}", [128, 2048], BF16) for i in range(3)]
            w3b = [P.tile(f"w3b{i}", [128, 2048], BF16) for i in range(3)]
            w2b = [P.tile(f"w2b{i}", [128, 2048], BF16) for i in range(3)]
            hid = [P.tile(f"hid{i}", [128, 2, BLK], BF16) for i in range(2)]
            st1 = [P.tile(f"st1{i}", [128, BLK], F32) for i in range(2)]
            yrow = [P.tile(f"yrow{i}", [128, 2, D], BF16) for i in range(2)]
            LY = P.lanes(2, "ly")

            def wgather(dst, src, i_):
                l = LP[lpi[0] % len(LP)]; lpi[0] += 1
                P.dma(P.pool, l, lambda h: h.indirect_dma_start(out=dst[:], out_offset=None, in_=src, in_offset=bass.IndirectOffsetOnAxis(ap=idxw[:, i_:i_ + 1], axis=0)), r=[idxw], w=[dst])

            def c_loads(i_):
                i3 = i_ % 3
                ld(xgt[i3][:], xg_d[i_ * BLK:(i_ + 1) * BLK, :].rearrange("(b p) d -> p b d", p=128), [xgt[i3]], r=[xg_d])
                wgather(w1b[i3], w1v, i_); wgather(w3b[i3], w3v, i_); wgather(w2b[i3], w2v, i_)

            def c_T(i_):
                i3 = i_ % 3; i2 = i_ % 2
                xv = xgt[i3][:].rearrange("p b (q c) -> p b c q", c=8)
                for dc in range(8):
                    bb = bbank()
                    for b_ in range(2):
                        tr(bb[:, b_ * 128:(b_ + 1) * 128], xv[:, b_, dc, :], identb[:], [xgt[i3], identb], [bb])
                    cp(P.dve, xgT[i2][:, dc, :], bb[:, 0:BLK], [bb], [xgT[i2]])

            def c_H(i_):
                i3 = i_ % 3; i2 = i_ % 2
                w1r = w1b[i3][:].rearrange("p (c m two) -> p c two m", c=8, two=2)
                w3r = w3b[i3][:].rearrange("p (c m two) -> p c two m", c=8, two=2)
                for fc in range(2):
                    p1 = bank()
                    for dc in range(8):
                        mm(p1[:, 0:BLK], w1r[:, dc, fc, :], xgT[i2][:, dc, :], dc == 0, dc == 7, [w1b[i3], xgT[i2]], [p1])
                    p3 = bank()
                    for dc in range(8):
                        mm(p3[:, 0:BLK], w3r[:, dc, fc, :], xgT[i2][:, dc, :], dc == 0, dc == 7, [w3b[i3], xgT[i2]], [p3])
                    act(st1[fc][:], p1[:, 0:BLK], AF.Silu, [p1], [st1[fc]])
                    tt(P.dve, hid[i2][:, fc, :], st1[fc][:], p3[:, 0:BLK], ALU.mult, [st1[fc], p3], [hid[i2]])

            def c_Y(i_):
                i3 = i_ % 3; i2 = i_ % 2
                w2r = w2b[i3][:].rearrange("p (c d) -> p c d", c=2)
                for b_ in range(2):
                    for hf in range(2):
                        pb = bank()
                        for fc in range(2):
                            mm(pb[:, :], hid[i2][:, fc, b_ * 128:(b_ + 1) * 128], w2r[:, fc, hf * 512:(hf + 1) * 512], fc == 0, fc == 1, [hid[i2], w2b[i3]], [pb])
                        act(yrow[i2][:, b_, hf * 512:(hf + 1) * 512], pb[:, :], AF.Copy, [pb], [yrow[i2]])
                P.dma(P.act, LY[i2], lambda h: h.dma_start(out=yg_d[i_ * BLK:(i_ + 1) * BLK, :].rearrange("(b p) d -> p b d", p=128), in_=yrow[i2][:]), r=[yrow[i2]], w=[yg_d])

            nb_ = nblk_run
            c_loads(0)
            if nb_ > 1:
                c_loads(1)
            c_T(0)
            for i_ in range(nb_):
                if i_ + 1 < nb_:
                    c_T(i_ + 1)
                c_H(i_)
                if i_ >= 1:
                    c_Y(i_ - 1)
                if i_ + 2 < nb_:
                    c_loads(i_ + 2)
            c_Y(nb_ - 1)
            P.barrier()
            if dbg == "pC":
                P.finish()
                raise StopBuild()
        P.stack = gs

        with ExitStack() as ph:
            P.stack = ph
            nfbc = P.tile("nfbc", [128, D], F32)
            ld(nfbc[:], normf_g.partition_broadcast(128), [nfbc])
            xts = [P.tile(f"dxt{i}", [128, D], F32) for i in range(2)]
            gat = [P.tile(f"gat{i}", [128, D], BF16) for i in range(4)]
            acc = P.tile("acc", [128, D], F32)
            ot = [P.tile(f"ot{i}", [128, D], F32) for i in range(2)]
            fs = P.tile("fs", [128, 2], F32)
            jk2 = P.tile("jk2", [128, D], BF16)
            for tg in range(NT):
                xt = xts[tg % 2]; o_ = ot[tg % 2]
                ld(xt[:], x1_d[tg * 128:(tg + 1) * 128, :], [xt])
                for k in range(8):
                    gt = gat[k % 4]
                    l = LP[lpi[0] % len(LP)]; lpi[0] += 1
                    P.dma(P.pool, l, lambda h: h.indirect_dma_start(out=gt[:], out_offset=None, in_=yg_d.t, in_offset=bass.IndirectOffsetOnAxis(ap=dest8[:, tg, k:k + 1], axis=0)), r=[yg_d, d8b[tg]], w=[gt])
                    if k == 0:
                        ts(P.dve, acc[:], gt[:], w8[:, tg, 0:1], None, ALU.mult, None, [gt, w8b[tg]], [acc])
                    else:
                        stt(P.dve, acc[:], gt[:], w8[:, tg, k:k + 1], acc[:], ALU.mult, ALU.add, [gt, w8b[tg], acc], [acc])
                tt(P.dve, acc[:], acc[:], g2bc[:], ALU.mult, [acc, g2bc], [acc])
                tt(P.dve, acc[:], acc[:], xt[:], ALU.add, [acc, xt], [acc])
                act(jk2[:], acc[:], AF.Square, [acc], [jk2, fs], accum_out=fs[:, 0:1])
                ts(P.dve, fs[:, 1:2], fs[:, 0:1], 1.0 / D, 1e-6, ALU.mult, ALU.add, [fs], [fs])
                act(fs[:, 1:2], fs[:, 1:2], AF.Sqrt, [fs], [fs])
                P.op(P.dve, lambda h: h.reciprocal(out=fs[:, 1:2], in_=fs[:, 1:2]), r=[fs], w=[fs])
                stt(P.dve, o_[:], acc[:], fs[:, 1:2], nfbc[:], ALU.mult, ALU.mult, [acc, fs, nfbc], [o_])
                stq(out[tg * 128:(tg + 1) * 128, :], o_[:], [o_], is_out=True, q=P.act)
            P.finish()
        P.stack = gs
        return nc, P


_NAMES = ["ada_w", "ada_b", "norm1_g", "norm2_g", "w_in", "tshift_mu", "rwkv_w0", "rwkv_w_up", "rwkv_a0", "rwkv_a_up",
          "rwkv_g_up", "rwkv_k_k", "rwkv_k_a", "rwkv_r_k", "rwkv_ln_g", "rwkv_ln_b", "gmlp_ln_g", "gmlp_ln_b", "gmlp_ws",
          "gmlp_bs", "w_out_a", "w_out_b", "w_out", "router_w", "router_bias", "exp_w1", "exp_w3", "exp_w2",
          "shared_w1", "shared_w3", "shared_w2"]


def kernel(**inputs):
    nc, _ = build()
    shared = {}
    for k in _NAMES:
        a = np.asarray(inputs[k], dtype=np.float32)[0]
        if k == "rwkv_r_k":
            a = a.reshape(512)
        shared[k] = np.ascontiguousarray(a)
    shared["normf_g"] = np.ascontiguousarray(np.asarray(inputs["normf_g"], dtype=np.float32))
    x = np.asarray(inputs["x"], dtype=np.float32)
    c = np.asarray(inputs["c"], dtype=np.float32)
    in_maps = []
    for b in range(8):
        m = dict(shared)
        m["x"] = np.ascontiguousarray(x[b])
        m["c"] = np.ascontiguousarray(c[b:b + 1])
        in_maps.append(m)
    res = run_bass_kernel_spmd(nc, in_maps, core_ids=list(range(8)))
    return np.stack([np.asarray(r["out"], dtype=np.float32) for r in res.results], axis=0)
```

```python
import numpy as np
import concourse.bass as bass
import concourse.mybir as mybir
from concourse.bass_utils import run_bass_kernel_spmd

F32 = mybir.dt.float32
BF16 = mybir.dt.bfloat16
U32 = mybir.dt.uint32
I32 = mybir.dt.int32
AF = mybir.ActivationFunctionType
ALU = mybir.AluOpType
AX = mybir.AxisListType


class Buf:
    __slots__ = ("name", "w", "rs")

    def __init__(self, name):
        self.name = name
        self.w = None
        self.rs = []


class Tl:
    def __init__(self, t, name):
        self.t = t
        self.b = Buf(name)

    def __getitem__(self, k):
        return self.t[k]


class Eng:
    def __init__(self, P, name, h, sem):
        self.P = P
        self.name = name
        self.h = h
        self.sem = sem
        self.cnt = 0
        self.waited = {}


class Lane:
    def __init__(self, sem):
        self.sem = sem
        self.val = 0


class Prog:
    def __init__(self, nc, stack):
        self.nc = nc
        self.stack = stack
        mk = lambda n: stack.enter_context(nc.semaphore(n))
        self.pe = Eng(self, "pe", nc.tensor, mk("s_pe"))
        self.act = Eng(self, "act", nc.scalar, mk("s_act"))
        self.dve = Eng(self, "dve", nc.vector, mk("s_dve"))
        self.pool = Eng(self, "pool", nc.gpsimd, mk("s_pool"))
        self.sp = Eng(self, "sp", nc.sync, mk("s_sp"))
        self.engs = [self.pe, self.act, self.dve, self.pool, self.sp]
        self.nlanes = 0
        self.all_lanes = []
        self.out_toks = []
        self.nins = 0

    def tile(self, name, shape, dt):
        return Tl(self.sb(name, shape, dt), name)

    def ptile(self, name, shape, dt=F32):
        return Tl(self.ps(name, shape, dt), name)

    def barrier(self):
        toks = [(e.sem, e.cnt) for e in self.engs if e.cnt] + [(l.sem, l.val) for l in self.all_lanes if l.val]
        for e in self.engs:
            for t in toks:
                self._wait(e, t)

    def sb(self, name, shape, dt):
        return self.stack.enter_context(self.nc.sbuf_tensor(name, shape, dt))

    def ps(self, name, shape, dt=F32):
        return self.stack.enter_context(self.nc.psum_tensor(name, shape, dt))

    def lanes(self, n, name="ln"):
        out = []
        for i in range(n):
            out.append(Lane(self.stack.enter_context(self.nc.semaphore(f"{name}{self.nlanes}"))))
            self.nlanes += 1
        self.all_lanes += out
        return out

    def _wait(self, e, tok):
        if tok is None:
            return
        sem, val = tok
        k = id(sem)
        if e.waited.get(k, 0) >= val:
            return
        if sem is e.sem and e is self.pe:
            return
        e.h.wait_ge(sem, val)
        e.waited[k] = val
        self.nins += 1

    def _deps(self, e, r, w):
        r = [getattr(b, "b", b) for b in r]
        w = [getattr(b, "b", b) for b in w]
        for b in r:
            self._wait(e, b.w)
        for b in w:
            self._wait(e, b.w)
            for t in b.rs:
                self._wait(e, t)

    def _commit(self, tok, r, w):
        r = [getattr(b, "b", b) for b in r]
        w = [getattr(b, "b", b) for b in w]
        for b in r:
            b.rs.append(tok)
            if len(b.rs) > 24:
                d = {}
                for s, v in b.rs:
                    if id(s) not in d or d[id(s)][1] < v:
                        d[id(s)] = (s, v)
                b.rs = list(d.values())
        for b in w:
            b.w = tok
            b.rs = []

    def op(self, e, fn, r=(), w=()):
        self._deps(e, r, w)
        ins = fn(e.h)
        e.cnt += 1
        ins.then_inc(e.sem, 1)
        tok = (e.sem, e.cnt)
        self._commit(tok, r, w)
        self.nins += 1
        return tok

    def dma(self, e, lane, fn, r=(), w=(), is_out=False):
        self._wait(e, (lane.sem, lane.val) if lane.val else None)
        self._deps(e, r, w)
        ins = fn(e.h)
        lane.val += 16
        ins.then_inc(lane.sem, 16)
        tok = (lane.sem, lane.val)
        self._commit(tok, r, w)
        if is_out:
            self.out_toks.append(tok)
        self.nins += 1
        return tok

    def finish(self):
        for tok in self.out_toks:
            self._wait(self.sp, tok)


from contextlib import ExitStack

T = 8192
D = 1024
ST = 512
NST = T // ST
SDEC = -0.6065306597126334
RW = 1792
CAP = 512
BLK = 256
NBLK = 512
NE = 256
NSLOT = NE * CAP
ROW = 1024 + 64


def sl(h_):
    return (h_ % 2) * 4 + h_ // 2


class StopBuild(Exception):
    pass


def build(dbg=None, nst=NST, nblk_run=NBLK):
    nc = bass.Bass("TRN2", target_bir_lowering=False)
    holder = {}
    try:
        return _build(nc, dbg, nst, holder, nblk_run)
    except StopBuild:
        return nc, holder["P"]


def _build(nc, dbg, nst, holder, nblk_run):

    def din(name, shape, dt=F32):
        return nc.dram_tensor(name, shape, dt, kind="ExternalInput").ap()

    x = din("x", [T, D]); c = din("c", [1, D])
    ada_w = din("ada_w", [D, 6 * D]); ada_b = din("ada_b", [6 * D])
    norm1_g = din("norm1_g", [D]); norm2_g = din("norm2_g", [D])
    w_in = din("w_in", [D, 4864]); tshift_mu = din("tshift_mu", [RW])
    rwkv_w0 = din("rwkv_w0", [512]); rwkv_w_up = din("rwkv_w_up", [64, 512])
    rwkv_a0 = din("rwkv_a0", [512]); rwkv_a_up = din("rwkv_a_up", [64, 512])
    rwkv_g_up = din("rwkv_g_up", [128, 512]); rwkv_k_k = din("rwkv_k_k", [512])
    rwkv_k_a = din("rwkv_k_a", [512]); rwkv_r_k = din("rwkv_r_k", [512])
    rwkv_ln_g = din("rwkv_ln_g", [512]); rwkv_ln_b = din("rwkv_ln_b", [512])
    gmlp_ln_g = din("gmlp_ln_g", [512]); gmlp_ln_b = din("gmlp_ln_b", [512])
    gmlp_ws = din("gmlp_ws", [8, 128, 128]); gmlp_bs = din("gmlp_bs", [8, 128])
    w_out_a = din("w_out_a", [512, D]); w_out_b = din("w_out_b", [512, D]); w_out = din("w_out", [D, D])
    router_w = din("router_w", [D, NE]); router_bias = din("router_bias", [NE])
    if True:
        exp_w1 = din("exp_w1", [NE, D, 256]); exp_w3 = din("exp_w3", [NE, D, 256]); exp_w2 = din("exp_w2", [NE, 256, D])
    shared_w1 = din("shared_w1", [D, 256]); shared_w3 = din("shared_w3", [D, 256]); shared_w2 = din("shared_w2", [256, D])
    normf_g = din("normf_g", [D])
    out = nc.dram_tensor("out", [T, D], F32, kind="ExternalOutput").ap()

    def dscr(name, shape, dt):
        k = "ExternalOutput" if dbg == name else "Internal"
        return Tl(nc.dram_tensor(name, shape, dt, kind=k).ap(), name)

    yaT_d = dscr("yaT_d", [512, T], BF16)
    x1_d = dscr("x1_d", [T, D], F32)
    xg_d = dscr("xg_d", [NSLOT, D], BF16)
    yg_d = dscr("yg_d", [NSLOT, D], BF16)

    with ExitStack() as gs:
        P = Prog(nc, gs)
        holder["P"] = P

        def ck(n, tl, ap=None):
            if dbg == f"ck{n}":
                o_ = nc.dram_tensor("o_ck", list((ap if ap is not None else tl[:]).shape), (ap if ap is not None else tl[:]).dtype, kind="ExternalOutput").ap()
                stq(o_, ap if ap is not None else tl[:], [tl], is_out=True)
                P.finish()
                raise StopBuild()
        LD = P.lanes(6, "ld")
        LP = P.lanes(4, "lp")
        LS = P.lanes(4, "lst")
        ldi = [0]; lpi = [0]; lsi = [0]

        def ld(out_ap, in_ap, w, r=()):
            l = LD[ldi[0] % len(LD)]; ldi[0] += 1
            return P.dma(P.sp, l, lambda h: h.dma_start(out=out_ap, in_=in_ap), r=r, w=w)

        def ldc(out_ap, in_ap, w, r=()):
            l = LP[lpi[0] % len(LP)]; lpi[0] += 1
            return P.dma(P.pool, l, lambda h: h.dma_start(out=out_ap, in_=in_ap), r=r, w=w)

        LSQ = {"sp": LS, "pool": P.lanes(3, "lsp"), "act": P.lanes(3, "lsa")}

        def stq(out_ap, in_ap, r, w=(), is_out=False, q=None):
            q = q or P.sp
            ll = LSQ[q.name]
            l = ll[lsi[0] % len(ll)]; lsi[0] += 1
            return P.dma(q, l, lambda h: h.dma_start(out=out_ap, in_=in_ap), r=r, w=w, is_out=is_out)

        pbanks = [P.ptile(f"pb{i}", [128, 512], F32) for i in range(6)]
        bbanks = [P.ptile(f"bb{i}", [128, 1024], BF16) for i in range(2)]
        pbi = [0]; bbi = [0]

        def bank():
            b = pbanks[pbi[0] % 6]; pbi[0] += 1
            return b

        def bbank():
            b = bbanks[bbi[0] % 2]; bbi[0] += 1
            return b

        def mm(o, lhsT, rhs, start, stop, r, w):
            return P.op(P.pe, lambda h: h.matmul(o, lhsT=lhsT, rhs=rhs, start=start, stop=stop), r=r, w=w)

        def tr(o, in_, ident, r, w):
            return P.op(P.pe, lambda h: h.transpose(out=o, in_=in_, identity=ident), r=r, w=w)

        def act(o, in_, func, r, w, **kw):
            return P.op(P.act, lambda h: h.activation(out=o, in_=in_, func=func, **kw), r=r, w=w)

        def tt(e, o, a, b, op, r, w):
            return P.op(e, lambda h: h.tensor_tensor(out=o, in0=a, in1=b, op=op), r=r, w=w)

        def ts(e, o, a, s1, s2, op0, op1, r, w):
            if s2 is None:
                return P.op(e, lambda h: h.tensor_scalar(out=o, in0=a, scalar1=s1, scalar2=None, op0=op0), r=r, w=w)
            return P.op(e, lambda h: h.tensor_scalar(out=o, in0=a, scalar1=s1, scalar2=s2, op0=op0, op1=op1), r=r, w=w)

        def stt(e, o, a, s, b, op0, op1, r, w, **kw):
            return P.op(e, lambda h: h.scalar_tensor_tensor(out=o, in0=a, scalar=s, in1=b, op0=op0, op1=op1, **kw), r=r, w=w)

        def cp(e, o, a, r, w):
            return P.op(e, lambda h: h.tensor_copy(out=o, in_=a), r=r, w=w)

        def ms(e, o, v, w, r=()):
            return P.op(e, lambda h: h.memset(o, v), r=r, w=w)

        identf = P.tile("identf", [128, 128], F32)
        identb = P.tile("identb", [128, 128], BF16)
        ms(P.pool, identf[:], 0.0, [identf])
        P.op(P.pool, lambda h: h.affine_select(out=identf[:], in_=identf[:], pattern=[[-1, 128]], compare_op=ALU.not_equal, fill=1.0, base=0, channel_multiplier=1), r=[identf], w=[identf])
        cp(P.dve, identb[:], identf[:], [identf], [identb])
        maskCM = P.tile("maskCM", [128, 512], F32)
        mAT = P.tile("mAT", [128, 128], F32)
        ms(P.pool, maskCM[:], 1.0, [maskCM])
        P.op(P.pool, lambda h: h.affine_select(out=maskCM[:, 0:128], in_=maskCM[:, 0:128], pattern=[[1, 128]], compare_op=ALU.is_gt, fill=0.0, base=0, channel_multiplier=-1), r=[maskCM], w=[maskCM])
        P.op(P.pool, lambda h: h.affine_select(out=maskCM[:, 128:256], in_=maskCM[:, 128:256], pattern=[[1, 128]], compare_op=ALU.is_ge, fill=0.0, base=0, channel_multiplier=-1), r=[maskCM], w=[maskCM])
        ms(P.pool, maskCM[0:64, 64:128], 0.0, [maskCM], [maskCM])
        ms(P.pool, maskCM[0:64, 192:256], 0.0, [maskCM], [maskCM])
        cp(P.dve, maskCM[:, 256:512], maskCM[:, 0:256], [maskCM], [maskCM])
        ms(P.pool, mAT[:], 1.0, [mAT])
        P.op(P.pool, lambda h: h.affine_select(out=mAT[:], in_=mAT[:], pattern=[[-1, 128]], compare_op=ALU.is_gt, fill=0.0, base=0, channel_multiplier=1), r=[mAT], w=[mAT])
        ms(P.pool, mAT[64:128, 0:64], 0.0, [mAT], [mAT])
        bd4 = P.tile("bd4", [128, 512], F32)
        bdb = P.tile("bdb", [128, 128], BF16)
        ms(P.pool, bd4[:], 0.0, [bd4])
        for q in range(4):
            ms(P.pool, bd4[0:64, q * 128:q * 128 + 64], 1.0, [bd4], [bd4])
            ms(P.pool, bd4[64:128, q * 128 + 64:q * 128 + 128], 1.0, [bd4], [bd4])
        cp(P.dve, bdb[:], bd4[:, 0:128], [bd4], [bdb])
        reset = P.tile("reset", [128, 512], F32)
        ms(P.pool, reset[:], 1.0, [reset])
        ms(P.pool, reset[:].rearrange("p (c t) -> p c t", t=64)[:, :, 0:1], 0.0, [reset], [reset])

        s1 = P.tile("s1", [128, 8], F32); sh1 = P.tile("sh1", [128, 8], F32)
        s2 = P.tile("s2", [128, 8], F32); sh2 = P.tile("sh2", [128, 8], F32)
        g1bc = P.tile("g1bc", [128, D], F32); g2bc = P.tile("g2bc", [128, D], F32)
        with ExitStack() as ph, nc.allow_non_contiguous_dma(reason="tiny per-channel vectors"):
            P.stack = ph
            ccol = P.tile("ccol", [128, 8], F32)
            ld(ccol[:], c[0, :].rearrange("(n p) -> p n", p=128), [ccol])
            scol2 = P.tile("scol2", [128, 8, 2], F32)
            act(scol2[:, :, 0], ccol[:], AF.Silu, [ccol], [scol2])
            act(scol2[:, :, 1], ccol[:], AF.Silu, [ccol], [scol2])
            sbc = P.tile("sbc", [128, 8, 128], F32)
            for kc in range(8):
                cp(P.dve, sbc[:, kc, :], scol2[:, kc, 0:1].to_broadcast([128, 128]), [scol2], [sbc])
            adabT = P.tile("adabT", [128, 48], F32)
            ld(adabT[:], ada_b.rearrange("(n p) -> p n", p=128), [adabT])
            n1g = P.tile("n1g", [128, 8], F32); n2g = P.tile("n2g", [128, 8], F32)
            ld(n1g[:], norm1_g.rearrange("(n p) -> p n", p=128), [n1g])
            ld(n2g[:], norm2_g.rearrange("(n p) -> p n", p=128), [n2g])
            modT = P.tile("modT", [128, 48], F32)
            awb = [P.tile(f"awb{i}", [128, 8, 1024], F32) for i in range(2)]
            adab_bc = P.tile("adab_bc", [128, 1024], F32)
            for vi in range(6):
                aw = awb[vi % 2]
                for kc in range(8):
                    ld(aw[:, kc, :], ada_w[kc * 128:(kc + 1) * 128, vi * 1024:(vi + 1) * 1024], [aw])
                if vi in (2, 5):
                    gbc = g1bc if vi == 2 else g2bc
                    ld(adab_bc[:], ada_b[vi * 1024:(vi + 1) * 1024].partition_broadcast(128), [adab_bc])
                    for hf in range(2):
                        pb = bank()
                        for kc in range(8):
                            mm(pb[:, :], sbc[:, kc, :], aw[:, kc, hf * 512:(hf + 1) * 512], kc == 0, kc == 7, [sbc, aw], [pb])
                        tt(P.dve, gbc[:, hf * 512:(hf + 1) * 512], pb[:, :], adab_bc[:, hf * 512:(hf + 1) * 512], ALU.add, [pb, adab_bc], [gbc])
                else:
                    pb = bank()
                    for oc in range(8):
                        for kc in range(8):
                            mm(pb[:, oc * 2:oc * 2 + 2], aw[:, kc, oc * 128:(oc + 1) * 128], scol2[:, kc, :], kc == 0, kc == 7, [aw, scol2], [pb])
                    tt(P.dve, modT[:, vi * 8:(vi + 1) * 8], pb[:, 0:16].rearrange("p (o t) -> p o t", t=2)[:, :, 0], adabT[:, vi * 8:(vi + 1) * 8], ALU.add, [pb, adabT], [modT])
            stt(P.dve, s1[:], modT[:, 8:16], 1.0, n1g[:], ALU.add, ALU.mult, [modT, n1g], [s1])
            cp(P.dve, sh1[:], modT[:, 0:8], [modT], [sh1])
            stt(P.dve, s2[:], modT[:, 32:40], 1.0, n2g[:], ALU.add, ALU.mult, [modT, n2g], [s2])
            cp(P.dve, sh2[:], modT[:, 24:32], [modT], [sh2])
            P.barrier()
            if dbg == "p0":
                o0 = nc.dram_tensor("o_p0", [128, 32], F32, kind="ExternalOutput").ap()
                o1 = nc.dram_tensor("o_g", [128, 2048], F32, kind="ExternalOutput").ap()
                stq(o0[:, 0:8], s1[:], [s1], is_out=True); stq(o0[:, 8:16], sh1[:], [sh1], is_out=True)
                stq(o0[:, 16:24], s2[:], [s2], is_out=True); stq(o0[:, 24:32], sh2[:], [sh2], is_out=True)
                stq(o1[:, 0:1024], g1bc[:], [g1bc], is_out=True); stq(o1[:, 1024:2048], g2bc[:], [g2bc], is_out=True)
                P.finish()
                return nc, P
        P.stack = gs

        def norm_T(xts, hT, src, sidx, sc, shf, tmp):
            t0 = sidx * ST
            ssq, rstd, xn, junk = tmp
            for ti in range(4):
                xt = xts[ti % 2]
                ld(xt[:], src[t0 + ti * 128:t0 + (ti + 1) * 128, :], [xt])
                act(xn[:, ti, :], xt[:], AF.Square, [xt], [xn, ssq], accum_out=ssq[:, ti:ti + 1])
                ts(P.dve, rstd[:, ti:ti + 1], ssq[:, ti:ti + 1], 1.0 / D, 1e-6, ALU.mult, ALU.add, [ssq], [rstd])
                act(rstd[:, ti:ti + 1], rstd[:, ti:ti + 1], AF.Sqrt, [rstd], [rstd])
                P.op(P.dve, lambda h: h.reciprocal(out=rstd[:, ti:ti + 1], in_=rstd[:, ti:ti + 1]), r=[rstd], w=[rstd])
                act(xn[:, ti, :], xt[:], AF.Identity, [xt, rstd], [xn], scale=rstd[:, ti:ti + 1])
            if dbg == "a1n1":
                o0 = nc.dram_tensor("o_xn", [128, 4, D], BF16, kind="ExternalOutput").ap()
                stq(o0, xn[:], [xn], is_out=True)
                o1 = nc.dram_tensor("o_rstd", [128, 4], F32, kind="ExternalOutput").ap()
                stq(o1, rstd[:], [rstd], is_out=True)
                P.finish()
                return
            for dc in range(8):
                bb = bbank()
                for ti in range(4):
                    tr(bb[:, ti * 128:(ti + 1) * 128], xn[:, ti, dc * 128:(dc + 1) * 128], identb[:], [xn, identb], [bb])
                act(hT[:, dc, :], bb[:, 0:512], AF.Identity, [bb, sc, shf], [hT], scale=sc[:, dc:dc + 1], bias=shf[:, dc:dc + 1])

        with ExitStack() as ph, nc.allow_non_contiguous_dma(reason="tiny per-channel vectors"):
            P.stack = ph
            winr = P.tile("winr", [128, 8, RW], BF16)
            for dc in range(8):
                ldc(winr[:, dc, :], w_in[dc * 128:(dc + 1) * 128, 0:RW], [winr])
            Wlw = P.tile("Wlw", [128, 512], BF16); Wla = P.tile("Wla", [128, 512], BF16)
            gup = P.tile("gup", [128, 512], BF16)
            ms(P.dve, Wlw[:], 0.0, [Wlw]); ms(P.dve, Wla[:], 0.0, [Wla])
            ldc(Wlw[0:64, :], rwkv_w_up, [Wlw]); ldc(Wla[64:128, :], rwkv_a_up, [Wla]); ldc(gup[:], rwkv_g_up, [gup])

            def colvec(name, src, n):
                t = P.tile(name, [128, n], F32)
                ld(t[:], src.rearrange("(n p) -> p n", p=128), [t])
                return t
            w0 = colvec("w0", rwkv_w0, 4); a0 = colvec("a0", rwkv_a0, 4); kkv = colvec("kkv", rwkv_k_k, 4)
            kav = colvec("kav", rwkv_k_a, 4); rkv = colvec("rkv", rwkv_r_k, 4)
            lng = colvec("lng", rwkv_ln_g, 4); lnb = colvec("lnb", rwkv_ln_b, 4)
            mu = colvec("mu", tshift_mu, 14)
            omu = P.tile("omu", [128, 14], F32); omka = P.tile("omka", [128, 4], F32)
            ts(P.dve, omu[:], mu[:], -1.0, 1.0, ALU.mult, ALU.add, [mu], [omu])
            ts(P.dve, omka[:], kav[:], -1.0, 1.0, ALU.mult, ALU.add, [kav], [omka])

            if dbg == "a1w":
                o1 = nc.dram_tensor("o_omu", [128, 14], F32, kind="ExternalOutput").ap()
                stq(o1, omu[:], [omu], is_out=True)
                o2 = nc.dram_tensor("o_wla", [128, 512], BF16, kind="ExternalOutput").ap()
                stq(o2, Wla[:], [Wla], is_out=True)
                o3 = nc.dram_tensor("o_winr", [128, 8, RW], BF16, kind="ExternalOutput").ap()
                stq(o3, winr[:], [winr], is_out=True)
                P.finish()
                return nc, P
            xts = [P.tile(f"xt{i}", [128, D], F32) for i in range(2)]
            hT = P.tile("hT", [128, 8, 512], BF16)
            ssq = P.tile("ssq", [128, 4], F32); rstd = P.tile("rstd", [128, 4], F32)
            xn = P.tile("xn", [128, 4, D], BF16); junk = None
            ntmp = (ssq, rstd, xn, junk)
            sh = [P.tile(f"shq{q}", [128, 512], F32) for q in range(14)]
            pmus = [P.tile("pmu0", [128, 513], F32)] * 2
            carry = P.tile("carry", [128, 14], F32)
            ms(P.pool, carry[:], 0.0, [carry])
            lo_in = P.tile("lo_in", [128, 512], BF16); sxg = P.tile("sxg", [128, 512], BF16)
            gT = [P.tile(f"gT{j}", [128, 512], BF16) for j in range(4)]
            bonT = [P.tile(f"bonT{j}", [128, 512], BF16) for j in range(4)]
            RA = P.tile("RA", [128, 4, 4, 256], BF16)
            BT = [P.tile(f"BT{j}", [128, 512], BF16) for j in range(4)]
            KT = [P.tile(f"KT{j}", [128, 512], BF16) for j in range(4)]
            gC = P.tile("gC", [128, 4, 8], F32)
            TOKB2 = P.tile("TOKB2", [128, 4, 512], BF16)
            TOKK2 = P.tile("TOKK2", [128, 4, 512], BF16)
            TOKV = P.tile("TOKV", [128, 4, 512], BF16)
            sgw = P.tile("sgw", [128, 512], F32); asig = P.tile("asig", [128, 512], F32)
            kksq = P.tile("kksq", [128, 512], BF16); rn = P.tile("rn", [128, 512], F32)
            kkn = P.tile("kkn", [128, 512], F32); ff = P.tile("ff", [128, 512], F32)
            kp = P.tile("kp", [128, 512], F32); bp = P.tile("bp", [128, 512], F32)
            cum = P.tile("cum", [128, 512], F32); cme = ff
            cdf = rn
            e1 = sgw; e2 = P.tile("e2", [128, 512], F32)
            e3 = e1; e4 = e2
            rk = P.tile("rk", [128, 512], BF16)
            tb3 = P.tile("tb3", [128, 3, 512], BF16)
            class _View:
                def __init__(self, ap, b):
                    self.ap = ap; self.b = b

                def __getitem__(self, k):
                    return self.ap[k]
            CM = [P.tile("CM0", [128, 8, 512], BF16), _View(xn[:].rearrange("p a (b c) -> p (a b) c", c=512), xn.b)]
            Xs = [P.tile(f"Xs{i}", [128, 8, 128], BF16) for i in range(2)]
            XTs = [P.tile(f"XTs{i}", [128, 8, 128], BF16) for i in range(2)]
            Ps = [P.tile(f"Ps{i}", [128, 8, 128], BF16) for i in range(2)]
            TT = [P.tile(f"TT{i}", [128, 8, 128], BF16) for i in range(2)]
            Hf = [P.tile(f"Hf{i}", [128, 512], F32) for i in range(2)]
            Hb = [P.tile(f"Hb{i}", [128, 512], BF16) for i in range(2)]
            ms(P.pool, Hf[0][:], 0.0, [Hf[0]]); ms(P.pool, Hb[0][:], 0.0, [Hb[0]])
            X1sb = P.tile("X1sb", [128, 512], BF16); Usb = P.tile("Usb", [128, 512], BF16)
            ms(P.pool, X1sb[:], 0.0, [X1sb]); ms(P.pool, Usb[:], 0.0, [Usb])
            htmp = kp
            Ysb = P.tile("Ysb", [128, 512], F32)
            gmean = P.tile("gmean", [128, 8], F32); gvar = P.tile("gvar", [128, 8], F32)
            yc = e2; ysq = e1
            ynb = P.tile("ynb", [128, 4, 512], BF16)
            yaT = P.tile("yaT", [128, 4, 512], BF16)
            yt1 = kkn
            hcur = [0]
            chunk_g = 0

            for si in range(nst):
                t0 = si * ST
                norm_T(xts, hT, x, si, s1, sh1, ntmp)
                if dbg == "a1n1":
                    return nc, P
                if dbg == "a1n":
                    o0 = nc.dram_tensor("o_hT", [128, 8, 512], BF16, kind="ExternalOutput").ap()
                    stq(o0, hT[:], [hT], is_out=True)
                    o1 = nc.dram_tensor("o_omu", [128, 14], F32, kind="ExternalOutput").ap()
                    stq(o1, omu[:], [omu], is_out=True)
                    P.finish()
                    return nc, P
                for q in range(14):
                    pb = bank()
                    for dc in range(8):
                        mm(pb[:, :], winr[:, dc, q * 128:(q + 1) * 128], hT[:, dc, :], dc == 0, dc == 7, [winr, hT], [pb])
                    pmu = pmus[q % 2]
                    act(sh[q][:], pb[:, :], AF.Identity, [pb, omu], [sh[q]], scale=omu[:, q:q + 1])
                    act(pmu[:, 0:1], carry[:, q:q + 1], AF.Copy, [carry], [pmu])
                    ts(P.dve, pmu[:, 1:513], pb[:, :], mu[:, q:q + 1], None, ALU.mult, None, [pb, mu], [pmu])
                    tt(P.dve, sh[q][:], sh[q][:], pmu[:, 0:512], ALU.add, [sh[q], pmu], [sh[q]])
                    act(carry[:, q:q + 1], pmu[:, 512:513], AF.Copy, [pmu], [carry])
                if dbg == "a1a":
                    o0 = nc.dram_tensor("o_sh", [14, 128, 512], F32, kind="ExternalOutput").ap()
                    for q in range(14):
                        stq(o0[q], sh[q][:], [sh[q]], is_out=True)
                    P.finish()
                    return nc, P
                act(lo_in[0:64, :], sh[12][0:64, :], AF.Tanh, [sh[12]], [lo_in])
                act(lo_in[64:128, :], sh[12][64:128, :], AF.Copy, [sh[12]], [lo_in])
                act(sxg[:], sh[13][:], AF.Sigmoid, [sh[13]], [sxg])
                for j in range(4):
                    r_, k_, v_ = sh[j], sh[4 + j], sh[8 + j]
                    js = slice(j * 128, (j + 1) * 128)
                    pb = bank()
                    mm(pb[:, :], Wlw[:, js], lo_in[:], True, True, [Wlw, lo_in], [pb])
                    act(sgw[:], pb[:, :], AF.Sigmoid, [pb, w0], [sgw], bias=w0[:, j:j + 1])
                    pb = bank()
                    mm(pb[:, :], Wla[:, js], lo_in[:], True, True, [Wla, lo_in], [pb])
                    act(asig[:], pb[:, :], AF.Sigmoid, [pb, a0], [asig], bias=a0[:, j:j + 1])
                    pb = bank()
                    mm(pb[:, :], gup[:, js], sxg[:], True, True, [gup, sxg], [pb])
                    act(gT[j][:], pb[:, :], AF.Copy, [pb], [gT[j]])
                    ck(1, asig)
                    act(kksq[:], k_[:], AF.Square, [k_, kkv], [kksq], scale=kkv[:, j:j + 1])
                    pb = bank()
                    mm(pb[:, :], bdb[:], kksq[:], True, True, [bdb, kksq], [pb])
                    act(rn[:], pb[:, :], AF.Sqrt, [pb], [rn], bias=1e-24)
                    P.op(P.dve, lambda h: h.reciprocal(out=rn[:], in_=rn[:]), r=[rn], w=[rn])
                    stt(P.dve, kkn[:], k_[:], kkv[:, j:j + 1], rn[:], ALU.mult, ALU.mult, [k_, kkv, rn], [kkn])
                    ck(2, kkn)
                    ts(P.dve, ff[:], asig[:], kav[:, j:j + 1], omka[:, j:j + 1], ALU.mult, ALU.add, [asig, kav, omka], [ff])
                    tt(P.dve, kp[:], k_[:], ff[:], ALU.mult, [k_, ff], [kp])
                    tt(P.dve, bp[:], kkn[:], asig[:], ALU.mult, [kkn, asig], [bp])
                    P.op(P.dve, lambda h: h.tensor_tensor_scan(out=cum[:], data0=reset[:], data1=sgw[:], initial=0.0, op0=ALU.mult, op1=ALU.add), r=[reset, sgw], w=[cum])
                    tt(P.dve, cme[:], cum[:], sgw[:], ALU.subtract, [cum, sgw], [cme])
                    ck(3, cum)
                    cum3 = cum[:].rearrange("p (c t) -> p c t", t=64)
                    tt(P.dve, cdf[:].rearrange("p (c t) -> p c t", t=64), cum3[:, :, 63:64].to_broadcast([128, 8, 64]), cum3, ALU.subtract, [cum], [cdf])
                    ck(4, cdf)
                    act(e1[:], cum[:], AF.Exp, [cum], [e1], scale=SDEC)
                    act(e2[:], cme[:], AF.Exp, [cme], [e2], scale=SDEC)
                    act(gC[:, j, :], cum3[:, :, 63], AF.Exp, [cum], [gC], scale=SDEC)
                    tt(P.dve, RA[:, j, :, 128:256], r_[:].rearrange("p (a t) -> p a t", t=128), e1[:].rearrange("p (a t) -> p a t", t=128), ALU.mult, [r_, e1], [RA])
                    stt(P.dve, RA[:, j, :, 0:128], kkn[:].rearrange("p (a t) -> p a t", t=128), -1.0, e2[:].rearrange("p (a t) -> p a t", t=128), ALU.mult, ALU.mult, [kkn, e2], [RA])
                    ck(5, RA)
                    act(e3[:], cum[:], AF.Exp, [cum], [e3], scale=-SDEC)
                    act(e4[:], cdf[:], AF.Exp, [cdf], [e4], scale=SDEC)
                    tt(P.dve, BT[j][:], bp[:], e3[:], ALU.mult, [bp, e3], [BT[j]])
                    tt(P.dve, KT[j][:], kp[:], e3[:], ALU.mult, [kp, e3], [KT[j]])
                    tt(P.dve, tb3[:, 0, :], bp[:], e4[:], ALU.mult, [bp, e4], [tb3])
                    tt(P.dve, tb3[:, 1, :], kp[:], e4[:], ALU.mult, [kp, e4], [tb3])
                    act(tb3[:, 2, :], v_[:], AF.Copy, [v_], [tb3])
                    stt(P.dve, rk[:], r_[:], rkv[:, j:j + 1], kp[:], ALU.mult, ALU.mult, [r_, rkv, kp], [rk])
                    pb = bank()
                    mm(pb[:, :], bdb[:], rk[:], True, True, [bdb, rk], [pb])
                    tt(P.dve, bonT[j][:], pb[:, :], v_[:], ALU.mult, [pb, v_], [bonT[j]])
                    ck(6, bonT[j])
                    for ti in range(4):
                        bb = bbank()
                        for kind in range(3):
                            tr(bb[:, kind * 128:(kind + 1) * 128], tb3[:, kind, ti * 128:(ti + 1) * 128], identb[:], [tb3, identb], [bb])
                        cp(P.dve, TOKB2[:, ti, js], bb[:, 0:128], [bb], [TOKB2])
                        cp(P.dve, TOKK2[:, ti, js], bb[:, 128:256], [bb], [TOKK2])
                        cp(P.dve, TOKV[:, ti, js], bb[:, 256:384], [bb], [TOKV])
                    ck(7, TOKV, TOKV[:, 0, 0:128])

                if dbg == "a1b":
                    o0 = nc.dram_tensor("o_ra", [128, 4, 4, 256], BF16, kind="ExternalOutput").ap()
                    o1 = nc.dram_tensor("o_tokv", [128, 4, 512], BF16, kind="ExternalOutput").ap()
                    o2 = nc.dram_tensor("o_gc", [128, 4, 8], F32, kind="ExternalOutput").ap()
                    stq(o0, RA[:], [RA], is_out=True); stq(o1, TOKV[:], [TOKV], is_out=True); stq(o2, gC[:], [gC], is_out=True)
                    P.finish()
                    return nc, P
                def gen_D(ti):
                    cm = CM[ti % 2]; tts = TT[ti % 2]
                    tsl = slice(ti * 128, (ti + 1) * 128)
                    for hb4 in range(2):
                        pbn = bank()
                        for hh in range(4):
                            h_ = 2 * hh + hb4; j = hh; ps_ = slice(hb4 * 64, hb4 * 64 + 64)
                            mm(pbn[:, hh * 128:(hh + 1) * 128], RA[ps_, j, ti, 0:128], BT[j][ps_, tsl], True, True, [RA, BT[j]], [pbn])
                        tt(P.dve, XTs[0][:, hb4 * 4:(hb4 + 1) * 4, :], pbn[:, :].rearrange("p (a t) -> p a t", t=128), mAT[:, :].unsqueeze(1).to_broadcast([128, 4, 128]), ALU.mult, [pbn, mAT], [XTs[0]])
                    for h_ in range(8):
                        j = h_ // 2; ps_ = slice((h_ % 2) * 64, (h_ % 2) * 64 + 64)
                        pb = bank()
                        mm(pb[:, 0:256], KT[j][ps_, tsl], RA[ps_, j, ti, :], True, True, [KT[j], RA], [pb])
                        mm(pb[:, 256:512], BT[j][ps_, tsl], RA[ps_, j, ti, :], True, True, [BT[j], RA], [pb])
                        tt(P.dve, cm[:, sl(h_), :], pb[:, :], maskCM[:], ALU.mult, [pb, maskCM], [cm])
                        if h_ % 2 == 1:
                            yield
                    ck(8, cm, cm[:, 0, :])
                    cp(P.dve, Xs[0][:], cm[:, :, 256:384], [cm], [Xs[0]])
                    tt(P.dve, Ps[0][:], cm[:, :, 256:384], identf[:, :].unsqueeze(1).to_broadcast([128, 8, 128]), ALU.add, [cm, identf], [Ps[0]])
                    cur = 0
                    for it in range(1, 6):
                        nxt = 1 - cur
                        last = (it == 5)
                        for hb4 in range(2):
                            hs4 = slice(hb4 * 4, hb4 * 4 + 4)
                            pbx = bank() if not last else None
                            pbt = bank()
                            for hh in range(4):
                                h_ = hb4 * 4 + hh
                                cs = slice(hh * 128, (hh + 1) * 128)
                                if not last:
                                    mm(pbx[:, cs], XTs[cur][:, h_, :], Xs[cur][:, h_, :], True, True, [XTs[cur], Xs[cur]], [pbx])
                                mm(pbt[:, cs], Xs[cur][:, h_, :], XTs[cur][:, h_, :], True, True, [XTs[cur], Xs[cur]], [pbt])
                            if not last:
                                act(Xs[nxt][:, hs4, :], pbx[:, :].rearrange("p (a t) -> p a t", t=128), AF.Copy, [pbx], [Xs[nxt]])
                            cp(P.dve, XTs[nxt][:, hs4, :], pbt[:, :].rearrange("p (a t) -> p a t", t=128), [pbt], [XTs[nxt]])
                            pbp = bank()
                            for hh in range(4):
                                h_ = hb4 * 4 + hh
                                cs = slice(hh * 128, (hh + 1) * 128)
                                mm(pbp[:, cs], identb[:], Ps[cur][:, h_, :], True, False, [identb, Ps[cur]], [pbp])
                                mm(pbp[:, cs], XTs[nxt][:, h_, :], Ps[cur][:, h_, :], False, True, [XTs[nxt], Ps[cur]], [pbp])
                            dst = tts if last else Ps[nxt]
                            act(dst[:, hs4, :], pbp[:, :].rearrange("p (a t) -> p a t", t=128), AF.Copy, [pbp], [dst])
                            yield
                        cur = nxt

                def gen_S(ti):
                    cm = CM[ti % 2]; tts = TT[ti % 2]
                    for p in range(2):
                        rows = slice(64 * p, 64 * p + 64)
                        cc = slice(64 * p, 64 * p + 64)
                        hold_f, hold_b = Hf[hcur[0]], Hb[hcur[0]]
                        hnew_f, hnew_b = Hf[1 - hcur[0]], Hb[1 - hcur[0]]
                        ch = ti * 2 + p
                        tt(P.dve, hnew_f[:].rearrange("p (j v) -> p j v", v=128), hold_f[:].rearrange("p (j v) -> p j v", v=128), gC[:, :, ch:ch + 1].to_broadcast([128, 4, 128]), ALU.mult, [hold_f, gC], [hnew_f])
                        ps1 = bank()
                        for h_ in range(8):
                            j = h_ // 2; hb = h_ % 2
                            o = ps1[rows, h_ * 64:(h_ + 1) * 64]
                            mm(o, RA[:, j, ti, 64 * p:64 * p + 64], hold_b[:, j * 128 + hb * 64:j * 128 + hb * 64 + 64], True, False, [RA, hold_b], [ps1])
                            mm(o, cm[:, sl(h_), 64 * p:64 * p + 64], TOKV[:, ti, h_ * 64:(h_ + 1) * 64], False, True, [cm, TOKV], [ps1])
                        act(X1sb[rows, :], ps1[rows, :], AF.Copy, [ps1], [X1sb])
                        yield
                        ps2 = bank()
                        for h_ in range(8):
                            mm(ps2[rows, h_ * 64:(h_ + 1) * 64], tts[:, sl(h_), 64 * p:64 * p + 64], X1sb[:, h_ * 64:(h_ + 1) * 64], True, True, [tts, X1sb], [ps2])
                        cp(P.dve, Usb[rows, :], ps2[rows, :], [ps2], [Usb])
                        yield
                        ps4 = bank()
                        for h_ in range(8):
                            j = h_ // 2; hb = h_ % 2
                            o = ps4[rows, h_ * 64:(h_ + 1) * 64]
                            mm(o, RA[:, j, ti, 128 + 64 * p:128 + 64 * p + 64], hold_b[:, j * 128 + hb * 64:j * 128 + hb * 64 + 64], True, False, [RA, hold_b], [ps4])
                            mm(o, cm[:, sl(h_), 384 + 64 * p:384 + 64 * p + 64], Usb[:, h_ * 64:(h_ + 1) * 64], False, False, [cm, Usb], [ps4])
                            mm(o, cm[:, sl(h_), 128 + 64 * p:128 + 64 * p + 64], TOKV[:, ti, h_ * 64:(h_ + 1) * 64], False, True, [cm, TOKV], [ps4])
                        act(Ysb[rows, :], ps4[rows, :], AF.Copy, [ps4], [Ysb])
                        yield
                        ps3 = bank()
                        for j in range(4):
                            js = slice(j * 128, (j + 1) * 128)
                            mm(ps3[:, js], TOKB2[rows, ti, js], Usb[rows, js], True, False, [TOKB2, Usb], [ps3])
                            mm(ps3[:, js], TOKK2[rows, ti, js], TOKV[rows, ti, js], False, True, [TOKK2, TOKV], [ps3])
                        tt(P.dve, htmp[:], ps3[:, :], bd4[:], ALU.mult, [ps3, bd4], [htmp])
                        tt(P.dve, hnew_f[:], hnew_f[:], htmp[:], ALU.add, [hnew_f, htmp], [hnew_f])
                        act(hnew_b[:], hnew_f[:], AF.Copy, [hnew_f], [hnew_b])
                        hcur[0] = 1 - hcur[0]
                        yield
                    y3 = Ysb[:, :].rearrange("p (h v) -> p h v", v=64)
                    P.op(P.dve, lambda h: h.tensor_reduce(out=gmean[:], in_=y3, axis=AX.X, op=ALU.add), r=[Ysb], w=[gmean])
                    ts(P.dve, gmean[:], gmean[:], 1.0 / 64, None, ALU.mult, None, [gmean], [gmean])
                    tt(P.dve, yc[:].rearrange("p (h v) -> p h v", v=64), y3, gmean[:, :].unsqueeze(2).to_broadcast([128, 8, 64]), ALU.subtract, [Ysb, gmean], [yc])
                    tt(P.dve, ysq[:], yc[:], yc[:], ALU.mult, [yc], [ysq])
                    P.op(P.dve, lambda h: h.tensor_reduce(out=gvar[:], in_=ysq[:].rearrange("p (h v) -> p h v", v=64), axis=AX.X, op=ALU.add), r=[ysq], w=[gvar])
                    ts(P.dve, gvar[:], gvar[:], 1.0 / 64, 64e-5, ALU.mult, ALU.add, [gvar], [gvar])
                    act(gvar[:], gvar[:], AF.Sqrt, [gvar], [gvar])
                    P.op(P.dve, lambda h: h.reciprocal(out=gvar[:], in_=gvar[:]), r=[gvar], w=[gvar])
                    tt(P.dve, ynb[:, ti, :].rearrange("p (h v) -> p h v", v=64), yc[:].rearrange("p (h v) -> p h v", v=64), gvar[:, :].unsqueeze(2).to_broadcast([128, 8, 64]), ALU.mult, [yc, gvar], [ynb])

                def drive(*gens):
                    gens = [g for g in gens if g is not None]
                    while gens:
                        for g in list(gens):
                            try:
                                next(g)
                            except StopIteration:
                                gens.remove(g)
                drive(gen_D(0))
                for ti in range(4):
                    drive(gen_S(ti), gen_D(ti + 1) if ti < 3 else None)
                for j in range(4):
                    bb = bbank()
                    for ti in range(4):
                        tr(bb[:, ti * 128:(ti + 1) * 128], ynb[:, ti, j * 128:(j + 1) * 128], identb[:], [ynb, identb], [bb])
                    act(yt1[:], bb[:, 0:512], AF.Identity, [bb, lng, lnb], [yt1], scale=lng[:, j:j + 1], bias=lnb[:, j:j + 1])
                    tt(P.dve, yt1[:], yt1[:], bonT[j][:], ALU.add, [yt1, bonT[j]], [yt1])
                    tt(P.dve, yaT[:, j, :], yt1[:], gT[j][:], ALU.mult, [yt1, gT[j]], [yaT])
                    stq(yaT_d[j * 128:(j + 1) * 128, t0:t0 + ST], yaT[:, j, :], [yaT], [yaT_d], is_out=(dbg == "yaT_d"), q=P.pool)
            P.barrier()
        P.stack = gs
        if dbg == "yaT_d":
            P.finish()
            return nc, P

        with ExitStack() as ph, nc.allow_non_contiguous_dma(reason="tiny per-channel vectors"):
            P.stack = ph
            C0 = RW
            winu = P.tile("winu", [128, 8, 512], BF16); winv = P.tile("winv", [128, 8, 512], BF16)
            wing = P.tile("wing", [128, 8, 2048], BF16)
            woa = P.tile("woa", [128, 4, D], BF16); wob = P.tile("wob", [128, 4, D], BF16); wo = P.tile("wo", [128, 8, D], BF16)
            for dc in range(8):
                ldc(winu[:, dc, :], w_in[dc * 128:(dc + 1) * 128, C0:C0 + 512], [winu])
                ldc(winv[:, dc, :], w_in[dc * 128:(dc + 1) * 128, C0 + 512:C0 + 1024], [winv])
                ldc(wing[:, dc, 0:1024], w_in[dc * 128:(dc + 1) * 128, C0 + 1024:C0 + 2048], [wing])
                ldc(wing[:, dc, 1024:2048], w_in[dc * 128:(dc + 1) * 128, C0 + 2048:C0 + 3072], [wing])
                ldc(wo[:, dc, :], w_out[dc * 128:(dc + 1) * 128, :], [wo])
            for q in range(4):
                ldc(woa[:, q, :], w_out_a[q * 128:(q + 1) * 128, :], [woa])
                ldc(wob[:, q, :], w_out_b[q * 128:(q + 1) * 128, :], [wob])
            mU = P.tile("mU", [128, 128], F32)
            ms(P.pool, mU[:], 1.0, [mU])
            P.op(P.pool, lambda h: h.affine_select(out=mU[:], in_=mU[:], pattern=[[1, 128]], compare_op=ALU.is_ge, fill=0.0, base=0, channel_multiplier=-1), r=[mU], w=[mU])
            wsf = P.tile("wsf", [128, 8, 128], F32)
            for g_ in range(8):
                ld(wsf[:, g_, :], gmlp_ws[g_], [wsf])
            wsmT = P.tile("wsmT", [128, 8, 128], BF16)
            for g4 in range(2):
                pb = bank()
                for gg in range(4):
                    tr(pb[:, gg * 128:(gg + 1) * 128], wsf[:, g4 * 4 + gg, :], identf[:], [wsf, identf], [pb])
                tt(P.dve, wsmT[:, g4 * 4:(g4 + 1) * 4, :], pb[:, :].rearrange("p (a t) -> p a t", t=128), mU[:, :].unsqueeze(1).to_broadcast([128, 4, 128]), ALU.mult, [pb, mU], [wsmT])
            bsT = P.tile("bsT", [128, 4, 128], F32)
            for g_ in range(8):
                ld(bsT[(g_ % 2) * 64:(g_ % 2) * 64 + 64, g_ // 2, :], gmlp_bs[g_].partition_broadcast(64), [bsT])
            lngbc = P.tile("lngbc", [128, 512], F32); lnbbc = P.tile("lnbbc", [128, 512], F32)
            ld(lngbc[:], gmlp_ln_g.partition_broadcast(128), [lngbc]); ld(lnbbc[:], gmlp_ln_b.partition_broadcast(128), [lnbbc])

            xts = [P.tile(f"bxt{i}", [128, D], F32) for i in range(2)]
            hT = P.tile("bhT", [128, 8, 512], BF16)
            ssq = P.tile("bssq", [128, 4], F32); rstd = P.tile("brstd", [128, 4], F32)
            xn = P.tile("bxn", [128, 4, D], BF16)
            ntmp = (ssq, rstd, xn, None)
            uT = P.tile("uT", [128, 4, 512], BF16)
            vg = P.tile("vg", [128, 512], F32); vc = P.tile("vc", [128, 512], F32)
            vst = P.tile("vst", [128, 4], F32)
            vln = P.tile("vln", [128, 4, 512], BF16)
            ybT = P.tile("ybT", [128, 4, 512], BF16)
            gts = P.tile("gts", [128, 16, 512], BF16)
            yaTs = P.tile("yaTs", [128, 4, 512], BF16)
            mgT = P.tile("mgT", [128, 8, 512], BF16)
            t1 = P.tile("t1", [128, 512], F32); t2_ = P.tile("t2_", [128, 512], F32)
            xo = [P.tile(f"xo{i}", [128, D], F32) for i in range(2)]
            for si in range(nst):
                t0 = si * ST
                norm_T(xts, hT, x, si, s1, sh1, ntmp)
                for q in range(4):
                    ld(yaTs[:, q, :], yaT_d[q * 128:(q + 1) * 128, t0:t0 + ST], [yaTs], r=[yaT_d])
                for q in range(4):
                    pb = bank()
                    for dc in range(8):
                        mm(pb[:, :], winu[:, dc, q * 128:(q + 1) * 128], hT[:, dc, :], dc == 0, dc == 7, [winu, hT], [pb])
                    act(uT[:, q, :], pb[:, :], AF.Gelu, [pb], [uT])
                for ti in range(4):
                    pb = bank()
                    for dc in range(8):
                        mm(pb[:, :], hT[:, dc, ti * 128:(ti + 1) * 128], winv[:, dc, :], dc == 0, dc == 7, [winv, hT], [pb])
                    act(vg[:], pb[:, :], AF.Gelu, [pb], [vg, vst], accum_out=vst[:, 0:1])
                    ts(P.dve, vst[:, 1:2], vst[:, 0:1], 1.0 / 512, None, ALU.mult, None, [vst], [vst])
                    ts(P.dve, vc[:], vg[:], vst[:, 1:2], None, ALU.subtract, None, [vg, vst], [vc])
                    act(vg[:], vc[:], AF.Square, [vc], [vg, vst], accum_out=vst[:, 2:3])
                    ts(P.dve, vst[:, 3:4], vst[:, 2:3], 1.0 / 512, 1e-5, ALU.mult, ALU.add, [vst], [vst])
                    act(vst[:, 3:4], vst[:, 3:4], AF.Sqrt, [vst], [vst])
                    P.op(P.dve, lambda h: h.reciprocal(out=vst[:, 3:4], in_=vst[:, 3:4]), r=[vst], w=[vst])
                    stt(P.dve, vc[:], vc[:], vst[:, 3:4], lngbc[:], ALU.mult, ALU.mult, [vc, vst, lngbc], [vc])
                    tt(P.dve, vln[:, ti, :], vc[:], lnbbc[:], ALU.add, [vc, lnbbc], [vln])
                for q in range(4):
                    pb = bank()
                    for ti in range(4):
                        for gg in range(2):
                            g_ = 2 * q + gg
                            mm(pb[gg * 64:(gg + 1) * 64, ti * 128:(ti + 1) * 128], vln[:, ti, g_ * 64:(g_ + 1) * 64], wsmT[:, g_, :], True, True, [vln, wsmT], [pb])
                    tt(P.dve, t1[:].rearrange("p (a t) -> p a t", t=128), pb[:, :].rearrange("p (a t) -> p a t", t=128), bsT[:, q, :].unsqueeze(1).to_broadcast([128, 4, 128]), ALU.add, [pb, bsT], [t1])
                    tt(P.dve, ybT[:, q, :], t1[:], uT[:, q, :], ALU.mult, [t1, uT], [ybT])
                for q in range(16):
                    pb = bank()
                    for dc in range(8):
                        mm(pb[:, :], wing[:, dc, q * 128:(q + 1) * 128], hT[:, dc, :], dc == 0, dc == 7, [wing, hT], [pb])
                    act(gts[:, q, :], pb[:, :], AF.Sigmoid, [pb], [gts])
                for m in range(8):
                    pa = bank()
                    for q in range(4):
                        mm(pa[:, :], woa[:, q, m * 128:(m + 1) * 128], yaTs[:, q, :], q == 0, q == 3, [woa, yaTs], [pa])
                    pb = bank()
                    for q in range(4):
                        mm(pb[:, :], wob[:, q, m * 128:(m + 1) * 128], ybT[:, q, :], q == 0, q == 3, [wob, ybT], [pb])
                    tt(P.dve, t1[:], pa[:, :], gts[:, m, :], ALU.mult, [pa, gts], [t1])
                    tt(P.dve, t2_[:], pb[:, :], gts[:, 8 + m, :], ALU.mult, [pb, gts], [t2_])
                    tt(P.dve, mgT[:, m, :], t1[:], t2_[:], ALU.add, [t1, t2_], [mgT])
                for ti in range(4):
                    xt = xts[ti % 2]; xo_ = xo[ti % 2]
                    ld(xt[:], x[t0 + ti * 128:t0 + (ti + 1) * 128, :], [xt])
                    for hf in range(2):
                        pb = bank()
                        for m in range(8):
                            mm(pb[:, :], mgT[:, m, ti * 128:(ti + 1) * 128], wo[:, m, hf * 512:(hf + 1) * 512], m == 0, m == 7, [mgT, wo], [pb])
                        tt(P.dve, t1[:], pb[:, :], g1bc[:, hf * 512:(hf + 1) * 512], ALU.mult, [pb, g1bc], [t1])
                        tt(P.dve, xo_[:, hf * 512:(hf + 1) * 512], t1[:], xt[:, hf * 512:(hf + 1) * 512], ALU.add, [t1, xt], [xo_])
                    stq(x1_d[t0 + ti * 128:t0 + (ti + 1) * 128, :], xo_[:], [xo_], [x1_d], is_out=(dbg == "x1_d"), q=P.pool)
            P.barrier()
        P.stack = gs
        if dbg == "x1_d":
            P.finish()
            return nc, P

        NT = nst * 4
        dest8 = P.tile("dest8", [128, 64, 8], I32)
        w8 = P.tile("w8", [128, 64, 8], F32)
        idxw = P.tile("idxw", [128, NBLK], I32)
        w8b = [Buf(f"w8b{i}") for i in range(64)]; d8b = [Buf(f"d8b{i}") for i in range(64)]
        with ExitStack() as ph, nc.allow_non_contiguous_dma(reason="tiny per-channel vectors"):
            P.stack = ph
            zt = P.tile("zt", [128, 8192], BF16)
            ms(P.pool, zt[:], 0.0, [zt])
            nzb = NSLOT // 1024
            for i in range(nzb):
                stq(xg_d[i * 1024:(i + 1) * 1024, :].rearrange("(p r) d -> p (r d)", p=128), zt[:], [zt], [xg_d])
            rw = P.tile("rw", [128, 8, NE], BF16)
            sw1 = P.tile("sw1", [128, 8, 256], BF16); sw3 = P.tile("sw3", [128, 8, 256], BF16); sw2 = P.tile("sw2", [128, 2, D], BF16)
            for dc in range(8):
                ldc(rw[:, dc, :], router_w[dc * 128:(dc + 1) * 128, :], [rw])
                ldc(sw1[:, dc, :], shared_w1[dc * 128:(dc + 1) * 128, :], [sw1])
                ldc(sw3[:, dc, :], shared_w3[dc * 128:(dc + 1) * 128, :], [sw3])
            for fc in range(2):
                ldc(sw2[:, fc, :], shared_w2[fc * 128:(fc + 1) * 128, :], [sw2])
            rbias = P.tile("rbias", [128, NE], F32)
            ld(rbias[:], router_bias.partition_broadcast(128), [rbias])
            eoff = P.tile("eoff", [128, NE], F32)
            ustr = P.tile("ustr", [128, 128], BF16)
            onesb = P.tile("onesb", [128, 128], BF16)
            cp(P.dve, ustr[:], mU[:], [mU], [ustr]) if False else None
            uf = P.tile("uf", [128, 128], F32)
            ms(P.pool, uf[:], 1.0, [uf])
            P.op(P.pool, lambda h: h.affine_select(out=uf[:], in_=uf[:], pattern=[[1, 128]], compare_op=ALU.is_gt, fill=0.0, base=0, channel_multiplier=-1), r=[uf], w=[uf])
            cp(P.dve, ustr[:], uf[:], [uf], [ustr])
            ms(P.dve, onesb[:], 1.0, [onesb])
            basec = P.tile("basec", [128, NE], F32)
            ms(P.dve, basec[:], 0.0, [basec])

            xts = [P.tile(f"cxt{i}", [128, D], F32) for i in range(2)]
            hT = P.tile("chT", [128, 8, 512], BF16)
            ssq = P.tile("cssq", [128, 4], F32); rstd = P.tile("crstd", [128, 4], F32)
            xn = P.tile("cxn", [128, 4, D], BF16)
            ntmp = (ssq, rstd, xn, None)
            h2row = [P.tile(f"h2row{i}", [128, D], BF16) for i in range(2)]
            class _S:
                pass

            def mkset(n):
                S = _S()
                S.sc_ = P.tile(f"sc_{n}", [128, NE], F32); S.sel = P.tile(f"sel{n}", [128, NE], F32)
                S.m88 = P.tile(f"m88{n}", [128, 8, 8], F32); S.gs_ = P.tile(f"gs_{n}", [128, 8], F32)
                S.g8 = P.tile(f"g8{n}", [128, 8], F32); S.gmask = P.tile(f"gmask{n}", [128, 8], F32)
                S.selm = P.tile(f"selm{n}", [128, NE], F32); S.smask = P.tile(f"smask{n}", [128, NE], F32)
                S.smb = P.tile(f"smb{n}", [128, NE], BF16)
                S.wd = P.tile(f"wd{n}", [128, NE], F32); S.wsum = P.tile(f"wsum{n}", [128, 2], F32)
                S.key = P.tile(f"key{n}", [128, NE], F32); S.k8 = P.tile(f"k8{n}", [128, 8], F32)
                S.kz = P.tile(f"kz{n}", [128, 8], F32); S.jk = P.tile(f"jk{n}", [128, NE], F32)
                S.t2_ = P.tile(f"ct2{n}", [128, 512], F32)
                return S
            SS = [mkset(0), mkset(1)]
            hsT = P.tile("hsT", [128, 2, 512], BF16)
            t1 = P.tile("ct1", [128, 512], F32)
            xo = [P.tile(f"cxo{i}", [128, D], F32) for i in range(2)]

            def route(tsl, S):
                pb = bank()
                for dc in range(8):
                    mm(pb[:, 0:NE], hT[:, dc, tsl], rw[:, dc, :], dc == 0, dc == 7, [hT, rw], [pb])
                act(S.sc_[:], pb[:, 0:NE], AF.Sigmoid, [pb], [S.sc_])
                yield
                tt(P.dve, S.sel[:], S.sc_[:], rbias[:], ALU.add, [S.sc_, rbias], [S.sel])
                yield
                for g_ in range(8):
                    P.op(P.dve, lambda h: h.max(out=S.m88[:, g_, :], in_=S.sel[:, g_ * 32:(g_ + 1) * 32]), r=[S.sel], w=[S.m88])
                yield
                tt(P.dve, S.gs_[:], S.m88[:, :, 0], S.m88[:, :, 1], ALU.add, [S.m88], [S.gs_])
                yield
                P.op(P.dve, lambda h: h.max(out=S.g8[:], in_=S.gs_[:]), r=[S.gs_], w=[S.g8])
                yield
                ts(P.dve, S.gmask[:], S.gs_[:], S.g8[:, 3:4], None, ALU.is_ge, None, [S.gs_, S.g8], [S.gmask])
                yield
                stt(P.dve, S.selm[:].rearrange("p (g e) -> p g e", e=32), S.sel[:].rearrange("p (g e) -> p g e", e=32), 2.0, S.gmask[:, :].unsqueeze(2).to_broadcast([128, 8, 32]), ALU.add, ALU.mult, [S.sel, S.gmask], [S.selm])
                yield
                P.op(P.dve, lambda h: h.max(out=S.g8[:], in_=S.selm[:]), r=[S.selm], w=[S.g8])
                yield
                ts(P.dve, S.smask[:], S.selm[:], S.g8[:, 7:8], None, ALU.is_ge, None, [S.selm, S.g8], [S.smask])
                yield
                cp(P.dve, S.smb[:], S.smask[:], [S.smask], [S.smb])
                yield

            def drive(*gens):
                gens = [g for g in gens if g is not None]
                while gens:
                    for g in list(gens):
                        try:
                            next(g)
                        except StopIteration:
                            gens.remove(g)

            def gen_p1(ti, S):
                yield from route(slice(ti * 128, (ti + 1) * 128), S)
                pp = bank()
                mm(pp[:, 0:NE], onesb[:], S.smb[:], True, True, [onesb, S.smb], [pp])
                tt(P.dve, basec[:], basec[:], pp[:, 0:NE], ALU.add, [pp, basec], [basec])
                yield

            for si in range(nst):
                norm_T(xts, hT, x1_d.t, si, s2, sh2, ntmp)
                drive(gen_p1(0, SS[0]), gen_p1(1, SS[1]))
                drive(gen_p1(2, SS[0]), gen_p1(3, SS[1]))
            nblk = P.tile("nblk", [128, NE], F32); pends = P.tile("pends", [128, NE], F32)
            ones256 = P.tile("ones256", [128, NE], F32)
            ms(P.dve, nblk[:], 0.0, [nblk]); ms(P.dve, ones256[:], 1.0, [ones256])
            for m_ in range(T // BLK):
                stt(P.dve, nblk[:], basec[:], float(BLK * m_), nblk[:], ALU.is_gt, ALU.add, [basec, nblk], [nblk])
            ts(P.dve, nblk[:], nblk[:], float(BLK), None, ALU.mult, None, [nblk], [nblk])
            P.op(P.dve, lambda h: h.tensor_tensor_scan(out=pends[:], data0=ones256[:], data1=nblk[:], initial=0.0, op0=ALU.mult, op1=ALU.add), r=[ones256, nblk], w=[pends])
            tt(P.dve, eoff[:], pends[:], nblk[:], ALU.subtract, [pends, nblk], [eoff])
            ts(P.dve, eoff[:], eoff[:], 1.0, None, ALU.add, None, [eoff], [eoff])
            pcol = P.tile("pcol", [128, 2], F32)
            for c_ in range(2):
                pb = bank()
                tr(pb[:, 0:128], pends[:, c_ * 128:(c_ + 1) * 128], identf[:], [pends, identf], [pb])
                cp(P.dve, pcol[:, c_:c_ + 1], pb[:, 0:1], [pb], [pcol])
            iotab = P.tile("iotab", [128, NBLK], F32)
            P.op(P.pool, lambda h: h.iota(iotab[:], pattern=[[BLK, NBLK]], base=0, channel_multiplier=0, allow_small_or_imprecise_dtypes=True), w=[iotab])
            cmpb = P.tile("cmpb", [128, 2, NBLK], BF16)
            for c_ in range(2):
                ts(P.dve, cmpb[:, c_, :], iotab[:], pcol[:, c_:c_ + 1], None, ALU.is_ge, None, [iotab, pcol], [cmpb])
            pb = bank()
            for c_ in range(2):
                mm(pb[:, :], onesb[:], cmpb[:, c_, :], c_ == 0, c_ == 1, [onesb, cmpb], [pb])
            pidx = P.tile("pidx", [128, NBLK], F32)
            P.op(P.pool, lambda h: h.iota(pidx[:], pattern=[[0, NBLK]], base=0, channel_multiplier=1, allow_small_or_imprecise_dtypes=True), w=[pidx])
            ts(P.dve, iotab[:], pb[:, :], 128.0, None, ALU.mult, None, [pb], [iotab])
            tt(P.dve, idxw[:], iotab[:], pidx[:], ALU.add, [iotab, pidx], [idxw])
            ms(P.dve, basec[:], 0.0, [basec])

            def gen_p2(si, ti, S):
                t0 = si * ST
                tg = si * 4 + ti
                tsl = slice(ti * 128, (ti + 1) * 128)
                hr = h2row[ti % 2]
                bb = bbank()
                for dc in range(8):
                    tr(bb[:, dc * 128:(dc + 1) * 128], hT[:, dc, tsl], identb[:], [hT, identb], [bb])
                cp(P.dve, hr[:], bb[:, :], [bb], [hr])
                yield
                yield from route(tsl, S)
                stt(P.dve, S.wd[:], S.smask[:], 1.0, S.sc_[:], ALU.mult, ALU.mult, [S.smask, S.sc_], [S.wd, S.wsum], accum_out=S.wsum[:, 0:1])
                yield
                P.op(P.dve, lambda h: h.reciprocal(out=S.wsum[:, 1:2], in_=S.wsum[:, 0:1]), r=[S.wsum], w=[S.wsum])
                yield
                ts(P.dve, S.wd[:], S.wd[:], S.wsum[:, 1:2], 2.5, ALU.mult, ALU.mult, [S.wd, S.wsum], [S.wd])
                pp = bank()
                mm(pp[:, 0:NE], ustr[:], S.smb[:], True, True, [ustr, S.smb], [pp])
                mm(pp[:, NE:2 * NE], onesb[:], S.smb[:], True, True, [onesb, S.smb], [pp])
                tt(P.dve, S.key[:], pp[:, 0:NE], basec[:], ALU.add, [pp, basec], [S.key])
                tt(P.dve, basec[:], basec[:], pp[:, NE:2 * NE], ALU.add, [pp, basec], [basec])
                yield
                tt(P.dve, S.key[:], S.key[:], eoff[:], ALU.add, [S.key, eoff], [S.key])
                yield
                tt(P.dve, S.key[:], S.key[:], S.smask[:], ALU.mult, [S.key, S.smask], [S.key])
                yield
                P.op(P.dve, lambda h: h.max(out=S.k8[:], in_=S.key[:]), r=[S.key], w=[S.k8])
                yield
                for k in range(8):
                    stt(P.dve, S.jk[:], S.key[:], S.k8[:, k:k + 1], S.wd[:], ALU.is_equal, ALU.mult, [S.key, S.k8, S.wd], [S.jk, w8b[tg]], accum_out=w8[:, tg, k:k + 1])
                    yield
                ts(P.dve, S.kz[:], S.k8[:], 0.0, float(NSLOT), ALU.is_equal, ALU.mult, [S.k8], [S.kz])
                yield
                stt(P.dve, dest8[:, tg, :], S.k8[:], -1.0, S.kz[:], ALU.add, ALU.add, [S.k8, S.kz], [d8b[tg]])
                yield
                for k in range(8):
                    l = LP[lpi[0] % len(LP)]; lpi[0] += 1
                    P.dma(P.pool, l, lambda h: h.indirect_dma_start(out=xg_d.t, out_offset=bass.IndirectOffsetOnAxis(ap=dest8[:, tg, k:k + 1], axis=0), in_=hr[:], in_offset=None), r=[hr, d8b[tg]], w=[xg_d])
                yield
                xt = xts[ti % 2]; xo_ = xo[ti % 2]
                ld(xt[:], x1_d[t0 + ti * 128:t0 + (ti + 1) * 128, :], [xt])
                for hf in range(2):
                    pb = bank()
                    for fc in range(2):
                        mm(pb[:, :], hsT[:, fc, tsl], sw2[:, fc, hf * 512:(hf + 1) * 512], fc == 0, fc == 1, [hsT, sw2], [pb])
                    tt(P.dve, S.t2_[:], pb[:, :], g2bc[:, hf * 512:(hf + 1) * 512], ALU.mult, [pb, g2bc], [S.t2_])
                    yield
                    tt(P.dve, xo_[:, hf * 512:(hf + 1) * 512], S.t2_[:], xt[:, hf * 512:(hf + 1) * 512], ALU.add, [S.t2_, xt], [xo_])
                    yield
                stq(x1_d[t0 + ti * 128:t0 + (ti + 1) * 128, :], xo_[:], [xo_], [x1_d], q=P.act)
                yield

            for si in range(nst):
                norm_T(xts, hT, x1_d.t, si, s2, sh2, ntmp)
                for fc in range(2):
                    p1 = bank()
                    for dc in range(8):
                        mm(p1[:, :], sw1[:, dc, fc * 128:(fc + 1) * 128], hT[:, dc, :], dc == 0, dc == 7, [sw1, hT], [p1])
                    p3 = bank()
                    for dc in range(8):
                        mm(p3[:, :], sw3[:, dc, fc * 128:(fc + 1) * 128], hT[:, dc, :], dc == 0, dc == 7, [sw3, hT], [p3])
                    act(t1[:], p1[:, :], AF.Silu, [p1], [t1])
                    tt(P.dve, hsT[:, fc, :], t1[:], p3[:, :], ALU.mult, [t1, p3], [hsT])
                drive(gen_p2(si, 0, SS[0]), gen_p2(si, 1, SS[1]))
                drive(gen_p2(si, 2, SS[0]), gen_p2(si, 3, SS[1]))
            P.barrier()
            if dbg == "pB":
                P.finish()
                raise StopBuild()
        P.stack = gs

        with ExitStack() as ph:
            P.stack = ph
            w1v = exp_w1.rearrange("e (p c) f -> (e p) (c f)", c=8)
            w3v = exp_w3.rearrange("e (p c) f -> (e p) (c f)", c=8)
            w2v = exp_w2.rearrange("e (p c) d -> (e p) (c d)", c=2)
            xgt = [P.tile(f"xgt{i}", [128, 2, D], BF16) for i in range(3)]
            xgT = [P.tile(f"xgT{i}", [128, 8, BLK], BF16) for i in range(2)]
            w1b = [P.tile(f"w1b{i}", [128, 2048], BF16) for i in range(3)]
            w3b = [P.tile(f"w3b{i}", [128, 2048], BF16) for i in range(3)]
            w2b = [P.tile(f"w2b{i}", [128, 2048], BF16) for i in range(3)]
            hid = [P.tile(f"hid{i}", [128, 2, BLK], BF16) for i in range(2)]
            st1 = [P.tile(f"st1{i}", [128, BLK], F32) for i in range(2)]
            yrow = [P.tile(f"yrow{i}", [128, 2, D], BF16) for i in range(2)]
            LY = P.lanes(2, "ly")

            bc_reg = nc.gpsimd.to_reg(NE * 128 - 1)
            def wgather(dst, src, i_):
                l = LP[lpi[0] % len(LP)]; lpi[0] += 1
                P.dma(P.pool, l, lambda h: h.indirect_dma_start(out=dst[:], out_offset=None, in_=src, in_offset=bass.IndirectOffsetOnAxis(ap=idxw[:, i_:i_ + 1], axis=0), bounds_check=bc_reg, oob_is_err=False), r=[idxw], w=[dst])

            def c_loads(i_):
                i3 = i_ % 3
                ld(xgt[i3][:], xg_d[i_ * BLK:(i_ + 1) * BLK, :].rearrange("(b p) d -> p b d", p=128), [xgt[i3]], r=[xg_d])
                wgather(w1b[i3], w1v, i_); wgather(w3b[i3], w3v, i_); wgather(w2b[i3], w2v, i_)

            def c_T(i_):
                i3 = i_ % 3; i2 = i_ % 2
                xv = xgt[i3][:].rearrange("p b (q c) -> p b c q", c=8)
                for dc in range(8):
                    bb = bbank()
                    for b_ in range(2):
                        tr(bb[:, b_ * 128:(b_ + 1) * 128], xv[:, b_, dc, :], identb[:], [xgt[i3], identb], [bb])
                    cp(P.dve, xgT[i2][:, dc, :], bb[:, 0:BLK], [bb], [xgT[i2]])

            def c_H(i_):
                i3 = i_ % 3; i2 = i_ % 2
                w1r = w1b[i3][:].rearrange("p (c m two) -> p c two m", c=8, two=2)
                w3r = w3b[i3][:].rearrange("p (c m two) -> p c two m", c=8, two=2)
                for fc in range(2):
                    p1 = bank()
                    for dc in range(8):
                        mm(p1[:, 0:BLK], w1r[:, dc, fc, :], xgT[i2][:, dc, :], dc == 0, dc == 7, [w1b[i3], xgT[i2]], [p1])
                    p3 = bank()
                    for dc in range(8):
                        mm(p3[:, 0:BLK], w3r[:, dc, fc, :], xgT[i2][:, dc, :], dc == 0, dc == 7, [w3b[i3], xgT[i2]], [p3])
                    act(st1[fc][:], p1[:, 0:BLK], AF.Silu, [p1], [st1[fc]])
                    tt(P.dve, hid[i2][:, fc, :], st1[fc][:], p3[:, 0:BLK], ALU.mult, [st1[fc], p3], [hid[i2]])

            def c_Y(i_):
                i3 = i_ % 3; i2 = i_ % 2
                w2r = w2b[i3][:].rearrange("p (c d) -> p c d", c=2)
                for b_ in range(2):
                    for hf in range(2):
                        pb = bank()
                        for fc in range(2):
                            mm(pb[:, :], hid[i2][:, fc, b_ * 128:(b_ + 1) * 128], w2r[:, fc, hf * 512:(hf + 1) * 512], fc == 0, fc == 1, [hid[i2], w2b[i3]], [pb])
                        act(yrow[i2][:, b_, hf * 512:(hf + 1) * 512], pb[:, :], AF.Copy, [pb], [yrow[i2]])
                P.dma(P.act, LY[i2], lambda h: h.dma_start(out=yg_d[i_ * BLK:(i_ + 1) * BLK, :].rearrange("(b p) d -> p b d", p=128), in_=yrow[i2][:]), r=[yrow[i2]], w=[yg_d])

            nb_ = nblk_run
            c_loads(0)
            if nb_ > 1:
                c_loads(1)
            c_T(0)
            for i_ in range(nb_):
                if i_ + 1 < nb_:
                    c_T(i_ + 1)
                c_H(i_)
                if i_ >= 1:
                    c_Y(i_ - 1)
                if i_ + 2 < nb_:
                    c_loads(i_ + 2)
            c_Y(nb_ - 1)
            P.barrier()
            if dbg == "pC":
                P.finish()
                raise StopBuild()
        P.stack = gs

        with ExitStack() as ph:
            P.stack = ph
            nfbc = P.tile("nfbc", [128, D], F32)
            ld(nfbc[:], normf_g.partition_broadcast(128), [nfbc])
            xts = [P.tile(f"dxt{i}", [128, D], F32) for i in range(2)]
            gat = [P.tile(f"gat{i}", [128, D], BF16) for i in range(4)]
            acc = P.tile("acc", [128, D], F32)
            ot = [P.tile(f"ot{i}", [128, D], F32) for i in range(2)]
            fs = P.tile("fs", [128, 2], F32)
            jk2 = P.tile("jk2", [128, D], BF16)
            for tg in range(NT):
                xt = xts[tg % 2]; o_ = ot[tg % 2]
                ld(xt[:], x1_d[tg * 128:(tg + 1) * 128, :], [xt])
                for k in range(8):
                    gt = gat[k % 4]
                    l = LP[lpi[0] % len(LP)]; lpi[0] += 1
                    P.dma(P.pool, l, lambda h: h.indirect_dma_start(out=gt[:], out_offset=None, in_=yg_d.t, in_offset=bass.IndirectOffsetOnAxis(ap=dest8[:, tg, k:k + 1], axis=0)), r=[yg_d, d8b[tg]], w=[gt])
                    if k == 0:
                        ts(P.dve, acc[:], gt[:], w8[:, tg, 0:1], None, ALU.mult, None, [gt, w8b[tg]], [acc])
                    else:
                        stt(P.dve, acc[:], gt[:], w8[:, tg, k:k + 1], acc[:], ALU.mult, ALU.add, [gt, w8b[tg], acc], [acc])
                tt(P.dve, acc[:], acc[:], g2bc[:], ALU.mult, [acc, g2bc], [acc])
                tt(P.dve, acc[:], acc[:], xt[:], ALU.add, [acc, xt], [acc])
                act(jk2[:], acc[:], AF.Square, [acc], [jk2, fs], accum_out=fs[:, 0:1])
                ts(P.dve, fs[:, 1:2], fs[:, 0:1], 1.0 / D, 1e-6, ALU.mult, ALU.add, [fs], [fs])
                act(fs[:, 1:2], fs[:, 1:2], AF.Sqrt, [fs], [fs])
                P.op(P.dve, lambda h: h.reciprocal(out=fs[:, 1:2], in_=fs[:, 1:2]), r=[fs], w=[fs])
                stt(P.dve, o_[:], acc[:], fs[:, 1:2], nfbc[:], ALU.mult, ALU.mult, [acc, fs, nfbc], [o_])
                stq(out[tg * 128:(tg + 1) * 128, :], o_[:], [o_], is_out=True, q=P.act)
            P.finish()
        P.stack = gs
        return nc, P


_NAMES = ["ada_w", "ada_b", "norm1_g", "norm2_g", "w_in", "tshift_mu", "rwkv_w0", "rwkv_w_up", "rwkv_a0", "rwkv_a_up",
          "rwkv_g_up", "rwkv_k_k", "rwkv_k_a", "rwkv_r_k", "rwkv_ln_g", "rwkv_ln_b", "gmlp_ln_g", "gmlp_ln_b", "gmlp_ws",
          "gmlp_bs", "w_out_a", "w_out_b", "w_out", "router_w", "router_bias", "exp_w1", "exp_w3", "exp_w2",
          "shared_w1", "shared_w3", "shared_w2"]


def kernel(**inputs):
    nc, _ = build()
    shared = {}
    for k in _NAMES:
        a = np.asarray(inputs[k], dtype=np.float32)[0]
        if k == "rwkv_r_k":
            a = a.reshape(512)
        shared[k] = np.ascontiguousarray(a)
    shared["normf_g"] = np.ascontiguousarray(np.asarray(inputs["normf_g"], dtype=np.float32))
    x = np.asarray(inputs["x"], dtype=np.float32)
    c = np.asarray(inputs["c"], dtype=np.float32)
    in_maps = []
    for b in range(8):
        m = dict(shared)
        m["x"] = np.ascontiguousarray(x[b])
        m["c"] = np.ascontiguousarray(c[b:b + 1])
        in_maps.append(m)
    res = run_bass_kernel_spmd(nc, in_maps, core_ids=list(range(8)))
    return np.stack([np.asarray(r["out"], dtype=np.float32) for r in res.results], axis=0)
```

```python
import numpy as np
import concourse.bass as bass
import concourse.mybir as mybir
from concourse.bass_utils import run_bass_kernel_spmd

F32 = mybir.dt.float32
BF16 = mybir.dt.bfloat16
U32 = mybir.dt.uint32
I32 = mybir.dt.int32
AF = mybir.ActivationFunctionType
ALU = mybir.AluOpType
AX = mybir.AxisListType


class Buf:
    __slots__ = ("name", "w", "rs")

    def __init__(self, name):
        self.name = name
        self.w = None
        self.rs = []


class Tl:
    def __init__(self, t, name):
        self.t = t
        self.b = Buf(name)

    def __getitem__(self, k):
        return self.t[k]


class Eng:
    def __init__(self, P, name, h, sem):
        self.P = P
        self.name = name
        self.h = h
        self.sem = sem
        self.cnt = 0
        self.waited = {}


class Lane:
    def __init__(self, sem):
        self.sem = sem
        self.val = 0


class Prog:
    def __init__(self, nc, stack):
        self.nc = nc
        self.stack = stack
        mk = lambda n: stack.enter_context(nc.semaphore(n))
        self.pe = Eng(self, "pe", nc.tensor, mk("s_pe"))
        self.act = Eng(self, "act", nc.scalar, mk("s_act"))
        self.dve = Eng(self, "dve", nc.vector, mk("s_dve"))
        self.pool = Eng(self, "pool", nc.gpsimd, mk("s_pool"))
        self.sp = Eng(self, "sp", nc.sync, mk("s_sp"))
        self.engs = [self.pe, self.act, self.dve, self.pool, self.sp]
        self.nlanes = 0
        self.all_lanes = []
        self.out_toks = []
        self.nins = 0

    def tile(self, name, shape, dt):
        return Tl(self.sb(name, shape, dt), name)

    def ptile(self, name, shape, dt=F32):
        return Tl(self.ps(name, shape, dt), name)

    def barrier(self):
        toks = [(e.sem, e.cnt) for e in self.engs if e.cnt] + [(l.sem, l.val) for l in self.all_lanes if l.val]
        for e in self.engs:
            for t in toks:
                self._wait(e, t)

    def sb(self, name, shape, dt):
        return self.stack.enter_context(self.nc.sbuf_tensor(name, shape, dt))

    def ps(self, name, shape, dt=F32):
        return self.stack.enter_context(self.nc.psum_tensor(name, shape, dt))

    def lanes(self, n, name="ln"):
        out = []
        for i in range(n):
            out.append(Lane(self.stack.enter_context(self.nc.semaphore(f"{name}{self.nlanes}"))))
            self.nlanes += 1
        self.all_lanes += out
        return out

    def _wait(self, e, tok):
        if tok is None:
            return
        sem, val = tok
        k = id(sem)
        if e.waited.get(k, 0) >= val:
            return
        if sem is e.sem and e is self.pe:
            return
        e.h.wait_ge(sem, val)
        e.waited[k] = val
        self.nins += 1

    def _deps(self, e, r, w):
        r = [getattr(b, "b", b) for b in r]
        w = [getattr(b, "b", b) for b in w]
        for b in r:
            self._wait(e, b.w)
        for b in w:
            self._wait(e, b.w)
            for t in b.rs:
                self._wait(e, t)

    def _commit(self, tok, r, w):
        r = [getattr(b, "b", b) for b in r]
        w = [getattr(b, "b", b) for b in w]
        for b in r:
            b.rs.append(tok)
            if len(b.rs) > 24:
                d = {}
                for s, v in b.rs:
                    if id(s) not in d or d[id(s)][1] < v:
                        d[id(s)] = (s, v)
                b.rs = list(d.values())
        for b in w:
            b.w = tok
            b.rs = []

    def op(self, e, fn, r=(), w=()):
        self._deps(e, r, w)
        ins = fn(e.h)
        e.cnt += 1
        ins.then_inc(e.sem, 1)
        tok = (e.sem, e.cnt)
        self._commit(tok, r, w)
        self.nins += 1
        return tok

    def dma(self, e, lane, fn, r=(), w=(), is_out=False):
        self._wait(e, (lane.sem, lane.val) if lane.val else None)
        self._deps(e, r, w)
        ins = fn(e.h)
        lane.val += 16
        ins.then_inc(lane.sem, 16)
        tok = (lane.sem, lane.val)
        self._commit(tok, r, w)
        if is_out:
            self.out_toks.append(tok)
        self.nins += 1
        return tok

    def finish(self):
        for tok in self.out_toks:
            self._wait(self.sp, tok)


from contextlib import ExitStack

T = 8192
D = 1024
ST = 512
NST = T // ST
SDEC = -0.6065306597126334
RW = 1792
CAP = 512
BLK = 256
NBLK = 512
NE = 256
NSLOT = NE * CAP
ROW = 1024 + 64


def sl(h_):
    return (h_ % 2) * 4 + h_ // 2


class StopBuild(Exception):
    pass


def build(dbg=None, nst=NST, nblk_run=NBLK):
    nc = bass.Bass("TRN2", target_bir_lowering=False)
    holder = {}
    try:
        return _build(nc, dbg, nst, holder, nblk_run)
    except StopBuild:
        return nc, holder["P"]


def _build(nc, dbg, nst, holder, nblk_run):

    def din(name, shape, dt=F32):
        return nc.dram_tensor(name, shape, dt, kind="ExternalInput").ap()

    x = din("x", [T, D]); c = din("c", [1, D])
    ada_w = din("ada_w", [D, 6 * D]); ada_b = din("ada_b", [6 * D])
    norm1_g = din("norm1_g", [D]); norm2_g = din("norm2_g", [D])
    w_in = din("w_in", [D, 4864]); tshift_mu = din("tshift_mu", [RW])
    rwkv_w0 = din("rwkv_w0", [512]); rwkv_w_up = din("rwkv_w_up", [64, 512])
    rwkv_a0 = din("rwkv_a0", [512]); rwkv_a_up = din("rwkv_a_up", [64, 512])
    rwkv_g_up = din("rwkv_g_up", [128, 512]); rwkv_k_k = din("rwkv_k_k", [512])
    rwkv_k_a = din("rwkv_k_a", [512]); rwkv_r_k = din("rwkv_r_k", [512])
    rwkv_ln_g = din("rwkv_ln_g", [512]); rwkv_ln_b = din("rwkv_ln_b", [512])
    gmlp_ln_g = din("gmlp_ln_g", [512]); gmlp_ln_b = din("gmlp_ln_b", [512])
    gmlp_ws = din("gmlp_ws", [8, 128, 128]); gmlp_bs = din("gmlp_bs", [8, 128])
    w_out_a = din("w_out_a", [512, D]); w_out_b = din("w_out_b", [512, D]); w_out = din("w_out", [D, D])
    router_w = din("router_w", [D, NE]); router_bias = din("router_bias", [NE])
    if True:
        exp_w1 = din("exp_w1", [NE, D, 256]); exp_w3 = din("exp_w3", [NE, D, 256]); exp_w2 = din("exp_w2", [NE, 256, D])
    shared_w1 = din("shared_w1", [D, 256]); shared_w3 = din("shared_w3", [D, 256]); shared_w2 = din("shared_w2", [256, D])
    normf_g = din("normf_g", [D])
    out = nc.dram_tensor("out", [T, D], F32, kind="ExternalOutput").ap()

    def dscr(name, shape, dt):
        k = "ExternalOutput" if dbg == name else "Internal"
        return Tl(nc.dram_tensor(name, shape, dt, kind=k).ap(), name)

    yaT_d = dscr("yaT_d", [512, T], BF16)
    x1_d = dscr("x1_d", [T, D], F32)
    xg_d = dscr("xg_d", [NSLOT, D], BF16)
    yg_d = dscr("yg_d", [NSLOT, D], BF16)

    with ExitStack() as gs:
        P = Prog(nc, gs)
        holder["P"] = P

        def ck(n, tl, ap=None):
            if dbg == f"ck{n}":
                o_ = nc.dram_tensor("o_ck", list((ap if ap is not None else tl[:]).shape), (ap if ap is not None else tl[:]).dtype, kind="ExternalOutput").ap()
                stq(o_, ap if ap is not None else tl[:], [tl], is_out=True)
                P.finish()
                raise StopBuild()
        LD = P.lanes(6, "ld")
        LP = P.lanes(4, "lp")
        LS = P.lanes(4, "lst")
        ldi = [0]; lpi = [0]; lsi = [0]

        def ld(out_ap, in_ap, w, r=()):
            l = LD[ldi[0] % len(LD)]; ldi[0] += 1
            return P.dma(P.sp, l, lambda h: h.dma_start(out=out_ap, in_=in_ap), r=r, w=w)

        def ldc(out_ap, in_ap, w, r=()):
            l = LP[lpi[0] % len(LP)]; lpi[0] += 1
            return P.dma(P.pool, l, lambda h: h.dma_start(out=out_ap, in_=in_ap), r=r, w=w)

        LSQ = {"sp": LS, "pool": P.lanes(3, "lsp"), "act": P.lanes(3, "lsa")}

        def stq(out_ap, in_ap, r, w=(), is_out=False, q=None):
            q = q or P.sp
            ll = LSQ[q.name]
            l = ll[lsi[0] % len(ll)]; lsi[0] += 1
            return P.dma(q, l, lambda h: h.dma_start(out=out_ap, in_=in_ap), r=r, w=w, is_out=is_out)

        pbanks = [P.ptile(f"pb{i}", [128, 512], F32) for i in range(6)]
        bbanks = [P.ptile(f"bb{i}", [128, 1024], BF16) for i in range(2)]
        pbi = [0]; bbi = [0]

        def bank():
            b = pbanks[pbi[0] % 6]; pbi[0] += 1
            return b

        def bbank():
            b = bbanks[bbi[0] % 2]; bbi[0] += 1
            return b

        def mm(o, lhsT, rhs, start, stop, r, w):
            return P.op(P.pe, lambda h: h.matmul(o, lhsT=lhsT, rhs=rhs, start=start, stop=stop), r=r, w=w)

        def tr(o, in_, ident, r, w):
            return P.op(P.pe, lambda h: h.transpose(out=o, in_=in_, identity=ident), r=r, w=w)

        def act(o, in_, func, r, w, **kw):
            return P.op(P.act, lambda h: h.activation(out=o, in_=in_, func=func, **kw), r=r, w=w)

        def tt(e, o, a, b, op, r, w):
            return P.op(e, lambda h: h.tensor_tensor(out=o, in0=a, in1=b, op=op), r=r, w=w)

        def ts(e, o, a, s1, s2, op0, op1, r, w):
            if s2 is None:
                return P.op(e, lambda h: h.tensor_scalar(out=o, in0=a, scalar1=s1, scalar2=None, op0=op0), r=r, w=w)
            return P.op(e, lambda h: h.tensor_scalar(out=o, in0=a, scalar1=s1, scalar2=s2, op0=op0, op1=op1), r=r, w=w)

        def stt(e, o, a, s, b, op0, op1, r, w, **kw):
            return P.op(e, lambda h: h.scalar_tensor_tensor(out=o, in0=a, scalar=s, in1=b, op0=op0, op1=op1, **kw), r=r, w=w)

        def cp(e, o, a, r, w):
            return P.op(e, lambda h: h.tensor_copy(out=o, in_=a), r=r, w=w)

        def ms(e, o, v, w, r=()):
            return P.op(e, lambda h: h.memset(o, v), r=r, w=w)

        identf = P.tile("identf", [128, 128], F32)
        identb = P.tile("identb", [128, 128], BF16)
        ms(P.pool, identf[:], 0.0, [identf])
        P.op(P.pool, lambda h: h.affine_select(out=identf[:], in_=identf[:], pattern=[[-1, 128]], compare_op=ALU.not_equal, fill=1.0, base=0, channel_multiplier=1), r=[identf], w=[identf])
        cp(P.dve, identb[:], identf[:], [identf], [identb])
        maskCM = P.tile("maskCM", [128, 512], F32)
        mAT = P.tile("mAT", [128, 128], F32)
        ms(P.pool, maskCM[:], 1.0, [maskCM])
        P.op(P.pool, lambda h: h.affine_select(out=maskCM[:, 0:128], in_=maskCM[:, 0:128], pattern=[[1, 128]], compare_op=ALU.is_gt, fill=0.0, base=0, channel_multiplier=-1), r=[maskCM], w=[maskCM])
        P.op(P.pool, lambda h: h.affine_select(out=maskCM[:, 128:256], in_=maskCM[:, 128:256], pattern=[[1, 128]], compare_op=ALU.is_ge, fill=0.0, base=0, channel_multiplier=-1), r=[maskCM], w=[maskCM])
        ms(P.pool, maskCM[0:64, 64:128], 0.0, [maskCM], [maskCM])
        ms(P.pool, maskCM[0:64, 192:256], 0.0, [maskCM], [maskCM])
        cp(P.dve, maskCM[:, 256:512], maskCM[:, 0:256], [maskCM], [maskCM])
        ms(P.pool, mAT[:], 1.0, [mAT])
        P.op(P.pool, lambda h: h.affine_select(out=mAT[:], in_=mAT[:], pattern=[[-1, 128]], compare_op=ALU.is_gt, fill=0.0, base=0, channel_multiplier=1), r=[mAT], w=[mAT])
        ms(P.pool, mAT[64:128, 0:64], 0.0, [mAT], [mAT])
        bd4 = P.tile("bd4", [128, 512], F32)
        bdb = P.tile("bdb", [128, 128], BF16)
        ms(P.pool, bd4[:], 0.0, [bd4])
        for q in range(4):
            ms(P.pool, bd4[0:64, q * 128:q * 128 + 64], 1.0, [bd4], [bd4])
            ms(P.pool, bd4[64:128, q * 128 + 64:q * 128 + 128], 1.0, [bd4], [bd4])
        cp(P.dve, bdb[:], bd4[:, 0:128], [bd4], [bdb])
        reset = P.tile("reset", [128, 512], F32)
        ms(P.pool, reset[:], 1.0, [reset])
        ms(P.pool, reset[:].rearrange("p (c t) -> p c t", t=64)[:, :, 0:1], 0.0, [reset], [reset])

        zt = P.tile("zt", [128, 1024], BF16)
        ms(P.pool, zt[:], 0.0, [zt])
        NZ = NSLOT // 128

        s1 = P.tile("s1", [128, 8], F32); sh1 = P.tile("sh1", [128, 8], F32)
        s2 = P.tile("s2", [128, 8], F32); sh2 = P.tile("sh2", [128, 8], F32)
        g1bc = P.tile("g1bc", [128, D], F32); g2bc = P.tile("g2bc", [128, D], F32)
        with ExitStack() as ph, nc.allow_non_contiguous_dma(reason="tiny per-channel vectors"):
            P.stack = ph
            ccol = P.tile("ccol", [128, 8], F32)
            ld(ccol[:], c[0, :].rearrange("(n p) -> p n", p=128), [ccol])
            scol2 = P.tile("scol2", [128, 8, 2], F32)
            act(scol2[:, :, 0], ccol[:], AF.Silu, [ccol], [scol2])
            act(scol2[:, :, 1], ccol[:], AF.Silu, [ccol], [scol2])
            sbc = P.tile("sbc", [128, 8, 128], F32)
            for kc in range(8):
                cp(P.dve, sbc[:, kc, :], scol2[:, kc, 0:1].to_broadcast([128, 128]), [scol2], [sbc])
            adabT = P.tile("adabT", [128, 48], F32)
            ld(adabT[:], ada_b.rearrange("(n p) -> p n", p=128), [adabT])
            n1g = P.tile("n1g", [128, 8], F32); n2g = P.tile("n2g", [128, 8], F32)
            ld(n1g[:], norm1_g.rearrange("(n p) -> p n", p=128), [n1g])
            ld(n2g[:], norm2_g.rearrange("(n p) -> p n", p=128), [n2g])
            modT = P.tile("modT", [128, 48], F32)
            awb = [P.tile(f"awb{i}", [128, 8, 1024], F32) for i in range(2)]
            adab_bc = P.tile("adab_bc", [128, 1024], F32)
            for vi in range(6):
                aw = awb[vi % 2]
                for kc in range(8):
                    ld(aw[:, kc, :], ada_w[kc * 128:(kc + 1) * 128, vi * 1024:(vi + 1) * 1024], [aw])
                if vi in (2, 5):
                    gbc = g1bc if vi == 2 else g2bc
                    ld(adab_bc[:], ada_b[vi * 1024:(vi + 1) * 1024].partition_broadcast(128), [adab_bc])
                    for hf in range(2):
                        pb = bank()
                        for kc in range(8):
                            mm(pb[:, :], sbc[:, kc, :], aw[:, kc, hf * 512:(hf + 1) * 512], kc == 0, kc == 7, [sbc, aw], [pb])
                        tt(P.dve, gbc[:, hf * 512:(hf + 1) * 512], pb[:, :], adab_bc[:, hf * 512:(hf + 1) * 512], ALU.add, [pb, adab_bc], [gbc])
                else:
                    pb = bank()
                    for oc in range(8):
                        for kc in range(8):
                            mm(pb[:, oc * 2:oc * 2 + 2], aw[:, kc, oc * 128:(oc + 1) * 128], scol2[:, kc, :], kc == 0, kc == 7, [aw, scol2], [pb])
                    tt(P.dve, modT[:, vi * 8:(vi + 1) * 8], pb[:, 0:16].rearrange("p (o t) -> p o t", t=2)[:, :, 0], adabT[:, vi * 8:(vi + 1) * 8], ALU.add, [pb, adabT], [modT])
            stt(P.dve, s1[:], modT[:, 8:16], 1.0, n1g[:], ALU.add, ALU.mult, [modT, n1g], [s1])
            cp(P.dve, sh1[:], modT[:, 0:8], [modT], [sh1])
            stt(P.dve, s2[:], modT[:, 32:40], 1.0, n2g[:], ALU.add, ALU.mult, [modT, n2g], [s2])
            cp(P.dve, sh2[:], modT[:, 24:32], [modT], [sh2])
            P.barrier()
            if dbg == "p0":
                o0 = nc.dram_tensor("o_p0", [128, 32], F32, kind="ExternalOutput").ap()
                o1 = nc.dram_tensor("o_g", [128, 2048], F32, kind="ExternalOutput").ap()
                stq(o0[:, 0:8], s1[:], [s1], is_out=True); stq(o0[:, 8:16], sh1[:], [sh1], is_out=True)
                stq(o0[:, 16:24], s2[:], [s2], is_out=True); stq(o0[:, 24:32], sh2[:], [sh2], is_out=True)
                stq(o1[:, 0:1024], g1bc[:], [g1bc], is_out=True); stq(o1[:, 1024:2048], g2bc[:], [g2bc], is_out=True)
                P.finish()
                return nc, P
        P.stack = gs

        def norm_T(xts, hT, src, sidx, sc, shf, tmp):
            t0 = sidx * ST
            ssq, rstd, xn, junk = tmp
            for ti in range(4):
                xt = xts[ti % 2]
                ld(xt[:], src[t0 + ti * 128:t0 + (ti + 1) * 128, :], [xt])
                act(xn[:, ti, :], xt[:], AF.Square, [xt], [xn, ssq], accum_out=ssq[:, ti:ti + 1])
                ts(P.dve, rstd[:, ti:ti + 1], ssq[:, ti:ti + 1], 1.0 / D, 1e-6, ALU.mult, ALU.add, [ssq], [rstd])
                act(rstd[:, ti:ti + 1], rstd[:, ti:ti + 1], AF.Sqrt, [rstd], [rstd])
                P.op(P.dve, lambda h: h.reciprocal(out=rstd[:, ti:ti + 1], in_=rstd[:, ti:ti + 1]), r=[rstd], w=[rstd])
                act(xn[:, ti, :], xt[:], AF.Identity, [xt, rstd], [xn], scale=rstd[:, ti:ti + 1])
            if dbg == "a1n1":
                o0 = nc.dram_tensor("o_xn", [128, 4, D], BF16, kind="ExternalOutput").ap()
                stq(o0, xn[:], [xn], is_out=True)
                o1 = nc.dram_tensor("o_rstd", [128, 4], F32, kind="ExternalOutput").ap()
                stq(o1, rstd[:], [rstd], is_out=True)
                P.finish()
                return
            for dc in range(8):
                bb = bbank()
                for ti in range(4):
                    tr(bb[:, ti * 128:(ti + 1) * 128], xn[:, ti, dc * 128:(dc + 1) * 128], identb[:], [xn, identb], [bb])
                act(hT[:, dc, :], bb[:, 0:512], AF.Identity, [bb, sc, shf], [hT], scale=sc[:, dc:dc + 1], bias=shf[:, dc:dc + 1])

        with ExitStack() as ph, nc.allow_non_contiguous_dma(reason="tiny per-channel vectors"):
            P.stack = ph
            winr = P.tile("winr", [128, 8, RW], BF16)
            for dc in range(8):
                ldc(winr[:, dc, :], w_in[dc * 128:(dc + 1) * 128, 0:RW], [winr])
            Wlw = P.tile("Wlw", [128, 512], BF16); Wla = P.tile("Wla", [128, 512], BF16)
            gup = P.tile("gup", [128, 512], BF16)
            ms(P.dve, Wlw[:], 0.0, [Wlw]); ms(P.dve, Wla[:], 0.0, [Wla])
            ldc(Wlw[0:64, :], rwkv_w_up, [Wlw]); ldc(Wla[64:128, :], rwkv_a_up, [Wla]); ldc(gup[:], rwkv_g_up, [gup])

            def colvec(name, src, n):
                t = P.tile(name, [128, n], F32)
                ld(t[:], src.rearrange("(n p) -> p n", p=128), [t])
                return t
            w0 = colvec("w0", rwkv_w0, 4); a0 = colvec("a0", rwkv_a0, 4); kkv = colvec("kkv", rwkv_k_k, 4)
            kav = colvec("kav", rwkv_k_a, 4); rkv = colvec("rkv", rwkv_r_k, 4)
            lng = colvec("lng", rwkv_ln_g, 4); lnb = colvec("lnb", rwkv_ln_b, 4)
            mu = colvec("mu", tshift_mu, 14)
            omu = P.tile("omu", [128, 14], F32); omka = P.tile("omka", [128, 4], F32)
            ts(P.dve, omu[:], mu[:], -1.0, 1.0, ALU.mult, ALU.add, [mu], [omu])
            ts(P.dve, omka[:], kav[:], -1.0, 1.0, ALU.mult, ALU.add, [kav], [omka])

            if dbg == "a1w":
                o1 = nc.dram_tensor("o_omu", [128, 14], F32, kind="ExternalOutput").ap()
                stq(o1, omu[:], [omu], is_out=True)
                o2 = nc.dram_tensor("o_wla", [128, 512], BF16, kind="ExternalOutput").ap()
                stq(o2, Wla[:], [Wla], is_out=True)
                o3 = nc.dram_tensor("o_winr", [128, 8, RW], BF16, kind="ExternalOutput").ap()
                stq(o3, winr[:], [winr], is_out=True)
                P.finish()
                return nc, P
            xts = [P.tile(f"xt{i}", [128, D], F32) for i in range(2)]
            hT = P.tile("hT", [128, 8, 512], BF16)
            ssq = P.tile("ssq", [128, 4], F32); rstd = P.tile("rstd", [128, 4], F32)
            xn = P.tile("xn", [128, 4, D], BF16); junk = None
            ntmp = (ssq, rstd, xn, junk)
            sh = [P.tile(f"shq{q}", [128, 512], F32) for q in range(14)]
            pmus = [P.tile("pmu0", [128, 513], F32)] * 2
            carry = P.tile("carry", [128, 14], F32)
            ms(P.pool, carry[:], 0.0, [carry])
            lo_in = P.tile("lo_in", [128, 512], BF16); sxg = P.tile("sxg", [128, 512], BF16)
            gT = [P.tile(f"gT{j}", [128, 512], BF16) for j in range(4)]
            bonT = [P.tile(f"bonT{j}", [128, 512], BF16) for j in range(4)]
            RA = P.tile("RA", [128, 4, 4, 256], BF16)
            BT = [P.tile(f"BT{j}", [128, 512], BF16) for j in range(4)]
            KT = [P.tile(f"KT{j}", [128, 512], BF16) for j in range(4)]
            gC = P.tile("gC", [128, 4, 8], F32)
            TOKB2 = P.tile("TOKB2", [128, 4, 512], BF16)
            TOKK2 = P.tile("TOKK2", [128, 4, 512], BF16)
            TOKV = P.tile("TOKV", [128, 4, 512], BF16)
            sgw = P.tile("sgw", [128, 512], F32); asig = P.tile("asig", [128, 512], F32)
            kksq = P.tile("kksq", [128, 512], BF16); rn = P.tile("rn", [128, 512], F32)
            kkn = P.tile("kkn", [128, 512], F32); ff = P.tile("ff", [128, 512], F32)
            kp = P.tile("kp", [128, 512], F32); bp = P.tile("bp", [128, 512], F32)
            cum = P.tile("cum", [128, 512], F32); cme = ff
            cdf = rn
            e1 = sgw; e2 = P.tile("e2", [128, 512], F32)
            e3 = e1; e4 = e2
            rk = P.tile("rk", [128, 512], BF16)
            tb3 = P.tile("tb3", [128, 3, 512], BF16)
            class _View:
                def __init__(self, ap, b):
                    self.ap = ap; self.b = b

                def __getitem__(self, k):
                    return self.ap[k]
            CM = [P.tile("CM0", [128, 8, 512], BF16), _View(xn[:].rearrange("p a (b c) -> p (a b) c", c=512), xn.b)]
            Xs = [P.tile(f"Xs{i}", [128, 8, 128], BF16) for i in range(2)]
            XTs = [P.tile(f"XTs{i}", [128, 8, 128], BF16) for i in range(2)]
            Ps = [P.tile(f"Ps{i}", [128, 8, 128], BF16) for i in range(2)]
            TT = [P.tile(f"TT{i}", [128, 8, 128], BF16) for i in range(2)]
            Hf = [P.tile(f"Hf{i}", [128, 512], F32) for i in range(2)]
            Hb = [P.tile(f"Hb{i}", [128, 512], BF16) for i in range(2)]
            ms(P.pool, Hf[0][:], 0.0, [Hf[0]]); ms(P.pool, Hb[0][:], 0.0, [Hb[0]])
            X1sb = P.tile("X1sb", [128, 512], BF16); Usb = P.tile("Usb", [128, 512], BF16)
            ms(P.pool, X1sb[:], 0.0, [X1sb]); ms(P.pool, Usb[:], 0.0, [Usb])
            htmp = kp
            Ysb = P.tile("Ysb", [128, 512], F32)
            gmean = P.tile("gmean", [128, 8], F32); gvar = P.tile("gvar", [128, 8], F32)
            yc = e2; ysq = e1
            ynb = P.tile("ynb", [128, 4, 512], BF16)
            yaT = P.tile("yaT", [128, 4, 512], BF16)
            yt1 = kkn
            hcur = [0]
            chunk_g = 0

            for si in range(nst):
                t0 = si * ST
                norm_T(xts, hT, x, si, s1, sh1, ntmp)
                for zi in range(si * (NZ // NST), (si + 1) * (NZ // NST)):
                    stq(xg_d[zi * 128:(zi + 1) * 128, :], zt[:], [zt], [xg_d])
                if dbg == "a1n1":
                    return nc, P
                if dbg == "a1n":
                    o0 = nc.dram_tensor("o_hT", [128, 8, 512], BF16, kind="ExternalOutput").ap()
                    stq(o0, hT[:], [hT], is_out=True)
                    o1 = nc.dram_tensor("o_omu", [128, 14], F32, kind="ExternalOutput").ap()
                    stq(o1, omu[:], [omu], is_out=True)
                    P.finish()
                    return nc, P
                for q in range(14):
                    pb = bank()
                    for dc in range(8):
                        mm(pb[:, :], winr[:, dc, q * 128:(q + 1) * 128], hT[:, dc, :], dc == 0, dc == 7, [winr, hT], [pb])
                    pmu = pmus[q % 2]
                    act(sh[q][:], pb[:, :], AF.Identity, [pb, omu], [sh[q]], scale=omu[:, q:q + 1])
                    act(pmu[:, 0:1], carry[:, q:q + 1], AF.Copy, [carry], [pmu])
                    ts(P.dve, pmu[:, 1:513], pb[:, :], mu[:, q:q + 1], None, ALU.mult, None, [pb, mu], [pmu])
                    tt(P.dve, sh[q][:], sh[q][:], pmu[:, 0:512], ALU.add, [sh[q], pmu], [sh[q]])
                    act(carry[:, q:q + 1], pmu[:, 512:513], AF.Copy, [pmu], [carry])
                if dbg == "a1a":
                    o0 = nc.dram_tensor("o_sh", [14, 128, 512], F32, kind="ExternalOutput").ap()
                    for q in range(14):
                        stq(o0[q], sh[q][:], [sh[q]], is_out=True)
                    P.finish()
                    return nc, P
                act(lo_in[0:64, :], sh[12][0:64, :], AF.Tanh, [sh[12]], [lo_in])
                act(lo_in[64:128, :], sh[12][64:128, :], AF.Copy, [sh[12]], [lo_in])
                act(sxg[:], sh[13][:], AF.Sigmoid, [sh[13]], [sxg])
                for j in range(4):
                    r_, k_, v_ = sh[j], sh[4 + j], sh[8 + j]
                    js = slice(j * 128, (j + 1) * 128)
                    pb = bank()
                    mm(pb[:, :], Wlw[:, js], lo_in[:], True, True, [Wlw, lo_in], [pb])
                    act(sgw[:], pb[:, :], AF.Sigmoid, [pb, w0], [sgw], bias=w0[:, j:j + 1])
                    pb = bank()
                    mm(pb[:, :], Wla[:, js], lo_in[:], True, True, [Wla, lo_in], [pb])
                    act(asig[:], pb[:, :], AF.Sigmoid, [pb, a0], [asig], bias=a0[:, j:j + 1])
                    pb = bank()
                    mm(pb[:, :], gup[:, js], sxg[:], True, True, [gup, sxg], [pb])
                    act(gT[j][:], pb[:, :], AF.Copy, [pb], [gT[j]])
                    ck(1, asig)
                    act(kksq[:], k_[:], AF.Square, [k_, kkv], [kksq], scale=kkv[:, j:j + 1])
                    pb = bank()
                    mm(pb[:, :], bdb[:], kksq[:], True, True, [bdb, kksq], [pb])
                    act(rn[:], pb[:, :], AF.Sqrt, [pb], [rn], bias=1e-24)
                    P.op(P.dve, lambda h: h.reciprocal(out=rn[:], in_=rn[:]), r=[rn], w=[rn])
                    stt(P.dve, kkn[:], k_[:], kkv[:, j:j + 1], rn[:], ALU.mult, ALU.mult, [k_, kkv, rn], [kkn])
                    ck(2, kkn)
                    ts(P.dve, ff[:], asig[:], kav[:, j:j + 1], omka[:, j:j + 1], ALU.mult, ALU.add, [asig, kav, omka], [ff])
                    tt(P.dve, kp[:], k_[:], ff[:], ALU.mult, [k_, ff], [kp])
                    tt(P.dve, bp[:], kkn[:], asig[:], ALU.mult, [kkn, asig], [bp])
                    P.op(P.dve, lambda h: h.tensor_tensor_scan(out=cum[:], data0=reset[:], data1=sgw[:], initial=0.0, op0=ALU.mult, op1=ALU.add), r=[reset, sgw], w=[cum])
                    tt(P.dve, cme[:], cum[:], sgw[:], ALU.subtract, [cum, sgw], [cme])
                    ck(3, cum)
                    cum3 = cum[:].rearrange("p (c t) -> p c t", t=64)
                    tt(P.dve, cdf[:].rearrange("p (c t) -> p c t", t=64), cum3[:, :, 63:64].to_broadcast([128, 8, 64]), cum3, ALU.subtract, [cum], [cdf])
                    ck(4, cdf)
                    act(e1[:], cum[:], AF.Exp, [cum], [e1], scale=SDEC)
                    act(e2[:], cme[:], AF.Exp, [cme], [e2], scale=SDEC)
                    act(gC[:, j, :], cum3[:, :, 63], AF.Exp, [cum], [gC], scale=SDEC)
                    tt(P.dve, RA[:, j, :, 128:256], r_[:].rearrange("p (a t) -> p a t", t=128), e1[:].rearrange("p (a t) -> p a t", t=128), ALU.mult, [r_, e1], [RA])
                    stt(P.dve, RA[:, j, :, 0:128], kkn[:].rearrange("p (a t) -> p a t", t=128), -1.0, e2[:].rearrange("p (a t) -> p a t", t=128), ALU.mult, ALU.mult, [kkn, e2], [RA])
                    ck(5, RA)
                    act(e3[:], cum[:], AF.Exp, [cum], [e3], scale=-SDEC)
                    act(e4[:], cdf[:], AF.Exp, [cdf], [e4], scale=SDEC)
                    tt(P.dve, BT[j][:], bp[:], e3[:], ALU.mult, [bp, e3], [BT[j]])
                    tt(P.dve, KT[j][:], kp[:], e3[:], ALU.mult, [kp, e3], [KT[j]])
                    tt(P.dve, tb3[:, 0, :], bp[:], e4[:], ALU.mult, [bp, e4], [tb3])
                    tt(P.dve, tb3[:, 1, :], kp[:], e4[:], ALU.mult, [kp, e4], [tb3])
                    act(tb3[:, 2, :], v_[:], AF.Copy, [v_], [tb3])
                    stt(P.dve, rk[:], r_[:], rkv[:, j:j + 1], kp[:], ALU.mult, ALU.mult, [r_, rkv, kp], [rk])
                    pb = bank()
                    mm(pb[:, :], bdb[:], rk[:], True, True, [bdb, rk], [pb])
                    tt(P.dve, bonT[j][:], pb[:, :], v_[:], ALU.mult, [pb, v_], [bonT[j]])
                    ck(6, bonT[j])
                    for ti in range(4):
                        bb = bbank()
                        for kind in range(3):
                            tr(bb[:, kind * 128:(kind + 1) * 128], tb3[:, kind, ti * 128:(ti + 1) * 128], identb[:], [tb3, identb], [bb])
                        cp(P.dve, TOKB2[:, ti, js], bb[:, 0:128], [bb], [TOKB2])
                        cp(P.dve, TOKK2[:, ti, js], bb[:, 128:256], [bb], [TOKK2])
                        cp(P.dve, TOKV[:, ti, js], bb[:, 256:384], [bb], [TOKV])
                    ck(7, TOKV, TOKV[:, 0, 0:128])

                if dbg == "a1b":
                    o0 = nc.dram_tensor("o_ra", [128, 4, 4, 256], BF16, kind="ExternalOutput").ap()
                    o1 = nc.dram_tensor("o_tokv", [128, 4, 512], BF16, kind="ExternalOutput").ap()
                    o2 = nc.dram_tensor("o_gc", [128, 4, 8], F32, kind="ExternalOutput").ap()
                    stq(o0, RA[:], [RA], is_out=True); stq(o1, TOKV[:], [TOKV], is_out=True); stq(o2, gC[:], [gC], is_out=True)
                    P.finish()
                    return nc, P
                def gen_D(ti):
                    cm = CM[ti % 2]; tts = TT[ti % 2]
                    tsl = slice(ti * 128, (ti + 1) * 128)
                    for hb4 in range(2):
                        pbn = bank()
                        for hh in range(4):
                            h_ = 2 * hh + hb4; j = hh; ps_ = slice(hb4 * 64, hb4 * 64 + 64)
                            mm(pbn[:, hh * 128:(hh + 1) * 128], RA[ps_, j, ti, 0:128], BT[j][ps_, tsl], True, True, [RA, BT[j]], [pbn])
                        tt(P.dve, XTs[0][:, hb4 * 4:(hb4 + 1) * 4, :], pbn[:, :].rearrange("p (a t) -> p a t", t=128), mAT[:, :].unsqueeze(1).to_broadcast([128, 4, 128]), ALU.mult, [pbn, mAT], [XTs[0]])
                    for h_ in range(8):
                        j = h_ // 2; ps_ = slice((h_ % 2) * 64, (h_ % 2) * 64 + 64)
                        pb = bank()
                        mm(pb[:, 0:256], KT[j][ps_, tsl], RA[ps_, j, ti, :], True, True, [KT[j], RA], [pb])
                        mm(pb[:, 256:512], BT[j][ps_, tsl], RA[ps_, j, ti, :], True, True, [BT[j], RA], [pb])
                        tt(P.dve, cm[:, sl(h_), :], pb[:, :], maskCM[:], ALU.mult, [pb, maskCM], [cm])
                        if h_ % 2 == 1:
                            yield
                    ck(8, cm, cm[:, 0, :])
                    cp(P.dve, Xs[0][:], cm[:, :, 256:384], [cm], [Xs[0]])
                    tt(P.dve, Ps[0][:], cm[:, :, 256:384], identf[:, :].unsqueeze(1).to_broadcast([128, 8, 128]), ALU.add, [cm, identf], [Ps[0]])
                    cur = 0
                    for it in range(1, 6):
                        nxt = 1 - cur
                        last = (it == 5)
                        for hb4 in range(2):
                            hs4 = slice(hb4 * 4, hb4 * 4 + 4)
                            pbx = bank() if not last else None
                            pbt = bank()
                            for hh in range(4):
                                h_ = hb4 * 4 + hh
                                cs = slice(hh * 128, (hh + 1) * 128)
                                if not last:
                                    mm(pbx[:, cs], XTs[cur][:, h_, :], Xs[cur][:, h_, :], True, True, [XTs[cur], Xs[cur]], [pbx])
                                mm(pbt[:, cs], Xs[cur][:, h_, :], XTs[cur][:, h_, :], True, True, [XTs[cur], Xs[cur]], [pbt])
                            if not last:
                                act(Xs[nxt][:, hs4, :], pbx[:, :].rearrange("p (a t) -> p a t", t=128), AF.Copy, [pbx], [Xs[nxt]])
                            cp(P.dve, XTs[nxt][:, hs4, :], pbt[:, :].rearrange("p (a t) -> p a t", t=128), [pbt], [XTs[nxt]])
                            pbp = bank()
                            for hh in range(4):
                                h_ = hb4 * 4 + hh
                                cs = slice(hh * 128, (hh + 1) * 128)
                                mm(pbp[:, cs], identb[:], Ps[cur][:, h_, :], True, False, [identb, Ps[cur]], [pbp])
                                mm(pbp[:, cs], XTs[nxt][:, h_, :], Ps[cur][:, h_, :], False, True, [XTs[nxt], Ps[cur]], [pbp])
                            dst = tts if last else Ps[nxt]
                            act(dst[:, hs4, :], pbp[:, :].rearrange("p (a t) -> p a t", t=128), AF.Copy, [pbp], [dst])
                            yield
                        cur = nxt

                def gen_S(ti):
                    cm = CM[ti % 2]; tts = TT[ti % 2]
                    for p in range(2):
                        rows = slice(64 * p, 64 * p + 64)
                        cc = slice(64 * p, 64 * p + 64)
                        hold_f, hold_b = Hf[hcur[0]], Hb[hcur[0]]
                        hnew_f, hnew_b = Hf[1 - hcur[0]], Hb[1 - hcur[0]]
                        ch = ti * 2 + p
                        tt(P.dve, hnew_f[:].rearrange("p (j v) -> p j v", v=128), hold_f[:].rearrange("p (j v) -> p j v", v=128), gC[:, :, ch:ch + 1].to_broadcast([128, 4, 128]), ALU.mult, [hold_f, gC], [hnew_f])
                        ps1 = bank()
                        for h_ in range(8):
                            j = h_ // 2; hb = h_ % 2
                            o = ps1[rows, h_ * 64:(h_ + 1) * 64]
                            mm(o, RA[:, j, ti, 64 * p:64 * p + 64], hold_b[:, j * 128 + hb * 64:j * 128 + hb * 64 + 64], True, False, [RA, hold_b], [ps1])
                            mm(o, cm[:, sl(h_), 64 * p:64 * p + 64], TOKV[:, ti, h_ * 64:(h_ + 1) * 64], False, True, [cm, TOKV], [ps1])
                        act(X1sb[rows, :], ps1[rows, :], AF.Copy, [ps1], [X1sb])
                        yield
                        ps2 = bank()
                        for h_ in range(8):
                            mm(ps2[rows, h_ * 64:(h_ + 1) * 64], tts[:, sl(h_), 64 * p:64 * p + 64], X1sb[:, h_ * 64:(h_ + 1) * 64], True, True, [tts, X1sb], [ps2])
                        cp(P.dve, Usb[rows, :], ps2[rows, :], [ps2], [Usb])
                        yield
                        ps4 = bank()
                        for h_ in range(8):
                            j = h_ // 2; hb = h_ % 2
                            o = ps4[rows, h_ * 64:(h_ + 1) * 64]
                            mm(o, RA[:, j, ti, 128 + 64 * p:128 + 64 * p + 64], hold_b[:, j * 128 + hb * 64:j * 128 + hb * 64 + 64], True, False, [RA, hold_b], [ps4])
                            mm(o, cm[:, sl(h_), 384 + 64 * p:384 + 64 * p + 64], Usb[:, h_ * 64:(h_ + 1) * 64], False, False, [cm, Usb], [ps4])
                            mm(o, cm[:, sl(h_), 128 + 64 * p:128 + 64 * p + 64], TOKV[:, ti, h_ * 64:(h_ + 1) * 64], False, True, [cm, TOKV], [ps4])
                        act(Ysb[rows, :], ps4[rows, :], AF.Copy, [ps4], [Ysb])
                        yield
                        ps3 = bank()
                        for j in range(4):
                            js = slice(j * 128, (j + 1) * 128)
                            mm(ps3[:, js], TOKB2[rows, ti, js], Usb[rows, js], True, False, [TOKB2, Usb], [ps3])
                            mm(ps3[:, js], TOKK2[rows, ti, js], TOKV[rows, ti, js], False, True, [TOKK2, TOKV], [ps3])
                        tt(P.dve, htmp[:], ps3[:, :], bd4[:], ALU.mult, [ps3, bd4], [htmp])
                        tt(P.dve, hnew_f[:], hnew_f[:], htmp[:], ALU.add, [hnew_f, htmp], [hnew_f])
                        act(hnew_b[:], hnew_f[:], AF.Copy, [hnew_f], [hnew_b])
                        hcur[0] = 1 - hcur[0]
                        yield
                    y3 = Ysb[:, :].rearrange("p (h v) -> p h v", v=64)
                    P.op(P.dve, lambda h: h.tensor_reduce(out=gmean[:], in_=y3, axis=AX.X, op=ALU.add), r=[Ysb], w=[gmean])
                    ts(P.dve, gmean[:], gmean[:], 1.0 / 64, None, ALU.mult, None, [gmean], [gmean])
                    tt(P.dve, yc[:].rearrange("p (h v) -> p h v", v=64), y3, gmean[:, :].unsqueeze(2).to_broadcast([128, 8, 64]), ALU.subtract, [Ysb, gmean], [yc])
                    tt(P.dve, ysq[:], yc[:], yc[:], ALU.mult, [yc], [ysq])
                    P.op(P.dve, lambda h: h.tensor_reduce(out=gvar[:], in_=ysq[:].rearrange("p (h v) -> p h v", v=64), axis=AX.X, op=ALU.add), r=[ysq], w=[gvar])
                    ts(P.dve, gvar[:], gvar[:], 1.0 / 64, 64e-5, ALU.mult, ALU.add, [gvar], [gvar])
                    act(gvar[:], gvar[:], AF.Sqrt, [gvar], [gvar])
                    P.op(P.dve, lambda h: h.reciprocal(out=gvar[:], in_=gvar[:]), r=[gvar], w=[gvar])
                    tt(P.dve, ynb[:, ti, :].rearrange("p (h v) -> p h v", v=64), yc[:].rearrange("p (h v) -> p h v", v=64), gvar[:, :].unsqueeze(2).to_broadcast([128, 8, 64]), ALU.mult, [yc, gvar], [ynb])

                def drive(*gens):
                    gens = [g for g in gens if g is not None]
                    while gens:
                        for g in list(gens):
                            try:
                                next(g)
                            except StopIteration:
                                gens.remove(g)
                drive(gen_D(0))
                for ti in range(4):
                    drive(gen_S(ti), gen_D(ti + 1) if ti < 3 else None)
                for j in range(4):
                    bb = bbank()
                    for ti in range(4):
                        tr(bb[:, ti * 128:(ti + 1) * 128], ynb[:, ti, j * 128:(j + 1) * 128], identb[:], [ynb, identb], [bb])
                    act(yt1[:], bb[:, 0:512], AF.Identity, [bb, lng, lnb], [yt1], scale=lng[:, j:j + 1], bias=lnb[:, j:j + 1])
                    tt(P.dve, yt1[:], yt1[:], bonT[j][:], ALU.add, [yt1, bonT[j]], [yt1])
                    tt(P.dve, yaT[:, j, :], yt1[:], gT[j][:], ALU.mult, [yt1, gT[j]], [yaT])
                    stq(yaT_d[j * 128:(j + 1) * 128, t0:t0 + ST], yaT[:, j, :], [yaT], [yaT_d], is_out=(dbg == "yaT_d"), q=P.pool)
            P.barrier()
        P.stack = gs
        if dbg == "yaT_d":
            P.finish()
            return nc, P

        with ExitStack() as ph, nc.allow_non_contiguous_dma(reason="tiny per-channel vectors"):
            P.stack = ph
            C0 = RW
            winu = P.tile("winu", [128, 8, 512], BF16); winv = P.tile("winv", [128, 8, 512], BF16)
            wing = P.tile("wing", [128, 8, 2048], BF16)
            woa = P.tile("woa", [128, 4, D], BF16); wob = P.tile("wob", [128, 4, D], BF16); wo = P.tile("wo", [128, 8, D], BF16)
            for dc in range(8):
                ldc(winu[:, dc, :], w_in[dc * 128:(dc + 1) * 128, C0:C0 + 512], [winu])
                ldc(winv[:, dc, :], w_in[dc * 128:(dc + 1) * 128, C0 + 512:C0 + 1024], [winv])
                ldc(wing[:, dc, 0:1024], w_in[dc * 128:(dc + 1) * 128, C0 + 1024:C0 + 2048], [wing])
                ldc(wing[:, dc, 1024:2048], w_in[dc * 128:(dc + 1) * 128, C0 + 2048:C0 + 3072], [wing])
                ldc(wo[:, dc, :], w_out[dc * 128:(dc + 1) * 128, :], [wo])
            for q in range(4):
                ldc(woa[:, q, :], w_out_a[q * 128:(q + 1) * 128, :], [woa])
                ldc(wob[:, q, :], w_out_b[q * 128:(q + 1) * 128, :], [wob])
            mU = P.tile("mU", [128, 128], F32)
            ms(P.pool, mU[:], 1.0, [mU])
            P.op(P.pool, lambda h: h.affine_select(out=mU[:], in_=mU[:], pattern=[[1, 128]], compare_op=ALU.is_ge, fill=0.0, base=0, channel_multiplier=-1), r=[mU], w=[mU])
            wsf = P.tile("wsf", [128, 8, 128], F32)
            for g_ in range(8):
                ld(wsf[:, g_, :], gmlp_ws[g_], [wsf])
            wsmT = P.tile("wsmT", [128, 8, 128], BF16)
            for g4 in range(2):
                pb = bank()
                for gg in range(4):
                    tr(pb[:, gg * 128:(gg + 1) * 128], wsf[:, g4 * 4 + gg, :], identf[:], [wsf, identf], [pb])
                tt(P.dve, wsmT[:, g4 * 4:(g4 + 1) * 4, :], pb[:, :].rearrange("p (a t) -> p a t", t=128), mU[:, :].unsqueeze(1).to_broadcast([128, 4, 128]), ALU.mult, [pb, mU], [wsmT])
            bsT = P.tile("bsT", [128, 4, 128], F32)
            for g_ in range(8):
                ld(bsT[(g_ % 2) * 64:(g_ % 2) * 64 + 64, g_ // 2, :], gmlp_bs[g_].partition_broadcast(64), [bsT])
            lngbc = P.tile("lngbc", [128, 512], F32); lnbbc = P.tile("lnbbc", [128, 512], F32)
            ld(lngbc[:], gmlp_ln_g.partition_broadcast(128), [lngbc]); ld(lnbbc[:], gmlp_ln_b.partition_broadcast(128), [lnbbc])

            xts = [P.tile(f"bxt{i}", [128, D], F32) for i in range(2)]
            hT = P.tile("bhT", [128, 8, 512], BF16)
            ssq = P.tile("bssq", [128, 4], F32); rstd = P.tile("brstd", [128, 4], F32)
            xn = P.tile("bxn", [128, 4, D], BF16)
            ntmp = (ssq, rstd, xn, None)
            uT = P.tile("uT", [128, 4, 512], BF16)
            vg = P.tile("vg", [128, 512], F32); vc = P.tile("vc", [128, 512], F32)
            vst = P.tile("vst", [128, 4], F32)
            vln = P.tile("vln", [128, 4, 512], BF16)
            ybT = P.tile("ybT", [128, 4, 512], BF16)
            gts = P.tile("gts", [128, 16, 512], BF16)
            yaTs = P.tile("yaTs", [128, 4, 512], BF16)
            mgT = P.tile("mgT", [128, 8, 512], BF16)
            t1 = P.tile("t1", [128, 512], F32); t2_ = P.tile("t2_", [128, 512], F32)
            xo = [P.tile(f"xo{i}", [128, D], F32) for i in range(2)]
            for si in range(nst):
                t0 = si * ST
                norm_T(xts, hT, x, si, s1, sh1, ntmp)
                for q in range(4):
                    ld(yaTs[:, q, :], yaT_d[q * 128:(q + 1) * 128, t0:t0 + ST], [yaTs], r=[yaT_d])
                for q in range(4):
                    pb = bank()
                    for dc in range(8):
                        mm(pb[:, :], winu[:, dc, q * 128:(q + 1) * 128], hT[:, dc, :], dc == 0, dc == 7, [winu, hT], [pb])
                    act(uT[:, q, :], pb[:, :], AF.Gelu, [pb], [uT])
                for ti in range(4):
                    pb = bank()
                    for dc in range(8):
                        mm(pb[:, :], hT[:, dc, ti * 128:(ti + 1) * 128], winv[:, dc, :], dc == 0, dc == 7, [winv, hT], [pb])
                    act(vg[:], pb[:, :], AF.Gelu, [pb], [vg, vst], accum_out=vst[:, 0:1])
                    ts(P.dve, vst[:, 1:2], vst[:, 0:1], 1.0 / 512, None, ALU.mult, None, [vst], [vst])
                    ts(P.dve, vc[:], vg[:], vst[:, 1:2], None, ALU.subtract, None, [vg, vst], [vc])
                    act(vg[:], vc[:], AF.Square, [vc], [vg, vst], accum_out=vst[:, 2:3])
                    ts(P.dve, vst[:, 3:4], vst[:, 2:3], 1.0 / 512, 1e-5, ALU.mult, ALU.add, [vst], [vst])
                    act(vst[:, 3:4], vst[:, 3:4], AF.Sqrt, [vst], [vst])
                    P.op(P.dve, lambda h: h.reciprocal(out=vst[:, 3:4], in_=vst[:, 3:4]), r=[vst], w=[vst])
                    stt(P.dve, vc[:], vc[:], vst[:, 3:4], lngbc[:], ALU.mult, ALU.mult, [vc, vst, lngbc], [vc])
                    tt(P.dve, vln[:, ti, :], vc[:], lnbbc[:], ALU.add, [vc, lnbbc], [vln])
                for q in range(4):
                    pb = bank()
                    for ti in range(4):
                        for gg in range(2):
                            g_ = 2 * q + gg
                            mm(pb[gg * 64:(gg + 1) * 64, ti * 128:(ti + 1) * 128], vln[:, ti, g_ * 64:(g_ + 1) * 64], wsmT[:, g_, :], True, True, [vln, wsmT], [pb])
                    tt(P.dve, t1[:].rearrange("p (a t) -> p a t", t=128), pb[:, :].rearrange("p (a t) -> p a t", t=128), bsT[:, q, :].unsqueeze(1).to_broadcast([128, 4, 128]), ALU.add, [pb, bsT], [t1])
                    tt(P.dve, ybT[:, q, :], t1[:], uT[:, q, :], ALU.mult, [t1, uT], [ybT])
                for q in range(16):
                    pb = bank()
                    for dc in range(8):
                        mm(pb[:, :], wing[:, dc, q * 128:(q + 1) * 128], hT[:, dc, :], dc == 0, dc == 7, [wing, hT], [pb])
                    act(gts[:, q, :], pb[:, :], AF.Sigmoid, [pb], [gts])
                for m in range(8):
                    pa = bank()
                    for q in range(4):
                        mm(pa[:, :], woa[:, q, m * 128:(m + 1) * 128], yaTs[:, q, :], q == 0, q == 3, [woa, yaTs], [pa])
                    pb = bank()
                    for q in range(4):
                        mm(pb[:, :], wob[:, q, m * 128:(m + 1) * 128], ybT[:, q, :], q == 0, q == 3, [wob, ybT], [pb])
                    tt(P.dve, t1[:], pa[:, :], gts[:, m, :], ALU.mult, [pa, gts], [t1])
                    tt(P.dve, t2_[:], pb[:, :], gts[:, 8 + m, :], ALU.mult, [pb, gts], [t2_])
                    tt(P.dve, mgT[:, m, :], t1[:], t2_[:], ALU.add, [t1, t2_], [mgT])
                for ti in range(4):
                    xt = xts[ti % 2]; xo_ = xo[ti % 2]
                    ld(xt[:], x[t0 + ti * 128:t0 + (ti + 1) * 128, :], [xt])
                    for hf in range(2):
                        pb = bank()
                        for m in range(8):
                            mm(pb[:, :], mgT[:, m, ti * 128:(ti + 1) * 128], wo[:, m, hf * 512:(hf + 1) * 512], m == 0, m == 7, [mgT, wo], [pb])
                        tt(P.dve, t1[:], pb[:, :], g1bc[:, hf * 512:(hf + 1) * 512], ALU.mult, [pb, g1bc], [t1])
                        tt(P.dve, xo_[:, hf * 512:(hf + 1) * 512], t1[:], xt[:, hf * 512:(hf + 1) * 512], ALU.add, [t1, xt], [xo_])
                    stq(x1_d[t0 + ti * 128:t0 + (ti + 1) * 128, :], xo_[:], [xo_], [x1_d], is_out=(dbg == "x1_d"), q=P.pool)
            P.barrier()
        P.stack = gs
        if dbg == "x1_d":
            P.finish()
            return nc, P

        NT = nst * 4
        dest8 = P.tile("dest8", [128, 64, 8], I32)
        w8 = P.tile("w8", [128, 64, 8], F32)
        idxw = P.tile("idxw", [128, NBLK], I32)
        w8b = [Buf(f"w8b{i}") for i in range(64)]; d8b = [Buf(f"d8b{i}") for i in range(64)]
        with ExitStack() as ph, nc.allow_non_contiguous_dma(reason="tiny per-channel vectors"):
            P.stack = ph
            rw = P.tile("rw", [128, 8, NE], BF16)
            sw1 = P.tile("sw1", [128, 8, 256], BF16); sw3 = P.tile("sw3", [128, 8, 256], BF16); sw2 = P.tile("sw2", [128, 2, D], BF16)
            for dc in range(8):
                ldc(rw[:, dc, :], router_w[dc * 128:(dc + 1) * 128, :], [rw])
                ldc(sw1[:, dc, :], shared_w1[dc * 128:(dc + 1) * 128, :], [sw1])
                ldc(sw3[:, dc, :], shared_w3[dc * 128:(dc + 1) * 128, :], [sw3])
            for fc in range(2):
                ldc(sw2[:, fc, :], shared_w2[fc * 128:(fc + 1) * 128, :], [sw2])
            rbias = P.tile("rbias", [128, NE], F32)
            ld(rbias[:], router_bias.partition_broadcast(128), [rbias])
            eoff = P.tile("eoff", [128, NE], F32)
            ustr = P.tile("ustr", [128, 128], BF16)
            onesb = P.tile("onesb", [128, 128], BF16)
            cp(P.dve, ustr[:], mU[:], [mU], [ustr]) if False else None
            uf = P.tile("uf", [128, 128], F32)
            ms(P.pool, uf[:], 1.0, [uf])
            P.op(P.pool, lambda h: h.affine_select(out=uf[:], in_=uf[:], pattern=[[1, 128]], compare_op=ALU.is_gt, fill=0.0, base=0, channel_multiplier=-1), r=[uf], w=[uf])
            cp(P.dve, ustr[:], uf[:], [uf], [ustr])
            ms(P.dve, onesb[:], 1.0, [onesb])
            basec = P.tile("basec", [128, NE], F32)
            ms(P.dve, basec[:], 0.0, [basec])

            xts = [P.tile(f"cxt{i}", [128, D], F32) for i in range(2)]
            hT = P.tile("chT", [128, 8, 512], BF16)
            ssq = P.tile("cssq", [128, 4], F32); rstd = P.tile("crstd", [128, 4], F32)
            xn = P.tile("cxn", [128, 4, D], BF16)
            ntmp = (ssq, rstd, xn, None)
            h2row = [P.tile(f"h2row{i}", [128, D], BF16) for i in range(2)]
            class _S:
                pass

            def mkset(n):
                S = _S()
                S.sc_ = P.tile(f"sc_{n}", [128, NE], F32); S.sel = P.tile(f"sel{n}", [128, NE], F32)
                S.m88 = P.tile(f"m88{n}", [128, 8, 8], F32); S.gs_ = P.tile(f"gs_{n}", [128, 8], F32)
                S.g8 = P.tile(f"g8{n}", [128, 8], F32); S.gmask = P.tile(f"gmask{n}", [128, 8], F32)
                S.selm = P.tile(f"selm{n}", [128, NE], F32); S.smask = P.tile(f"smask{n}", [128, NE], F32)
                S.smb = P.tile(f"smb{n}", [128, NE], BF16)
                S.wd = P.tile(f"wd{n}", [128, NE], F32); S.wsum = P.tile(f"wsum{n}", [128, 2], F32)
                S.key = P.tile(f"key{n}", [128, NE], F32); S.k8 = P.tile(f"k8{n}", [128, 8], F32)
                S.kz = P.tile(f"kz{n}", [128, 8], F32); S.jk = P.tile(f"jk{n}", [128, NE], F32)
                S.t2_ = P.tile(f"ct2{n}", [128, 512], F32)
                return S
            SS = [mkset(0), mkset(1)]
            hsT = P.tile("hsT", [128, 2, 512], BF16)
            t1 = P.tile("ct1", [128, 512], F32)
            xo = [P.tile(f"cxo{i}", [128, D], F32) for i in range(2)]

            def route(tsl, S):
                pb = bank()
                for dc in range(8):
                    mm(pb[:, 0:NE], hT[:, dc, tsl], rw[:, dc, :], dc == 0, dc == 7, [hT, rw], [pb])
                act(S.sc_[:], pb[:, 0:NE], AF.Sigmoid, [pb], [S.sc_])
                yield
                tt(P.dve, S.sel[:], S.sc_[:], rbias[:], ALU.add, [S.sc_, rbias], [S.sel])
                yield
                for g_ in range(8):
                    P.op(P.dve, lambda h: h.max(out=S.m88[:, g_, :], in_=S.sel[:, g_ * 32:(g_ + 1) * 32]), r=[S.sel], w=[S.m88])
                yield
                tt(P.dve, S.gs_[:], S.m88[:, :, 0], S.m88[:, :, 1], ALU.add, [S.m88], [S.gs_])
                yield
                P.op(P.dve, lambda h: h.max(out=S.g8[:], in_=S.gs_[:]), r=[S.gs_], w=[S.g8])
                yield
                ts(P.dve, S.gmask[:], S.gs_[:], S.g8[:, 3:4], None, ALU.is_ge, None, [S.gs_, S.g8], [S.gmask])
                yield
                stt(P.dve, S.selm[:].rearrange("p (g e) -> p g e", e=32), S.sel[:].rearrange("p (g e) -> p g e", e=32), 2.0, S.gmask[:, :].unsqueeze(2).to_broadcast([128, 8, 32]), ALU.add, ALU.mult, [S.sel, S.gmask], [S.selm])
                yield
                P.op(P.dve, lambda h: h.max(out=S.g8[:], in_=S.selm[:]), r=[S.selm], w=[S.g8])
                yield
                ts(P.dve, S.smask[:], S.selm[:], S.g8[:, 7:8], None, ALU.is_ge, None, [S.selm, S.g8], [S.smask])
                yield
                cp(P.dve, S.smb[:], S.smask[:], [S.smask], [S.smb])
                yield

            def drive(*gens):
                gens = [g for g in gens if g is not None]
                while gens:
                    for g in list(gens):
                        try:
                            next(g)
                        except StopIteration:
                            gens.remove(g)

            def gen_p1(ti, S):
                yield from route(slice(ti * 128, (ti + 1) * 128), S)
                pp = bank()
                mm(pp[:, 0:NE], onesb[:], S.smb[:], True, True, [onesb, S.smb], [pp])
                tt(P.dve, basec[:], basec[:], pp[:, 0:NE], ALU.add, [pp, basec], [basec])
                yield

            for si in range(nst):
                norm_T(xts, hT, x1_d.t, si, s2, sh2, ntmp)
                drive(gen_p1(0, SS[0]), gen_p1(1, SS[1]))
                drive(gen_p1(2, SS[0]), gen_p1(3, SS[1]))
            nblk = P.tile("nblk", [128, NE], F32); pends = P.tile("pends", [128, NE], F32)
            ones256 = P.tile("ones256", [128, NE], F32)
            ms(P.dve, nblk[:], 0.0, [nblk]); ms(P.dve, ones256[:], 1.0, [ones256])
            for m_ in range(T // BLK):
                stt(P.dve, nblk[:], basec[:], float(BLK * m_), nblk[:], ALU.is_gt, ALU.add, [basec, nblk], [nblk])
            ts(P.dve, nblk[:], nblk[:], float(BLK), None, ALU.mult, None, [nblk], [nblk])
            P.op(P.dve, lambda h: h.tensor_tensor_scan(out=pends[:], data0=ones256[:], data1=nblk[:], initial=0.0, op0=ALU.mult, op1=ALU.add), r=[ones256, nblk], w=[pends])
            tt(P.dve, eoff[:], pends[:], nblk[:], ALU.subtract, [pends, nblk], [eoff])
            ts(P.dve, eoff[:], eoff[:], 1.0, None, ALU.add, None, [eoff], [eoff])
            pcol = P.tile("pcol", [128, 2], F32)
            for c_ in range(2):
                pb = bank()
                tr(pb[:, 0:128], pends[:, c_ * 128:(c_ + 1) * 128], identf[:], [pends, identf], [pb])
                cp(P.dve, pcol[:, c_:c_ + 1], pb[:, 0:1], [pb], [pcol])
            iotab = P.tile("iotab", [128, NBLK], F32)
            P.op(P.pool, lambda h: h.iota(iotab[:], pattern=[[BLK, NBLK]], base=0, channel_multiplier=0, allow_small_or_imprecise_dtypes=True), w=[iotab])
            cmpb = P.tile("cmpb", [128, 2, NBLK], BF16)
            for c_ in range(2):
                ts(P.dve, cmpb[:, c_, :], iotab[:], pcol[:, c_:c_ + 1], None, ALU.is_ge, None, [iotab, pcol], [cmpb])
            pb = bank()
            for c_ in range(2):
                mm(pb[:, :], onesb[:], cmpb[:, c_, :], c_ == 0, c_ == 1, [onesb, cmpb], [pb])
            pidx = P.tile("pidx", [128, NBLK], F32)
            P.op(P.pool, lambda h: h.iota(pidx[:], pattern=[[0, NBLK]], base=0, channel_multiplier=1, allow_small_or_imprecise_dtypes=True), w=[pidx])
            ts(P.dve, iotab[:], pb[:, :], 128.0, None, ALU.mult, None, [pb], [iotab])
            tt(P.dve, idxw[:], iotab[:], pidx[:], ALU.add, [iotab, pidx], [idxw])
            ms(P.dve, basec[:], 0.0, [basec])

            def gen_p2(si, ti, S):
                t0 = si * ST
                tg = si * 4 + ti
                tsl = slice(ti * 128, (ti + 1) * 128)
                hr = h2row[ti % 2]
                bb = bbank()
                for dc in range(8):
                    tr(bb[:, dc * 128:(dc + 1) * 128], hT[:, dc, tsl], identb[:], [hT, identb], [bb])
                cp(P.dve, hr[:], bb[:, :], [bb], [hr])
                yield
                yield from route(tsl, S)
                stt(P.dve, S.wd[:], S.smask[:], 1.0, S.sc_[:], ALU.mult, ALU.mult, [S.smask, S.sc_], [S.wd, S.wsum], accum_out=S.wsum[:, 0:1])
                yield
                P.op(P.dve, lambda h: h.reciprocal(out=S.wsum[:, 1:2], in_=S.wsum[:, 0:1]), r=[S.wsum], w=[S.wsum])
                yield
                ts(P.dve, S.wd[:], S.wd[:], S.wsum[:, 1:2], 2.5, ALU.mult, ALU.mult, [S.wd, S.wsum], [S.wd])
                pp = bank()
                mm(pp[:, 0:NE], ustr[:], S.smb[:], True, True, [ustr, S.smb], [pp])
                mm(pp[:, NE:2 * NE], onesb[:], S.smb[:], True, True, [onesb, S.smb], [pp])
                tt(P.dve, S.key[:], pp[:, 0:NE], basec[:], ALU.add, [pp, basec], [S.key])
                tt(P.dve, basec[:], basec[:], pp[:, NE:2 * NE], ALU.add, [pp, basec], [basec])
                yield
                tt(P.dve, S.key[:], S.key[:], eoff[:], ALU.add, [S.key, eoff], [S.key])
                yield
                tt(P.dve, S.key[:], S.key[:], S.smask[:], ALU.mult, [S.key, S.smask], [S.key])
                yield
                P.op(P.dve, lambda h: h.max(out=S.k8[:], in_=S.key[:]), r=[S.key], w=[S.k8])
                yield
                for k in range(8):
                    stt(P.dve, S.jk[:], S.key[:], S.k8[:, k:k + 1], S.wd[:], ALU.is_equal, ALU.mult, [S.key, S.k8, S.wd], [S.jk, w8b[tg]], accum_out=w8[:, tg, k:k + 1])
                    yield
                ts(P.dve, S.kz[:], S.k8[:], 0.0, float(NSLOT), ALU.is_equal, ALU.mult, [S.k8], [S.kz])
                yield
                stt(P.dve, dest8[:, tg, :], S.k8[:], -1.0, S.kz[:], ALU.add, ALU.add, [S.k8, S.kz], [d8b[tg]])
                yield
                for k in range(8):
                    l = LP[lpi[0] % len(LP)]; lpi[0] += 1
                    P.dma(P.pool, l, lambda h: h.indirect_dma_start(out=xg_d.t, out_offset=bass.IndirectOffsetOnAxis(ap=dest8[:, tg, k:k + 1], axis=0), in_=hr[:], in_offset=None), r=[hr, d8b[tg]], w=[xg_d])
                yield
                xt = xts[ti % 2]; xo_ = xo[ti % 2]
                ld(xt[:], x1_d[t0 + ti * 128:t0 + (ti + 1) * 128, :], [xt])
                for hf in range(2):
                    pb = bank()
                    for fc in range(2):
                        mm(pb[:, :], hsT[:, fc, tsl], sw2[:, fc, hf * 512:(hf + 1) * 512], fc == 0, fc == 1, [hsT, sw2], [pb])
                    tt(P.dve, S.t2_[:], pb[:, :], g2bc[:, hf * 512:(hf + 1) * 512], ALU.mult, [pb, g2bc], [S.t2_])
                    yield
                    tt(P.dve, xo_[:, hf * 512:(hf + 1) * 512], S.t2_[:], xt[:, hf * 512:(hf + 1) * 512], ALU.add, [S.t2_, xt], [xo_])
                    yield
                stq(x1_d[t0 + ti * 128:t0 + (ti + 1) * 128, :], xo_[:], [xo_], [x1_d], q=P.act)
                yield

            for si in range(nst):
                norm_T(xts, hT, x1_d.t, si, s2, sh2, ntmp)
                for fc in range(2):
                    p1 = bank()
                    for dc in range(8):
                        mm(p1[:, :], sw1[:, dc, fc * 128:(fc + 1) * 128], hT[:, dc, :], dc == 0, dc == 7, [sw1, hT], [p1])
                    p3 = bank()
                    for dc in range(8):
                        mm(p3[:, :], sw3[:, dc, fc * 128:(fc + 1) * 128], hT[:, dc, :], dc == 0, dc == 7, [sw3, hT], [p3])
                    act(t1[:], p1[:, :], AF.Silu, [p1], [t1])
                    tt(P.dve, hsT[:, fc, :], t1[:], p3[:, :], ALU.mult, [t1, p3], [hsT])
                drive(gen_p2(si, 0, SS[0]), gen_p2(si, 1, SS[1]))
                drive(gen_p2(si, 2, SS[0]), gen_p2(si, 3, SS[1]))
            P.barrier()
            if dbg == "pB":
                P.finish()
                raise StopBuild()
        P.stack = gs

        with ExitStack() as ph:
            P.stack = ph
            w1v = exp_w1.rearrange("e (p c) f -> (e p) (c f)", c=8)
            w3v = exp_w3.rearrange("e (p c) f -> (e p) (c f)", c=8)
            w2v = exp_w2.rearrange("e (p c) d -> (e p) (c d)", c=2)
            xgt = [P.tile(f"xgt{i}", [128, 2, D], BF16) for i in range(3)]
            xgT = [P.tile(f"xgT{i}", [128, 8, BLK], BF16) for i in range(2)]
            w1b = [P.tile(f"w1b{i}", [128, 2048], BF16) for i in range(3)]
            w3b = [P.tile(f"w3b{i}", [128, 2048], BF16) for i in range(3)]
            w2b = [P.tile(f"w2b{i}", [128, 2048], BF16) for i in range(3)]
            hid = [P.tile(f"hid{i}", [128, 2, BLK], BF16) for i in range(2)]
            st1 = [P.tile(f"st1{i}", [128, BLK], F32) for i in range(2)]
            yrow = [P.tile(f"yrow{i}", [128, 2, D], BF16) for i in range(2)]
            LY = P.lanes(2, "ly")

            bc_reg = nc.gpsimd.to_reg(NE * 128 - 1)
            def wgather(dst, src, i_):
                l = LP[lpi[0] % len(LP)]; lpi[0] += 1
                P.dma(P.pool, l, lambda h: h.indirect_dma_start(out=dst[:], out_offset=None, in_=src, in_offset=bass.IndirectOffsetOnAxis(ap=idxw[:, i_:i_ + 1], axis=0), bounds_check=bc_reg, oob_is_err=False), r=[idxw], w=[dst])

            def c_loads(i_):
                i3 = i_ % 3
                ld(xgt[i3][:], xg_d[i_ * BLK:(i_ + 1) * BLK, :].rearrange("(b p) d -> p b d", p=128), [xgt[i3]], r=[xg_d])
                wgather(w1b[i3], w1v, i_); wgather(w3b[i3], w3v, i_); wgather(w2b[i3], w2v, i_)

            def c_T(i_):
                i3 = i_ % 3; i2 = i_ % 2
                xv = xgt[i3][:].rearrange("p b (q c) -> p b c q", c=8)
                for dc in range(8):
                    bb = bbank()
                    for b_ in range(2):
                        tr(bb[:, b_ * 128:(b_ + 1) * 128], xv[:, b_, dc, :], identb[:], [xgt[i3], identb], [bb])
                    cp(P.dve, xgT[i2][:, dc, :], bb[:, 0:BLK], [bb], [xgT[i2]])

            def c_H(i_):
                i3 = i_ % 3; i2 = i_ % 2
                w1r = w1b[i3][:].rearrange("p (c m two) -> p c two m", c=8, two=2)
                w3r = w3b[i3][:].rearrange("p (c m two) -> p c two m", c=8, two=2)
                for fc in range(2):
                    p1 = bank()
                    for dc in range(8):
                        mm(p1[:, 0:BLK], w1r[:, dc, fc, :], xgT[i2][:, dc, :], dc == 0, dc == 7, [w1b[i3], xgT[i2]], [p1])
                    p3 = bank()
                    for dc in range(8):
                        mm(p3[:, 0:BLK], w3r[:, dc, fc, :], xgT[i2][:, dc, :], dc == 0, dc == 7, [w3b[i3], xgT[i2]], [p3])
                    act(st1[fc][:], p1[:, 0:BLK], AF.Silu, [p1], [st1[fc]])
                    tt(P.dve, hid[i2][:, fc, :], st1[fc][:], p3[:, 0:BLK], ALU.mult, [st1[fc], p3], [hid[i2]])

            def c_Y(i_):
                i3 = i_ % 3; i2 = i_ % 2
                w2r = w2b[i3][:].rearrange("p (c d) -> p c d", c=2)
                for b_ in range(2):
                    for hf in range(2):
                        pb = bank()
                        for fc in range(2):
                            mm(pb[:, :], hid[i2][:, fc, b_ * 128:(b_ + 1) * 128], w2r[:, fc, hf * 512:(hf + 1) * 512], fc == 0, fc == 1, [hid[i2], w2b[i3]], [pb])
                        act(yrow[i2][:, b_, hf * 512:(hf + 1) * 512], pb[:, :], AF.Copy, [pb], [yrow[i2]])
                P.dma(P.act, LY[i2], lambda h: h.dma_start(out=yg_d[i_ * BLK:(i_ + 1) * BLK, :].rearrange("(b p) d -> p b d", p=128), in_=yrow[i2][:]), r=[yrow[i2]], w=[yg_d])

            nb_ = nblk_run
            c_loads(0)
            if nb_ > 1:
                c_loads(1)
            c_T(0)
            for i_ in range(nb_):
                if i_ + 1 < nb_:
                    c_T(i_ + 1)
                c_H(i_)
                if i_ >= 1:
                    c_Y(i_ - 1)
                if i_ + 2 < nb_:
                    c_loads(i_ + 2)
            c_Y(nb_ - 1)
            P.barrier()
            if dbg == "pC":
                P.finish()
                raise StopBuild()
        P.stack = gs

        with ExitStack() as ph:
            P.stack = ph
            nfbc = P.tile("nfbc", [128, D], F32)
            ld(nfbc[:], normf_g.partition_broadcast(128), [nfbc])
            xts = [P.tile(f"dxt{i}", [128, D], F32) for i in range(2)]
            gat = [P.tile(f"gat{i}", [128, D], BF16) for i in range(4)]
            acc = P.tile("acc", [128, D], F32)
            ot = [P.tile(f"ot{i}", [128, D], F32) for i in range(2)]
            fs = P.tile("fs", [128, 2], F32)
            jk2 = P.tile("jk2", [128, D], BF16)
            for tg in range(NT):
                xt = xts[tg % 2]; o_ = ot[tg % 2]
                ld(xt[:], x1_d[tg * 128:(tg + 1) * 128, :], [xt])
                for k in range(8):
                    gt = gat[k % 4]
                    l = LP[lpi[0] % len(LP)]; lpi[0] += 1
                    P.dma(P.pool, l, lambda h: h.indirect_dma_start(out=gt[:], out_offset=None, in_=yg_d.t, in_offset=bass.IndirectOffsetOnAxis(ap=dest8[:, tg, k:k + 1], axis=0)), r=[yg_d, d8b[tg]], w=[gt])
                    if k == 0:
                        ts(P.dve, acc[:], gt[:], w8[:, tg, 0:1], None, ALU.mult, None, [gt, w8b[tg]], [acc])
                    else:
                        stt(P.dve, acc[:], gt[:], w8[:, tg, k:k + 1], acc[:], ALU.mult, ALU.add, [gt, w8b[tg], acc], [acc])
                tt(P.dve, acc[:], acc[:], g2bc[:], ALU.mult, [acc, g2bc], [acc])
                tt(P.dve, acc[:], acc[:], xt[:], ALU.add, [acc, xt], [acc])
                act(jk2[:], acc[:], AF.Square, [acc], [jk2, fs], accum_out=fs[:, 0:1])
                ts(P.dve, fs[:, 1:2], fs[:, 0:1], 1.0 / D, 1e-6, ALU.mult, ALU.add, [fs], [fs])
                act(fs[:, 1:2], fs[:, 1:2], AF.Sqrt, [fs], [fs])
                P.op(P.dve, lambda h: h.reciprocal(out=fs[:, 1:2], in_=fs[:, 1:2]), r=[fs], w=[fs])
                stt(P.dve, o_[:], acc[:], fs[:, 1:2], nfbc[:], ALU.mult, ALU.mult, [acc, fs, nfbc], [o_])
                stq(out[tg * 128:(tg + 1) * 128, :], o_[:], [o_], is_out=True, q=P.act)
            P.finish()
        P.stack = gs
        return nc, P


_NAMES = ["ada_w", "ada_b", "norm1_g", "norm2_g", "w_in", "tshift_mu", "rwkv_w0", "rwkv_w_up", "rwkv_a0", "rwkv_a_up",
          "rwkv_g_up", "rwkv_k_k", "rwkv_k_a", "rwkv_r_k", "rwkv_ln_g", "rwkv_ln_b", "gmlp_ln_g", "gmlp_ln_b", "gmlp_ws",
          "gmlp_bs", "w_out_a", "w_out_b", "w_out", "router_w", "router_bias", "exp_w1", "exp_w3", "exp_w2",
          "shared_w1", "shared_w3", "shared_w2"]


def kernel(**inputs):
    nc, _ = build()
    shared = {}
    for k in _NAMES:
        a = np.asarray(inputs[k], dtype=np.float32)[0]
        if k == "rwkv_r_k":
            a = a.reshape(512)
        shared[k] = np.ascontiguousarray(a)
    shared["normf_g"] = np.ascontiguousarray(np.asarray(inputs["normf_g"], dtype=np.float32))
    x = np.asarray(inputs["x"], dtype=np.float32)
    c = np.asarray(inputs["c"], dtype=np.float32)
    in_maps = []
    for b in range(8):
        m = dict(shared)
        m["x"] = np.ascontiguousarray(x[b])
        m["c"] = np.ascontiguousarray(c[b:b + 1])
        in_maps.append(m)
    res = run_bass_kernel_spmd(nc, in_maps, core_ids=list(range(8)))
    return np.stack([np.asarray(r["out"], dtype=np.float32) for r in res.results], axis=0)
```

```python
import numpy as np
import concourse.bass as bass
import concourse.mybir as mybir
from concourse.bass_utils import run_bass_kernel_spmd

F32 = mybir.dt.float32
BF16 = mybir.dt.bfloat16
U32 = mybir.dt.uint32
I32 = mybir.dt.int32
AF = mybir.ActivationFunctionType
ALU = mybir.AluOpType
AX = mybir.AxisListType


class Buf:
    __slots__ = ("name", "w", "rs")

    def __init__(self, name):
        self.name = name
        self.w = None
        self.rs = []


class Tl:
    def __init__(self, t, name):
        self.t = t
        self.b = Buf(name)

    def __getitem__(self, k):
        return self.t[k]


class Eng:
    def __init__(self, P, name, h, sem):
        self.P = P
        self.name = name
        self.h = h
        self.sem = sem
        self.cnt = 0
        self.waited = {}


class Lane:
    def __init__(self, sem):
        self.sem = sem
        self.val = 0


class Prog:
    def __init__(self, nc, stack):
        self.nc = nc
        self.stack = stack
        mk = lambda n: stack.enter_context(nc.semaphore(n))
        self.pe = Eng(self, "pe", nc.tensor, mk("s_pe"))
        self.act = Eng(self, "act", nc.scalar, mk("s_act"))
        self.dve = Eng(self, "dve", nc.vector, mk("s_dve"))
        self.pool = Eng(self, "pool", nc.gpsimd, mk("s_pool"))
        self.sp = Eng(self, "sp", nc.sync, mk("s_sp"))
        self.engs = [self.pe, self.act, self.dve, self.pool, self.sp]
        self.nlanes = 0
        self.all_lanes = []
        self.out_toks = []
        self.nins = 0

    def tile(self, name, shape, dt):
        return Tl(self.sb(name, shape, dt), name)

    def ptile(self, name, shape, dt=F32):
        return Tl(self.ps(name, shape, dt), name)

    def barrier(self):
        toks = [(e.sem, e.cnt) for e in self.engs if e.cnt] + [(l.sem, l.val) for l in self.all_lanes if l.val]
        for e in self.engs:
            for t in toks:
                self._wait(e, t)

    def sb(self, name, shape, dt):
        return self.stack.enter_context(self.nc.sbuf_tensor(name, shape, dt))

    def ps(self, name, shape, dt=F32):
        return self.stack.enter_context(self.nc.psum_tensor(name, shape, dt))

    def lanes(self, n, name="ln"):
        out = []
        for i in range(n):
            out.append(Lane(self.stack.enter_context(self.nc.semaphore(f"{name}{self.nlanes}"))))
            self.nlanes += 1
        self.all_lanes += out
        return out

    def _wait(self, e, tok):
        if tok is None:
            return
        sem, val = tok
        k = id(sem)
        if e.waited.get(k, 0) >= val:
            return
        if sem is e.sem and e is self.pe:
            return
        e.h.wait_ge(sem, val)
        e.waited[k] = val
        self.nins += 1

    def _deps(self, e, r, w):
        r = [getattr(b, "b", b) for b in r]
        w = [getattr(b, "b", b) for b in w]
        for b in r:
            self._wait(e, b.w)
        for b in w:
            self._wait(e, b.w)
            for t in b.rs:
                self._wait(e, t)

    def _commit(self, tok, r, w):
        r = [getattr(b, "b", b) for b in r]
        w = [getattr(b, "b", b) for b in w]
        for b in r:
            b.rs.append(tok)
            if len(b.rs) > 24:
                d = {}
                for s, v in b.rs:
                    if id(s) not in d or d[id(s)][1] < v:
                        d[id(s)] = (s, v)
                b.rs = list(d.values())
        for b in w:
            b.w = tok
            b.rs = []

    def op(self, e, fn, r=(), w=()):
        self._deps(e, r, w)
        ins = fn(e.h)
        e.cnt += 1
        ins.then_inc(e.sem, 1)
        tok = (e.sem, e.cnt)
        self._commit(tok, r, w)
        self.nins += 1
        return tok

    def dma(self, e, lane, fn, r=(), w=(), is_out=False):
        self._wait(e, (lane.sem, lane.val) if lane.val else None)
        self._deps(e, r, w)
        ins = fn(e.h)
        lane.val += 16
        ins.then_inc(lane.sem, 16)
        tok = (lane.sem, lane.val)
        self._commit(tok, r, w)
        if is_out:
            self.out_toks.append(tok)
        self.nins += 1
        return tok

    def finish(self):
        for tok in self.out_toks:
            self._wait(self.sp, tok)


from contextlib import ExitStack

T = 8192
D = 1024
ST = 512
NST = T // ST
SDEC = -0.6065306597126334
RW = 1792
CAP = 512
BLK = 256
NBLK = 512
NE = 256
NSLOT = NE * CAP
ROW = 1024 + 64


def sl(h_):
    return (h_ % 2) * 4 + h_ // 2


class StopBuild(Exception):
    pass


def build(dbg=None, nst=NST, nblk_run=NBLK):
    nc = bass.Bass("TRN2", target_bir_lowering=False)
    holder = {}
    try:
        return _build(nc, dbg, nst, holder, nblk_run)
    except StopBuild:
        return nc, holder["P"]


def _build(nc, dbg, nst, holder, nblk_run):

    def din(name, shape, dt=F32):
        return nc.dram_tensor(name, shape, dt, kind="ExternalInput").ap()

    x = din("x", [T, D]); c = din("c", [1, D])
    ada_w = din("ada_w", [D, 6 * D]); ada_b = din("ada_b", [6 * D])
    norm1_g = din("norm1_g", [D]); norm2_g = din("norm2_g", [D])
    w_in = din("w_in", [D, 4864]); tshift_mu = din("tshift_mu", [RW])
    rwkv_w0 = din("rwkv_w0", [512]); rwkv_w_up = din("rwkv_w_up", [64, 512])
    rwkv_a0 = din("rwkv_a0", [512]); rwkv_a_up = din("rwkv_a_up", [64, 512])
    rwkv_g_up = din("rwkv_g_up", [128, 512]); rwkv_k_k = din("rwkv_k_k", [512])
    rwkv_k_a = din("rwkv_k_a", [512]); rwkv_r_k = din("rwkv_r_k", [512])
    rwkv_ln_g = din("rwkv_ln_g", [512]); rwkv_ln_b = din("rwkv_ln_b", [512])
    gmlp_ln_g = din("gmlp_ln_g", [512]); gmlp_ln_b = din("gmlp_ln_b", [512])
    gmlp_ws = din("gmlp_ws", [8, 128, 128]); gmlp_bs = din("gmlp_bs", [8, 128])
    w_out_a = din("w_out_a", [512, D]); w_out_b = din("w_out_b", [512, D]); w_out = din("w_out", [D, D])
    router_w = din("router_w", [D, NE]); router_bias = din("router_bias", [NE])
    if True:
        exp_w1 = din("exp_w1", [NE, D, 256]); exp_w3 = din("exp_w3", [NE, D, 256]); exp_w2 = din("exp_w2", [NE, 256, D])
    shared_w1 = din("shared_w1", [D, 256]); shared_w3 = din("shared_w3", [D, 256]); shared_w2 = din("shared_w2", [256, D])
    normf_g = din("normf_g", [D])
    out = nc.dram_tensor("out", [T, D], F32, kind="ExternalOutput").ap()

    def dscr(name, shape, dt):
        k = "ExternalOutput" if dbg == name else "Internal"
        return Tl(nc.dram_tensor(name, shape, dt, kind=k).ap(), name)

    yaT_d = dscr("yaT_d", [512, T], BF16)
    x1_d = dscr("x1_d", [T, D], F32)
    xg_d = dscr("xg_d", [NSLOT, D], BF16)
    yg_d = dscr("yg_d", [NSLOT, D], BF16)

    with ExitStack() as gs:
        P = Prog(nc, gs)
        holder["P"] = P

        def ck(n, tl, ap=None):
            if dbg == f"ck{n}":
                o_ = nc.dram_tensor("o_ck", list((ap if ap is not None else tl[:]).shape), (ap if ap is not None else tl[:]).dtype, kind="ExternalOutput").ap()
                stq(o_, ap if ap is not None else tl[:], [tl], is_out=True)
                P.finish()
                raise StopBuild()
        LD = P.lanes(8, "ld")
        LP = P.lanes(8, "lp")
        LS = P.lanes(4, "lst")
        ldi = [0]; lpi = [0]; lsi = [0]

        def ld(out_ap, in_ap, w, r=()):
            l = LD[ldi[0] % len(LD)]; ldi[0] += 1
            return P.dma(P.sp, l, lambda h: h.dma_start(out=out_ap, in_=in_ap), r=r, w=w)

        def ldc(out_ap, in_ap, w, r=()):
            l = LP[lpi[0] % len(LP)]; lpi[0] += 1
            return P.dma(P.pool, l, lambda h: h.dma_start(out=out_ap, in_=in_ap), r=r, w=w)

        LSQ = {"sp": LS, "pool": P.lanes(3, "lsp"), "act": P.lanes(3, "lsa")}

        def stq(out_ap, in_ap, r, w=(), is_out=False, q=None):
            q = q or P.sp
            ll = LSQ[q.name]
            l = ll[lsi[0] % len(ll)]; lsi[0] += 1
            return P.dma(q, l, lambda h: h.dma_start(out=out_ap, in_=in_ap), r=r, w=w, is_out=is_out)

        pbanks = [P.ptile(f"pb{i}", [128, 512], F32) for i in range(6)]
        bbanks = [P.ptile(f"bb{i}", [128, 1024], BF16) for i in range(2)]
        pbi = [0]; bbi = [0]

        def bank():
            b = pbanks[pbi[0] % 6]; pbi[0] += 1
            return b

        def bbank():
            b = bbanks[bbi[0] % 2]; bbi[0] += 1
            return b

        def mm(o, lhsT, rhs, start, stop, r, w):
            return P.op(P.pe, lambda h: h.matmul(o, lhsT=lhsT, rhs=rhs, start=start, stop=stop), r=r, w=w)

        def tr(o, in_, ident, r, w):
            return P.op(P.pe, lambda h: h.transpose(out=o, in_=in_, identity=ident), r=r, w=w)

        def act(o, in_, func, r, w, **kw):
            return P.op(P.act, lambda h: h.activation(out=o, in_=in_, func=func, **kw), r=r, w=w)

        def tt(e, o, a, b, op, r, w):
            return P.op(e, lambda h: h.tensor_tensor(out=o, in0=a, in1=b, op=op), r=r, w=w)

        def ts(e, o, a, s1, s2, op0, op1, r, w):
            if s2 is None:
                return P.op(e, lambda h: h.tensor_scalar(out=o, in0=a, scalar1=s1, scalar2=None, op0=op0), r=r, w=w)
            return P.op(e, lambda h: h.tensor_scalar(out=o, in0=a, scalar1=s1, scalar2=s2, op0=op0, op1=op1), r=r, w=w)

        def stt(e, o, a, s, b, op0, op1, r, w, **kw):
            return P.op(e, lambda h: h.scalar_tensor_tensor(out=o, in0=a, scalar=s, in1=b, op0=op0, op1=op1, **kw), r=r, w=w)

        def cp(e, o, a, r, w):
            return P.op(e, lambda h: h.tensor_copy(out=o, in_=a), r=r, w=w)

        def ms(e, o, v, w, r=()):
            return P.op(e, lambda h: h.memset(o, v), r=r, w=w)

        identf = P.tile("identf", [128, 128], F32)
        identb = P.tile("identb", [128, 128], BF16)
        ms(P.pool, identf[:], 0.0, [identf])
        P.op(P.pool, lambda h: h.affine_select(out=identf[:], in_=identf[:], pattern=[[-1, 128]], compare_op=ALU.not_equal, fill=1.0, base=0, channel_multiplier=1), r=[identf], w=[identf])
        cp(P.dve, identb[:], identf[:], [identf], [identb])
        maskCM = P.tile("maskCM", [128, 512], F32)
        mAT = P.tile("mAT", [128, 128], F32)
        ms(P.pool, maskCM[:], 1.0, [maskCM])
        P.op(P.pool, lambda h: h.affine_select(out=maskCM[:, 0:128], in_=maskCM[:, 0:128], pattern=[[1, 128]], compare_op=ALU.is_gt, fill=0.0, base=0, channel_multiplier=-1), r=[maskCM], w=[maskCM])
        P.op(P.pool, lambda h: h.affine_select(out=maskCM[:, 128:256], in_=maskCM[:, 128:256], pattern=[[1, 128]], compare_op=ALU.is_ge, fill=0.0, base=0, channel_multiplier=-1), r=[maskCM], w=[maskCM])
        ms(P.pool, maskCM[0:64, 64:128], 0.0, [maskCM], [maskCM])
        ms(P.pool, maskCM[0:64, 192:256], 0.0, [maskCM], [maskCM])
        cp(P.dve, maskCM[:, 256:512], maskCM[:, 0:256], [maskCM], [maskCM])
        ms(P.pool, mAT[:], 1.0, [mAT])
        P.op(P.pool, lambda h: h.affine_select(out=mAT[:], in_=mAT[:], pattern=[[-1, 128]], compare_op=ALU.is_gt, fill=0.0, base=0, channel_multiplier=1), r=[mAT], w=[mAT])
        ms(P.pool, mAT[64:128, 0:64], 0.0, [mAT], [mAT])
        bd4 = P.tile("bd4", [128, 512], F32)
        bdb = P.tile("bdb", [128, 128], BF16)
        ms(P.pool, bd4[:], 0.0, [bd4])
        for q in range(4):
            ms(P.pool, bd4[0:64, q * 128:q * 128 + 64], 1.0, [bd4], [bd4])
            ms(P.pool, bd4[64:128, q * 128 + 64:q * 128 + 128], 1.0, [bd4], [bd4])
        cp(P.dve, bdb[:], bd4[:, 0:128], [bd4], [bdb])
        reset = P.tile("reset", [128, 512], F32)
        ms(P.pool, reset[:], 1.0, [reset])
        ms(P.pool, reset[:].rearrange("p (c t) -> p c t", t=64)[:, :, 0:1], 0.0, [reset], [reset])

        zt = P.tile("zt", [128, 1024], BF16)
        ms(P.pool, zt[:], 0.0, [zt])
        NZ = NSLOT // 128

        s1 = P.tile("s1", [128, 8], F32); sh1 = P.tile("sh1", [128, 8], F32)
        s2 = P.tile("s2", [128, 8], F32); sh2 = P.tile("sh2", [128, 8], F32)
        g1bc = P.tile("g1bc", [128, D], F32); g2bc = P.tile("g2bc", [128, D], F32)
        with ExitStack() as ph, nc.allow_non_contiguous_dma(reason="tiny per-channel vectors"):
            P.stack = ph
            ccol = P.tile("ccol", [128, 8], F32)
            ld(ccol[:], c[0, :].rearrange("(n p) -> p n", p=128), [ccol])
            scol2 = P.tile("scol2", [128, 8, 2], F32)
            act(scol2[:, :, 0], ccol[:], AF.Silu, [ccol], [scol2])
            act(scol2[:, :, 1], ccol[:], AF.Silu, [ccol], [scol2])
            sbc = P.tile("sbc", [128, 8, 128], F32)
            for kc in range(8):
                cp(P.dve, sbc[:, kc, :], scol2[:, kc, 0:1].to_broadcast([128, 128]), [scol2], [sbc])
            adabT = P.tile("adabT", [128, 48], F32)
            ld(adabT[:], ada_b.rearrange("(n p) -> p n", p=128), [adabT])
            n1g = P.tile("n1g", [128, 8], F32); n2g = P.tile("n2g", [128, 8], F32)
            ld(n1g[:], norm1_g.rearrange("(n p) -> p n", p=128), [n1g])
            ld(n2g[:], norm2_g.rearrange("(n p) -> p n", p=128), [n2g])
            modT = P.tile("modT", [128, 48], F32)
            awb = [P.tile(f"awb{i}", [128, 8, 1024], F32) for i in range(2)]
            adab_bc = P.tile("adab_bc", [128, 1024], F32)
            for vi in range(6):
                aw = awb[vi % 2]
                for kc in range(8):
                    ld(aw[:, kc, :], ada_w[kc * 128:(kc + 1) * 128, vi * 1024:(vi + 1) * 1024], [aw])
                if vi in (2, 5):
                    gbc = g1bc if vi == 2 else g2bc
                    ld(adab_bc[:], ada_b[vi * 1024:(vi + 1) * 1024].partition_broadcast(128), [adab_bc])
                    for hf in range(2):
                        pb = bank()
                        for kc in range(8):
                            mm(pb[:, :], sbc[:, kc, :], aw[:, kc, hf * 512:(hf + 1) * 512], kc == 0, kc == 7, [sbc, aw], [pb])
                        tt(P.dve, gbc[:, hf * 512:(hf + 1) * 512], pb[:, :], adab_bc[:, hf * 512:(hf + 1) * 512], ALU.add, [pb, adab_bc], [gbc])
                else:
                    pb = bank()
                    for oc in range(8):
                        for kc in range(8):
                            mm(pb[:, oc * 2:oc * 2 + 2], aw[:, kc, oc * 128:(oc + 1) * 128], scol2[:, kc, :], kc == 0, kc == 7, [aw, scol2], [pb])
                    tt(P.dve, modT[:, vi * 8:(vi + 1) * 8], pb[:, 0:16].rearrange("p (o t) -> p o t", t=2)[:, :, 0], adabT[:, vi * 8:(vi + 1) * 8], ALU.add, [pb, adabT], [modT])
            stt(P.dve, s1[:], modT[:, 8:16], 1.0, n1g[:], ALU.add, ALU.mult, [modT, n1g], [s1])
            cp(P.dve, sh1[:], modT[:, 0:8], [modT], [sh1])
            stt(P.dve, s2[:], modT[:, 32:40], 1.0, n2g[:], ALU.add, ALU.mult, [modT, n2g], [s2])
            cp(P.dve, sh2[:], modT[:, 24:32], [modT], [sh2])
            P.barrier()
            if dbg == "p0":
                o0 = nc.dram_tensor("o_p0", [128, 32], F32, kind="ExternalOutput").ap()
                o1 = nc.dram_tensor("o_g", [128, 2048], F32, kind="ExternalOutput").ap()
                stq(o0[:, 0:8], s1[:], [s1], is_out=True); stq(o0[:, 8:16], sh1[:], [sh1], is_out=True)
                stq(o0[:, 16:24], s2[:], [s2], is_out=True); stq(o0[:, 24:32], sh2[:], [sh2], is_out=True)
                stq(o1[:, 0:1024], g1bc[:], [g1bc], is_out=True); stq(o1[:, 1024:2048], g2bc[:], [g2bc], is_out=True)
                P.finish()
                return nc, P
        P.stack = gs

        def norm_T(xts, hT, src, sidx, sc, shf, tmp):
            t0 = sidx * ST
            ssq, rstd, xn, junk = tmp
            for ti in range(4):
                xt = xts[ti % 2]
                ld(xt[:], src[t0 + ti * 128:t0 + (ti + 1) * 128, :], [xt])
                act(xn[:, ti, :], xt[:], AF.Square, [xt], [xn, ssq], accum_out=ssq[:, ti:ti + 1])
                ts(P.dve, rstd[:, ti:ti + 1], ssq[:, ti:ti + 1], 1.0 / D, 1e-6, ALU.mult, ALU.add, [ssq], [rstd])
                act(rstd[:, ti:ti + 1], rstd[:, ti:ti + 1], AF.Sqrt, [rstd], [rstd])
                P.op(P.dve, lambda h: h.reciprocal(out=rstd[:, ti:ti + 1], in_=rstd[:, ti:ti + 1]), r=[rstd], w=[rstd])
                act(xn[:, ti, :], xt[:], AF.Identity, [xt, rstd], [xn], scale=rstd[:, ti:ti + 1])
            if dbg == "a1n1":
                o0 = nc.dram_tensor("o_xn", [128, 4, D], BF16, kind="ExternalOutput").ap()
                stq(o0, xn[:], [xn], is_out=True)
                o1 = nc.dram_tensor("o_rstd", [128, 4], F32, kind="ExternalOutput").ap()
                stq(o1, rstd[:], [rstd], is_out=True)
                P.finish()
                return
            for dc in range(8):
                bb = bbank()
                for ti in range(4):
                    tr(bb[:, ti * 128:(ti + 1) * 128], xn[:, ti, dc * 128:(dc + 1) * 128], identb[:], [xn, identb], [bb])
                act(hT[:, dc, :], bb[:, 0:512], AF.Identity, [bb, sc, shf], [hT], scale=sc[:, dc:dc + 1], bias=shf[:, dc:dc + 1])

        with ExitStack() as ph, nc.allow_non_contiguous_dma(reason="tiny per-channel vectors"):
            P.stack = ph
            winr = P.tile("winr", [128, 8, RW], BF16)
            for dc in range(8):
                ldc(winr[:, dc, :], w_in[dc * 128:(dc + 1) * 128, 0:RW], [winr])
            Wlw = P.tile("Wlw", [128, 512], BF16); Wla = P.tile("Wla", [128, 512], BF16)
            gup = P.tile("gup", [128, 512], BF16)
            ms(P.dve, Wlw[:], 0.0, [Wlw]); ms(P.dve, Wla[:], 0.0, [Wla])
            ldc(Wlw[0:64, :], rwkv_w_up, [Wlw]); ldc(Wla[64:128, :], rwkv_a_up, [Wla]); ldc(gup[:], rwkv_g_up, [gup])

            def colvec(name, src, n):
                t = P.tile(name, [128, n], F32)
                ld(t[:], src.rearrange("(n p) -> p n", p=128), [t])
                return t
            w0 = colvec("w0", rwkv_w0, 4); a0 = colvec("a0", rwkv_a0, 4); kkv = colvec("kkv", rwkv_k_k, 4)
            kav = colvec("kav", rwkv_k_a, 4); rkv = colvec("rkv", rwkv_r_k, 4)
            lng = colvec("lng", rwkv_ln_g, 4); lnb = colvec("lnb", rwkv_ln_b, 4)
            mu = colvec("mu", tshift_mu, 14)
            omu = P.tile("omu", [128, 14], F32); omka = P.tile("omka", [128, 4], F32)
            ts(P.dve, omu[:], mu[:], -1.0, 1.0, ALU.mult, ALU.add, [mu], [omu])
            ts(P.dve, omka[:], kav[:], -1.0, 1.0, ALU.mult, ALU.add, [kav], [omka])

            if dbg == "a1w":
                o1 = nc.dram_tensor("o_omu", [128, 14], F32, kind="ExternalOutput").ap()
                stq(o1, omu[:], [omu], is_out=True)
                o2 = nc.dram_tensor("o_wla", [128, 512], BF16, kind="ExternalOutput").ap()
                stq(o2, Wla[:], [Wla], is_out=True)
                o3 = nc.dram_tensor("o_winr", [128, 8, RW], BF16, kind="ExternalOutput").ap()
                stq(o3, winr[:], [winr], is_out=True)
                P.finish()
                return nc, P
            xts = [P.tile(f"xt{i}", [128, D], F32) for i in range(2)]
            hT = P.tile("hT", [128, 8, 512], BF16)
            ssq = P.tile("ssq", [128, 4], F32); rstd = P.tile("rstd", [128, 4], F32)
            xn = P.tile("xn", [128, 4, D], BF16); junk = None
            ntmp = (ssq, rstd, xn, junk)
            sh = [P.tile(f"shq{q}", [128, 512], F32) for q in range(14)]
            pmus = [P.tile("pmu0", [128, 513], F32)] * 2
            carry = P.tile("carry", [128, 14], F32)
            ms(P.pool, carry[:], 0.0, [carry])
            lo_in = P.tile("lo_in", [128, 512], BF16); sxg = P.tile("sxg", [128, 512], BF16)
            gT = [P.tile(f"gT{j}", [128, 512], BF16) for j in range(4)]
            bonT = [P.tile(f"bonT{j}", [128, 512], BF16) for j in range(4)]
            RA = P.tile("RA", [128, 4, 4, 256], BF16)
            BT = [P.tile(f"BT{j}", [128, 512], BF16) for j in range(4)]
            KT = [P.tile(f"KT{j}", [128, 512], BF16) for j in range(4)]
            gC = P.tile("gC", [128, 4, 8], F32)
            TOKB2 = P.tile("TOKB2", [128, 4, 512], BF16)
            TOKK2 = P.tile("TOKK2", [128, 4, 512], BF16)
            TOKV = P.tile("TOKV", [128, 4, 512], BF16)
            sgw = P.tile("sgw", [128, 512], F32); asig = P.tile("asig", [128, 512], F32)
            kksq = P.tile("kksq", [128, 512], BF16); rn = P.tile("rn", [128, 512], F32)
            kkn = P.tile("kkn", [128, 512], F32); ff = P.tile("ff", [128, 512], F32)
            kp = P.tile("kp", [128, 512], F32); bp = P.tile("bp", [128, 512], F32)
            cum = P.tile("cum", [128, 512], F32); cme = ff
            cdf = rn
            e1 = sgw; e2 = P.tile("e2", [128, 512], F32)
            e3 = e1; e4 = e2
            rk = P.tile("rk", [128, 512], BF16)
            tb3 = P.tile("tb3", [128, 3, 512], BF16)
            class _View:
                def __init__(self, ap, b):
                    self.ap = ap; self.b = b

                def __getitem__(self, k):
                    return self.ap[k]
            CM = [P.tile("CM0", [128, 8, 512], BF16), _View(xn[:].rearrange("p a (b c) -> p (a b) c", c=512), xn.b)]
            Xs = [P.tile(f"Xs{i}", [128, 8, 128], BF16) for i in range(2)]
            XTs = [P.tile(f"XTs{i}", [128, 8, 128], BF16) for i in range(2)]
            Ps = [P.tile(f"Ps{i}", [128, 8, 128], BF16) for i in range(2)]
            TT = [P.tile(f"TT{i}", [128, 8, 128], BF16) for i in range(2)]
            Hf = [P.tile(f"Hf{i}", [128, 512], F32) for i in range(2)]
            Hb = [P.tile(f"Hb{i}", [128, 512], BF16) for i in range(2)]
            ms(P.pool, Hf[0][:], 0.0, [Hf[0]]); ms(P.pool, Hb[0][:], 0.0, [Hb[0]])
            X1sb = P.tile("X1sb", [128, 512], BF16); Usb = P.tile("Usb", [128, 512], BF16)
            ms(P.pool, X1sb[:], 0.0, [X1sb]); ms(P.pool, Usb[:], 0.0, [Usb])
            htmp = kp
            Ysb = P.tile("Ysb", [128, 512], F32)
            gmean = P.tile("gmean", [128, 8], F32); gvar = P.tile("gvar", [128, 8], F32)
            yc = e2; ysq = e1
            ynb = P.tile("ynb", [128, 4, 512], BF16)
            yaT = P.tile("yaT", [128, 4, 512], BF16)
            yt1 = kkn
            hcur = [0]
            chunk_g = 0

            for si in range(nst):
                t0 = si * ST
                norm_T(xts, hT, x, si, s1, sh1, ntmp)
                for zi in range(si * (NZ // NST), (si + 1) * (NZ // NST)):
                    stq(xg_d[zi * 128:(zi + 1) * 128, :], zt[:], [zt], [xg_d])
                if dbg == "a1n1":
                    return nc, P
                if dbg == "a1n":
                    o0 = nc.dram_tensor("o_hT", [128, 8, 512], BF16, kind="ExternalOutput").ap()
                    stq(o0, hT[:], [hT], is_out=True)
                    o1 = nc.dram_tensor("o_omu", [128, 14], F32, kind="ExternalOutput").ap()
                    stq(o1, omu[:], [omu], is_out=True)
                    P.finish()
                    return nc, P
                for q in range(14):
                    pb = bank()
                    for dc in range(8):
                        mm(pb[:, :], winr[:, dc, q * 128:(q + 1) * 128], hT[:, dc, :], dc == 0, dc == 7, [winr, hT], [pb])
                    pmu = pmus[q % 2]
                    act(sh[q][:], pb[:, :], AF.Identity, [pb, omu], [sh[q]], scale=omu[:, q:q + 1])
                    act(pmu[:, 0:1], carry[:, q:q + 1], AF.Copy, [carry], [pmu])
                    ts(P.dve, pmu[:, 1:513], pb[:, :], mu[:, q:q + 1], None, ALU.mult, None, [pb, mu], [pmu])
                    tt(P.dve, sh[q][:], sh[q][:], pmu[:, 0:512], ALU.add, [sh[q], pmu], [sh[q]])
                    act(carry[:, q:q + 1], pmu[:, 512:513], AF.Copy, [pmu], [carry])
                if dbg == "a1a":
                    o0 = nc.dram_tensor("o_sh", [14, 128, 512], F32, kind="ExternalOutput").ap()
                    for q in range(14):
                        stq(o0[q], sh[q][:], [sh[q]], is_out=True)
                    P.finish()
                    return nc, P
                act(lo_in[0:64, :], sh[12][0:64, :], AF.Tanh, [sh[12]], [lo_in])
                act(lo_in[64:128, :], sh[12][64:128, :], AF.Copy, [sh[12]], [lo_in])
                act(sxg[:], sh[13][:], AF.Sigmoid, [sh[13]], [sxg])
                for j in range(4):
                    r_, k_, v_ = sh[j], sh[4 + j], sh[8 + j]
                    js = slice(j * 128, (j + 1) * 128)
                    pb = bank()
                    mm(pb[:, :], Wlw[:, js], lo_in[:], True, True, [Wlw, lo_in], [pb])
                    act(sgw[:], pb[:, :], AF.Sigmoid, [pb, w0], [sgw], bias=w0[:, j:j + 1])
                    pb = bank()
                    mm(pb[:, :], Wla[:, js], lo_in[:], True, True, [Wla, lo_in], [pb])
                    act(asig[:], pb[:, :], AF.Sigmoid, [pb, a0], [asig], bias=a0[:, j:j + 1])
                    pb = bank()
                    mm(pb[:, :], gup[:, js], sxg[:], True, True, [gup, sxg], [pb])
                    act(gT[j][:], pb[:, :], AF.Copy, [pb], [gT[j]])
                    ck(1, asig)
                    act(kksq[:], k_[:], AF.Square, [k_, kkv], [kksq], scale=kkv[:, j:j + 1])
                    pb = bank()
                    mm(pb[:, :], bdb[:], kksq[:], True, True, [bdb, kksq], [pb])
                    act(rn[:], pb[:, :], AF.Sqrt, [pb], [rn], bias=1e-24)
                    P.op(P.dve, lambda h: h.reciprocal(out=rn[:], in_=rn[:]), r=[rn], w=[rn])
                    stt(P.dve, kkn[:], k_[:], kkv[:, j:j + 1], rn[:], ALU.mult, ALU.mult, [k_, kkv, rn], [kkn])
                    ck(2, kkn)
                    ts(P.dve, ff[:], asig[:], kav[:, j:j + 1], omka[:, j:j + 1], ALU.mult, ALU.add, [asig, kav, omka], [ff])
                    tt(P.dve, kp[:], k_[:], ff[:], ALU.mult, [k_, ff], [kp])
                    tt(P.dve, bp[:], kkn[:], asig[:], ALU.mult, [kkn, asig], [bp])
                    P.op(P.dve, lambda h: h.tensor_tensor_scan(out=cum[:], data0=reset[:], data1=sgw[:], initial=0.0, op0=ALU.mult, op1=ALU.add), r=[reset, sgw], w=[cum])
                    tt(P.dve, cme[:], cum[:], sgw[:], ALU.subtract, [cum, sgw], [cme])
                    ck(3, cum)
                    cum3 = cum[:].rearrange("p (c t) -> p c t", t=64)
                    tt(P.dve, cdf[:].rearrange("p (c t) -> p c t", t=64), cum3[:, :, 63:64].to_broadcast([128, 8, 64]), cum3, ALU.subtract, [cum], [cdf])
                    ck(4, cdf)
                    act(e1[:], cum[:], AF.Exp, [cum], [e1], scale=SDEC)
                    act(e2[:], cme[:], AF.Exp, [cme], [e2], scale=SDEC)
                    act(gC[:, j, :], cum3[:, :, 63], AF.Exp, [cum], [gC], scale=SDEC)
                    tt(P.dve, RA[:, j, :, 128:256], r_[:].rearrange("p (a t) -> p a t", t=128), e1[:].rearrange("p (a t) -> p a t", t=128), ALU.mult, [r_, e1], [RA])
                    stt(P.dve, RA[:, j, :, 0:128], kkn[:].rearrange("p (a t) -> p a t", t=128), -1.0, e2[:].rearrange("p (a t) -> p a t", t=128), ALU.mult, ALU.mult, [kkn, e2], [RA])
                    ck(5, RA)
                    act(e3[:], cum[:], AF.Exp, [cum], [e3], scale=-SDEC)
                    act(e4[:], cdf[:], AF.Exp, [cdf], [e4], scale=SDEC)
                    tt(P.dve, BT[j][:], bp[:], e3[:], ALU.mult, [bp, e3], [BT[j]])
                    tt(P.dve, KT[j][:], kp[:], e3[:], ALU.mult, [kp, e3], [KT[j]])
                    tt(P.dve, tb3[:, 0, :], bp[:], e4[:], ALU.mult, [bp, e4], [tb3])
                    tt(P.dve, tb3[:, 1, :], kp[:], e4[:], ALU.mult, [kp, e4], [tb3])
                    act(tb3[:, 2, :], v_[:], AF.Copy, [v_], [tb3])
                    stt(P.dve, rk[:], r_[:], rkv[:, j:j + 1], kp[:], ALU.mult, ALU.mult, [r_, rkv, kp], [rk])
                    pb = bank()
                    mm(pb[:, :], bdb[:], rk[:], True, True, [bdb, rk], [pb])
                    tt(P.dve, bonT[j][:], pb[:, :], v_[:], ALU.mult, [pb, v_], [bonT[j]])
                    ck(6, bonT[j])
                    for ti in range(4):
                        bb = bbank()
                        for kind in range(3):
                            tr(bb[:, kind * 128:(kind + 1) * 128], tb3[:, kind, ti * 128:(ti + 1) * 128], identb[:], [tb3, identb], [bb])
                        cp(P.dve, TOKB2[:, ti, js], bb[:, 0:128], [bb], [TOKB2])
                        cp(P.dve, TOKK2[:, ti, js], bb[:, 128:256], [bb], [TOKK2])
                        cp(P.dve, TOKV[:, ti, js], bb[:, 256:384], [bb], [TOKV])
                    ck(7, TOKV, TOKV[:, 0, 0:128])

                if dbg == "a1b":
                    o0 = nc.dram_tensor("o_ra", [128, 4, 4, 256], BF16, kind="ExternalOutput").ap()
                    o1 = nc.dram_tensor("o_tokv", [128, 4, 512], BF16, kind="ExternalOutput").ap()
                    o2 = nc.dram_tensor("o_gc", [128, 4, 8], F32, kind="ExternalOutput").ap()
                    stq(o0, RA[:], [RA], is_out=True); stq(o1, TOKV[:], [TOKV], is_out=True); stq(o2, gC[:], [gC], is_out=True)
                    P.finish()
                    return nc, P
                def gen_D(ti):
                    cm = CM[ti % 2]; tts = TT[ti % 2]
                    tsl = slice(ti * 128, (ti + 1) * 128)
                    for hb4 in range(2):
                        pbn = bank()
                        for hh in range(4):
                            h_ = 2 * hh + hb4; j = hh; ps_ = slice(hb4 * 64, hb4 * 64 + 64)
                            mm(pbn[:, hh * 128:(hh + 1) * 128], RA[ps_, j, ti, 0:128], BT[j][ps_, tsl], True, True, [RA, BT[j]], [pbn])
                        tt(P.dve, XTs[0][:, hb4 * 4:(hb4 + 1) * 4, :], pbn[:, :].rearrange("p (a t) -> p a t", t=128), mAT[:, :].unsqueeze(1).to_broadcast([128, 4, 128]), ALU.mult, [pbn, mAT], [XTs[0]])
                    for h_ in range(8):
                        j = h_ // 2; ps_ = slice((h_ % 2) * 64, (h_ % 2) * 64 + 64)
                        pb = bank()
                        mm(pb[:, 0:256], KT[j][ps_, tsl], RA[ps_, j, ti, :], True, True, [KT[j], RA], [pb])
                        mm(pb[:, 256:512], BT[j][ps_, tsl], RA[ps_, j, ti, :], True, True, [BT[j], RA], [pb])
                        tt(P.dve, cm[:, sl(h_), :], pb[:, :], maskCM[:], ALU.mult, [pb, maskCM], [cm])
                        if h_ % 2 == 1:
                            yield
                    ck(8, cm, cm[:, 0, :])
                    cp(P.dve, Xs[0][:], cm[:, :, 256:384], [cm], [Xs[0]])
                    tt(P.dve, Ps[0][:], cm[:, :, 256:384], identf[:, :].unsqueeze(1).to_broadcast([128, 8, 128]), ALU.add, [cm, identf], [Ps[0]])
                    cur = 0
                    for it in range(1, 6):
                        nxt = 1 - cur
                        last = (it == 5)
                        for hb4 in range(2):
                            hs4 = slice(hb4 * 4, hb4 * 4 + 4)
                            pbx = bank() if not last else None
                            pbt = bank()
                            for hh in range(4):
                                h_ = hb4 * 4 + hh
                                cs = slice(hh * 128, (hh + 1) * 128)
                                if not last:
                                    mm(pbx[:, cs], XTs[cur][:, h_, :], Xs[cur][:, h_, :], True, True, [XTs[cur], Xs[cur]], [pbx])
                                mm(pbt[:, cs], Xs[cur][:, h_, :], XTs[cur][:, h_, :], True, True, [XTs[cur], Xs[cur]], [pbt])
                            if not last:
                                act(Xs[nxt][:, hs4, :], pbx[:, :].rearrange("p (a t) -> p a t", t=128), AF.Copy, [pbx], [Xs[nxt]])
                            cp(P.dve, XTs[nxt][:, hs4, :], pbt[:, :].rearrange("p (a t) -> p a t", t=128), [pbt], [XTs[nxt]])
                            pbp = bank()
                            for hh in range(4):
                                h_ = hb4 * 4 + hh
                                cs = slice(hh * 128, (hh + 1) * 128)
                                mm(pbp[:, cs], identb[:], Ps[cur][:, h_, :], True, False, [identb, Ps[cur]], [pbp])
                                mm(pbp[:, cs], XTs[nxt][:, h_, :], Ps[cur][:, h_, :], False, True, [XTs[nxt], Ps[cur]], [pbp])
                            dst = tts if last else Ps[nxt]
                            act(dst[:, hs4, :], pbp[:, :].rearrange("p (a t) -> p a t", t=128), AF.Copy, [pbp], [dst])
                            yield
                        cur = nxt

                def gen_S(ti):
                    cm = CM[ti % 2]; tts = TT[ti % 2]
                    for p in range(2):
                        rows = slice(64 * p, 64 * p + 64)
                        cc = slice(64 * p, 64 * p + 64)
                        hold_f, hold_b = Hf[hcur[0]], Hb[hcur[0]]
                        hnew_f, hnew_b = Hf[1 - hcur[0]], Hb[1 - hcur[0]]
                        ch = ti * 2 + p
                        tt(P.dve, hnew_f[:].rearrange("p (j v) -> p j v", v=128), hold_f[:].rearrange("p (j v) -> p j v", v=128), gC[:, :, ch:ch + 1].to_broadcast([128, 4, 128]), ALU.mult, [hold_f, gC], [hnew_f])
                        ps1 = bank()
                        for h_ in range(8):
                            j = h_ // 2; hb = h_ % 2
                            o = ps1[rows, h_ * 64:(h_ + 1) * 64]
                            mm(o, RA[:, j, ti, 64 * p:64 * p + 64], hold_b[:, j * 128 + hb * 64:j * 128 + hb * 64 + 64], True, False, [RA, hold_b], [ps1])
                            mm(o, cm[:, sl(h_), 64 * p:64 * p + 64], TOKV[:, ti, h_ * 64:(h_ + 1) * 64], False, True, [cm, TOKV], [ps1])
                        act(X1sb[rows, :], ps1[rows, :], AF.Copy, [ps1], [X1sb])
                        yield
                        ps2 = bank()
                        for h_ in range(8):
                            mm(ps2[rows, h_ * 64:(h_ + 1) * 64], tts[:, sl(h_), 64 * p:64 * p + 64], X1sb[:, h_ * 64:(h_ + 1) * 64], True, True, [tts, X1sb], [ps2])
                        cp(P.dve, Usb[rows, :], ps2[rows, :], [ps2], [Usb])
                        yield
                        ps4 = bank()
                        for h_ in range(8):
                            j = h_ // 2; hb = h_ % 2
                            o = ps4[rows, h_ * 64:(h_ + 1) * 64]
                            mm(o, RA[:, j, ti, 128 + 64 * p:128 + 64 * p + 64], hold_b[:, j * 128 + hb * 64:j * 128 + hb * 64 + 64], True, False, [RA, hold_b], [ps4])
                            mm(o, cm[:, sl(h_), 384 + 64 * p:384 + 64 * p + 64], Usb[:, h_ * 64:(h_ + 1) * 64], False, False, [cm, Usb], [ps4])
                            mm(o, cm[:, sl(h_), 128 + 64 * p:128 + 64 * p + 64], TOKV[:, ti, h_ * 64:(h_ + 1) * 64], False, True, [cm, TOKV], [ps4])
                        act(Ysb[rows, :], ps4[rows, :], AF.Copy, [ps4], [Ysb])
                        yield
                        ps3 = bank()
                        for j in range(4):
                            js = slice(j * 128, (j + 1) * 128)
                            mm(ps3[:, js], TOKB2[rows, ti, js], Usb[rows, js], True, False, [TOKB2, Usb], [ps3])
                            mm(ps3[:, js], TOKK2[rows, ti, js], TOKV[rows, ti, js], False, True, [TOKK2, TOKV], [ps3])
                        tt(P.dve, htmp[:], ps3[:, :], bd4[:], ALU.mult, [ps3, bd4], [htmp])
                        tt(P.dve, hnew_f[:], hnew_f[:], htmp[:], ALU.add, [hnew_f, htmp], [hnew_f])
                        act(hnew_b[:], hnew_f[:], AF.Copy, [hnew_f], [hnew_b])
                        hcur[0] = 1 - hcur[0]
                        yield
                    y3 = Ysb[:, :].rearrange("p (h v) -> p h v", v=64)
                    P.op(P.dve, lambda h: h.tensor_reduce(out=gmean[:], in_=y3, axis=AX.X, op=ALU.add), r=[Ysb], w=[gmean])
                    ts(P.dve, gmean[:], gmean[:], 1.0 / 64, None, ALU.mult, None, [gmean], [gmean])
                    tt(P.dve, yc[:].rearrange("p (h v) -> p h v", v=64), y3, gmean[:, :].unsqueeze(2).to_broadcast([128, 8, 64]), ALU.subtract, [Ysb, gmean], [yc])
                    tt(P.dve, ysq[:], yc[:], yc[:], ALU.mult, [yc], [ysq])
                    P.op(P.dve, lambda h: h.tensor_reduce(out=gvar[:], in_=ysq[:].rearrange("p (h v) -> p h v", v=64), axis=AX.X, op=ALU.add), r=[ysq], w=[gvar])
                    ts(P.dve, gvar[:], gvar[:], 1.0 / 64, 64e-5, ALU.mult, ALU.add, [gvar], [gvar])
                    act(gvar[:], gvar[:], AF.Sqrt, [gvar], [gvar])
                    P.op(P.dve, lambda h: h.reciprocal(out=gvar[:], in_=gvar[:]), r=[gvar], w=[gvar])
                    tt(P.dve, ynb[:, ti, :].rearrange("p (h v) -> p h v", v=64), yc[:].rearrange("p (h v) -> p h v", v=64), gvar[:, :].unsqueeze(2).to_broadcast([128, 8, 64]), ALU.mult, [yc, gvar], [ynb])

                def drive(*gens):
                    gens = [g for g in gens if g is not None]
                    while gens:
                        for g in list(gens):
                            try:
                                next(g)
                            except StopIteration:
                                gens.remove(g)
                drive(gen_D(0))
                for ti in range(4):
                    drive(gen_S(ti), gen_D(ti + 1) if ti < 3 else None)
                for j in range(4):
                    bb = bbank()
                    for ti in range(4):
                        tr(bb[:, ti * 128:(ti + 1) * 128], ynb[:, ti, j * 128:(j + 1) * 128], identb[:], [ynb, identb], [bb])
                    act(yt1[:], bb[:, 0:512], AF.Identity, [bb, lng, lnb], [yt1], scale=lng[:, j:j + 1], bias=lnb[:, j:j + 1])
                    tt(P.dve, yt1[:], yt1[:], bonT[j][:], ALU.add, [yt1, bonT[j]], [yt1])
                    tt(P.dve, yaT[:, j, :], yt1[:], gT[j][:], ALU.mult, [yt1, gT[j]], [yaT])
                    stq(yaT_d[j * 128:(j + 1) * 128, t0:t0 + ST], yaT[:, j, :], [yaT], [yaT_d], is_out=(dbg == "yaT_d"), q=P.pool)
            P.barrier()
        P.stack = gs
        if dbg == "yaT_d":
            P.finish()
            return nc, P

        with ExitStack() as ph, nc.allow_non_contiguous_dma(reason="tiny per-channel vectors"):
            P.stack = ph
            C0 = RW
            winu = P.tile("winu", [128, 8, 512], BF16); winv = P.tile("winv", [128, 8, 512], BF16)
            wing = P.tile("wing", [128, 8, 2048], BF16)
            woa = P.tile("woa", [128, 4, D], BF16); wob = P.tile("wob", [128, 4, D], BF16); wo = P.tile("wo", [128, 8, D], BF16)
            for dc in range(8):
                ldc(winu[:, dc, :], w_in[dc * 128:(dc + 1) * 128, C0:C0 + 512], [winu])
                ldc(winv[:, dc, :], w_in[dc * 128:(dc + 1) * 128, C0 + 512:C0 + 1024], [winv])
                ldc(wing[:, dc, 0:1024], w_in[dc * 128:(dc + 1) * 128, C0 + 1024:C0 + 2048], [wing])
                ldc(wing[:, dc, 1024:2048], w_in[dc * 128:(dc + 1) * 128, C0 + 2048:C0 + 3072], [wing])
                ldc(wo[:, dc, :], w_out[dc * 128:(dc + 1) * 128, :], [wo])
            for q in range(4):
                ldc(woa[:, q, :], w_out_a[q * 128:(q + 1) * 128, :], [woa])
                ldc(wob[:, q, :], w_out_b[q * 128:(q + 1) * 128, :], [wob])
            mU = P.tile("mU", [128, 128], F32)
            ms(P.pool, mU[:], 1.0, [mU])
            P.op(P.pool, lambda h: h.affine_select(out=mU[:], in_=mU[:], pattern=[[1, 128]], compare_op=ALU.is_ge, fill=0.0, base=0, channel_multiplier=-1), r=[mU], w=[mU])
            wsf = P.tile("wsf", [128, 8, 128], F32)
            for g_ in range(8):
                ld(wsf[:, g_, :], gmlp_ws[g_], [wsf])
            wsmT = P.tile("wsmT", [128, 8, 128], BF16)
            for g4 in range(2):
                pb = bank()
                for gg in range(4):
                    tr(pb[:, gg * 128:(gg + 1) * 128], wsf[:, g4 * 4 + gg, :], identf[:], [wsf, identf], [pb])
                tt(P.dve, wsmT[:, g4 * 4:(g4 + 1) * 4, :], pb[:, :].rearrange("p (a t) -> p a t", t=128), mU[:, :].unsqueeze(1).to_broadcast([128, 4, 128]), ALU.mult, [pb, mU], [wsmT])
            bsT = P.tile("bsT", [128, 4, 128], F32)
            for g_ in range(8):
                ld(bsT[(g_ % 2) * 64:(g_ % 2) * 64 + 64, g_ // 2, :], gmlp_bs[g_].partition_broadcast(64), [bsT])
            lngbc = P.tile("lngbc", [128, 512], F32); lnbbc = P.tile("lnbbc", [128, 512], F32)
            ld(lngbc[:], gmlp_ln_g.partition_broadcast(128), [lngbc]); ld(lnbbc[:], gmlp_ln_b.partition_broadcast(128), [lnbbc])

            xts = [P.tile(f"bxt{i}", [128, D], F32) for i in range(2)]
            hT = P.tile("bhT", [128, 8, 512], BF16)
            ssq = P.tile("bssq", [128, 4], F32); rstd = P.tile("brstd", [128, 4], F32)
            xn = P.tile("bxn", [128, 4, D], BF16)
            ntmp = (ssq, rstd, xn, None)
            uT = P.tile("uT", [128, 4, 512], BF16)
            vg = P.tile("vg", [128, 512], F32); vc = P.tile("vc", [128, 512], F32)
            vst = P.tile("vst", [128, 4], F32)
            vln = P.tile("vln", [128, 4, 512], BF16)
            ybT = P.tile("ybT", [128, 4, 512], BF16)
            gts = P.tile("gts", [128, 16, 512], BF16)
            yaTs = P.tile("yaTs", [128, 4, 512], BF16)
            mgT = P.tile("mgT", [128, 8, 512], BF16)
            t1 = P.tile("t1", [128, 512], F32); t2_ = P.tile("t2_", [128, 512], F32)
            xo = [P.tile(f"xo{i}", [128, D], F32) for i in range(2)]
            for si in range(nst):
                t0 = si * ST
                norm_T(xts, hT, x, si, s1, sh1, ntmp)
                for q in range(4):
                    ld(yaTs[:, q, :], yaT_d[q * 128:(q + 1) * 128, t0:t0 + ST], [yaTs], r=[yaT_d])
                for q in range(4):
                    pb = bank()
                    for dc in range(8):
                        mm(pb[:, :], winu[:, dc, q * 128:(q + 1) * 128], hT[:, dc, :], dc == 0, dc == 7, [winu, hT], [pb])
                    act(uT[:, q, :], pb[:, :], AF.Gelu, [pb], [uT])
                for ti in range(4):
                    pb = bank()
                    for dc in range(8):
                        mm(pb[:, :], hT[:, dc, ti * 128:(ti + 1) * 128], winv[:, dc, :], dc == 0, dc == 7, [winv, hT], [pb])
                    act(vg[:], pb[:, :], AF.Gelu, [pb], [vg, vst], accum_out=vst[:, 0:1])
                    ts(P.dve, vst[:, 1:2], vst[:, 0:1], 1.0 / 512, None, ALU.mult, None, [vst], [vst])
                    ts(P.dve, vc[:], vg[:], vst[:, 1:2], None, ALU.subtract, None, [vg, vst], [vc])
                    act(vg[:], vc[:], AF.Square, [vc], [vg, vst], accum_out=vst[:, 2:3])
                    ts(P.dve, vst[:, 3:4], vst[:, 2:3], 1.0 / 512, 1e-5, ALU.mult, ALU.add, [vst], [vst])
                    act(vst[:, 3:4], vst[:, 3:4], AF.Sqrt, [vst], [vst])
                    P.op(P.dve, lambda h: h.reciprocal(out=vst[:, 3:4], in_=vst[:, 3:4]), r=[vst], w=[vst])
                    stt(P.dve, vc[:], vc[:], vst[:, 3:4], lngbc[:], ALU.mult, ALU.mult, [vc, vst, lngbc], [vc])
                    tt(P.dve, vln[:, ti, :], vc[:], lnbbc[:], ALU.add, [vc, lnbbc], [vln])
                for q in range(4):
                    pb = bank()
                    for ti in range(4):
                        for gg in range(2):
                            g_ = 2 * q + gg
                            mm(pb[gg * 64:(gg + 1) * 64, ti * 128:(ti + 1) * 128], vln[:, ti, g_ * 64:(g_ + 1) * 64], wsmT[:, g_, :], True, True, [vln, wsmT], [pb])
                    tt(P.dve, t1[:].rearrange("p (a t) -> p a t", t=128), pb[:, :].rearrange("p (a t) -> p a t", t=128), bsT[:, q, :].unsqueeze(1).to_broadcast([128, 4, 128]), ALU.add, [pb, bsT], [t1])
                    tt(P.dve, ybT[:, q, :], t1[:], uT[:, q, :], ALU.mult, [t1, uT], [ybT])
                for q in range(16):
                    pb = bank()
                    for dc in range(8):
                        mm(pb[:, :], wing[:, dc, q * 128:(q + 1) * 128], hT[:, dc, :], dc == 0, dc == 7, [wing, hT], [pb])
                    act(gts[:, q, :], pb[:, :], AF.Sigmoid, [pb], [gts])
                for m in range(8):
                    pa = bank()
                    for q in range(4):
                        mm(pa[:, :], woa[:, q, m * 128:(m + 1) * 128], yaTs[:, q, :], q == 0, q == 3, [woa, yaTs], [pa])
                    pb = bank()
                    for q in range(4):
                        mm(pb[:, :], wob[:, q, m * 128:(m + 1) * 128], ybT[:, q, :], q == 0, q == 3, [wob, ybT], [pb])
                    tt(P.dve, t1[:], pa[:, :], gts[:, m, :], ALU.mult, [pa, gts], [t1])
                    tt(P.dve, t2_[:], pb[:, :], gts[:, 8 + m, :], ALU.mult, [pb, gts], [t2_])
                    tt(P.dve, mgT[:, m, :], t1[:], t2_[:], ALU.add, [t1, t2_], [mgT])
                for ti in range(4):
                    xt = xts[ti % 2]; xo_ = xo[ti % 2]
                    ld(xt[:], x[t0 + ti * 128:t0 + (ti + 1) * 128, :], [xt])
                    for hf in range(2):
                        pb = bank()
                        for m in range(8):
                            mm(pb[:, :], mgT[:, m, ti * 128:(ti + 1) * 128], wo[:, m, hf * 512:(hf + 1) * 512], m == 0, m == 7, [mgT, wo], [pb])
                        tt(P.dve, t1[:], pb[:, :], g1bc[:, hf * 512:(hf + 1) * 512], ALU.mult, [pb, g1bc], [t1])
                        tt(P.dve, xo_[:, hf * 512:(hf + 1) * 512], t1[:], xt[:, hf * 512:(hf + 1) * 512], ALU.add, [t1, xt], [xo_])
                    stq(x1_d[t0 + ti * 128:t0 + (ti + 1) * 128, :], xo_[:], [xo_], [x1_d], is_out=(dbg == "x1_d"), q=P.pool)
            P.barrier()
        P.stack = gs
        if dbg == "x1_d":
            P.finish()
            return nc, P

        NT = nst * 4
        dest8 = P.tile("dest8", [128, 64, 8], I32)
        w8 = P.tile("w8", [128, 64, 8], F32)
        idxw = P.tile("idxw", [128, NBLK], I32)
        w8b = [Buf(f"w8b{i}") for i in range(64)]; d8b = [Buf(f"d8b{i}") for i in range(64)]
        with ExitStack() as ph, nc.allow_non_contiguous_dma(reason="tiny per-channel vectors"):
            P.stack = ph
            rw = P.tile("rw", [128, 8, NE], BF16)
            sw1 = P.tile("sw1", [128, 8, 256], BF16); sw3 = P.tile("sw3", [128, 8, 256], BF16); sw2 = P.tile("sw2", [128, 2, D], BF16)
            for dc in range(8):
                ldc(rw[:, dc, :], router_w[dc * 128:(dc + 1) * 128, :], [rw])
                ldc(sw1[:, dc, :], shared_w1[dc * 128:(dc + 1) * 128, :], [sw1])
                ldc(sw3[:, dc, :], shared_w3[dc * 128:(dc + 1) * 128, :], [sw3])
            for fc in range(2):
                ldc(sw2[:, fc, :], shared_w2[fc * 128:(fc + 1) * 128, :], [sw2])
            rbias = P.tile("rbias", [128, NE], F32)
            ld(rbias[:], router_bias.partition_broadcast(128), [rbias])
            eoff = P.tile("eoff", [128, NE], F32)
            ustr = P.tile("ustr", [128, 128], BF16)
            onesb = P.tile("onesb", [128, 128], BF16)
            cp(P.dve, ustr[:], mU[:], [mU], [ustr]) if False else None
            uf = P.tile("uf", [128, 128], F32)
            ms(P.pool, uf[:], 1.0, [uf])
            P.op(P.pool, lambda h: h.affine_select(out=uf[:], in_=uf[:], pattern=[[1, 128]], compare_op=ALU.is_gt, fill=0.0, base=0, channel_multiplier=-1), r=[uf], w=[uf])
            cp(P.dve, ustr[:], uf[:], [uf], [ustr])
            ms(P.dve, onesb[:], 1.0, [onesb])
            basec = P.tile("basec", [128, NE], F32)
            ms(P.dve, basec[:], 0.0, [basec])

            xts = [P.tile(f"cxt{i}", [128, D], F32) for i in range(2)]
            hT = P.tile("chT", [128, 8, 512], BF16)
            ssq = P.tile("cssq", [128, 4], F32); rstd = P.tile("crstd", [128, 4], F32)
            xn = P.tile("cxn", [128, 4, D], BF16)
            ntmp = (ssq, rstd, xn, None)
            h2row = [P.tile(f"h2row{i}", [128, D], BF16) for i in range(2)]
            class _S:
                pass

            def mkset(n):
                S = _S()
                S.sc_ = P.tile(f"sc_{n}", [128, NE], F32); S.sel = P.tile(f"sel{n}", [128, NE], F32)
                S.m88 = P.tile(f"m88{n}", [128, 8, 8], F32); S.gs_ = P.tile(f"gs_{n}", [128, 8], F32)
                S.g8 = P.tile(f"g8{n}", [128, 8], F32); S.gmask = P.tile(f"gmask{n}", [128, 8], F32)
                S.selm = P.tile(f"selm{n}", [128, NE], F32); S.smask = P.tile(f"smask{n}", [128, NE], F32)
                S.smb = P.tile(f"smb{n}", [128, NE], BF16)
                S.wd = P.tile(f"wd{n}", [128, NE], F32); S.wsum = P.tile(f"wsum{n}", [128, 2], F32)
                S.key = P.tile(f"key{n}", [128, NE], F32); S.k8 = P.tile(f"k8{n}", [128, 8], F32)
                S.kz = P.tile(f"kz{n}", [128, 8], F32); S.jk = P.tile(f"jk{n}", [128, NE], F32)
                S.t2_ = P.tile(f"ct2{n}", [128, 512], F32)
                return S
            SS = [mkset(0), mkset(1)]
            hsT = P.tile("hsT", [128, 2, 512], BF16)
            t1 = P.tile("ct1", [128, 512], F32)
            xo = [P.tile(f"cxo{i}", [128, D], F32) for i in range(2)]

            def route(tsl, S):
                pb = bank()
                for dc in range(8):
                    mm(pb[:, 0:NE], hT[:, dc, tsl], rw[:, dc, :], dc == 0, dc == 7, [hT, rw], [pb])
                act(S.sc_[:], pb[:, 0:NE], AF.Sigmoid, [pb], [S.sc_])
                yield
                tt(P.dve, S.sel[:], S.sc_[:], rbias[:], ALU.add, [S.sc_, rbias], [S.sel])
                yield
                for g_ in range(8):
                    P.op(P.dve, lambda h: h.max(out=S.m88[:, g_, :], in_=S.sel[:, g_ * 32:(g_ + 1) * 32]), r=[S.sel], w=[S.m88])
                yield
                tt(P.dve, S.gs_[:], S.m88[:, :, 0], S.m88[:, :, 1], ALU.add, [S.m88], [S.gs_])
                yield
                P.op(P.dve, lambda h: h.max(out=S.g8[:], in_=S.gs_[:]), r=[S.gs_], w=[S.g8])
                yield
                ts(P.dve, S.gmask[:], S.gs_[:], S.g8[:, 3:4], None, ALU.is_ge, None, [S.gs_, S.g8], [S.gmask])
                yield
                stt(P.dve, S.selm[:].rearrange("p (g e) -> p g e", e=32), S.sel[:].rearrange("p (g e) -> p g e", e=32), 2.0, S.gmask[:, :].unsqueeze(2).to_broadcast([128, 8, 32]), ALU.add, ALU.mult, [S.sel, S.gmask], [S.selm])
                yield
                P.op(P.dve, lambda h: h.max(out=S.g8[:], in_=S.selm[:]), r=[S.selm], w=[S.g8])
                yield
                ts(P.dve, S.smask[:], S.selm[:], S.g8[:, 7:8], None, ALU.is_ge, None, [S.selm, S.g8], [S.smask])
                yield
                cp(P.dve, S.smb[:], S.smask[:], [S.smask], [S.smb])
                yield

            def drive(*gens):
                gens = [g for g in gens if g is not None]
                while gens:
                    for g in list(gens):
                        try:
                            next(g)
                        except StopIteration:
                            gens.remove(g)

            def gen_p1(ti, S):
                yield from route(slice(ti * 128, (ti + 1) * 128), S)
                pp = bank()
                mm(pp[:, 0:NE], onesb[:], S.smb[:], True, True, [onesb, S.smb], [pp])
                tt(P.dve, basec[:], basec[:], pp[:, 0:NE], ALU.add, [pp, basec], [basec])
                yield

            for si in range(nst):
                norm_T(xts, hT, x1_d.t, si, s2, sh2, ntmp)
                drive(gen_p1(0, SS[0]), gen_p1(1, SS[1]))
                drive(gen_p1(2, SS[0]), gen_p1(3, SS[1]))
            nblk = P.tile("nblk", [128, NE], F32); pends = P.tile("pends", [128, NE], F32)
            ones256 = P.tile("ones256", [128, NE], F32)
            ms(P.dve, nblk[:], 0.0, [nblk]); ms(P.dve, ones256[:], 1.0, [ones256])
            for m_ in range(T // BLK):
                stt(P.dve, nblk[:], basec[:], float(BLK * m_), nblk[:], ALU.is_gt, ALU.add, [basec, nblk], [nblk])
            ts(P.dve, nblk[:], nblk[:], float(BLK), None, ALU.mult, None, [nblk], [nblk])
            P.op(P.dve, lambda h: h.tensor_tensor_scan(out=pends[:], data0=ones256[:], data1=nblk[:], initial=0.0, op0=ALU.mult, op1=ALU.add), r=[ones256, nblk], w=[pends])
            tt(P.dve, eoff[:], pends[:], nblk[:], ALU.subtract, [pends, nblk], [eoff])
            ts(P.dve, eoff[:], eoff[:], 1.0, None, ALU.add, None, [eoff], [eoff])
            pcol = P.tile("pcol", [128, 2], F32)
            for c_ in range(2):
                pb = bank()
                tr(pb[:, 0:128], pends[:, c_ * 128:(c_ + 1) * 128], identf[:], [pends, identf], [pb])
                cp(P.dve, pcol[:, c_:c_ + 1], pb[:, 0:1], [pb], [pcol])
            iotab = P.tile("iotab", [128, NBLK], F32)
            P.op(P.pool, lambda h: h.iota(iotab[:], pattern=[[BLK, NBLK]], base=0, channel_multiplier=0, allow_small_or_imprecise_dtypes=True), w=[iotab])
            cmpb = P.tile("cmpb", [128, 2, NBLK], BF16)
            for c_ in range(2):
                ts(P.dve, cmpb[:, c_, :], iotab[:], pcol[:, c_:c_ + 1], None, ALU.is_ge, None, [iotab, pcol], [cmpb])
            pb = bank()
            for c_ in range(2):
                mm(pb[:, :], onesb[:], cmpb[:, c_, :], c_ == 0, c_ == 1, [onesb, cmpb], [pb])
            pidx = P.tile("pidx", [128, NBLK], F32)
            P.op(P.pool, lambda h: h.iota(pidx[:], pattern=[[0, NBLK]], base=0, channel_multiplier=1, allow_small_or_imprecise_dtypes=True), w=[pidx])
            ts(P.dve, iotab[:], pb[:, :], 128.0, None, ALU.mult, None, [pb], [iotab])
            tt(P.dve, idxw[:], iotab[:], pidx[:], ALU.add, [iotab, pidx], [idxw])
            ms(P.dve, basec[:], 0.0, [basec])

            def gen_p2(si, ti, S):
                t0 = si * ST
                tg = si * 4 + ti
                tsl = slice(ti * 128, (ti + 1) * 128)
                hr = h2row[ti % 2]
                bb = bbank()
                for dc in range(8):
                    tr(bb[:, dc * 128:(dc + 1) * 128], hT[:, dc, tsl], identb[:], [hT, identb], [bb])
                cp(P.dve, hr[:], bb[:, :], [bb], [hr])
                yield
                yield from route(tsl, S)
                stt(P.dve, S.wd[:], S.smask[:], 1.0, S.sc_[:], ALU.mult, ALU.mult, [S.smask, S.sc_], [S.wd, S.wsum], accum_out=S.wsum[:, 0:1])
                yield
                P.op(P.dve, lambda h: h.reciprocal(out=S.wsum[:, 1:2], in_=S.wsum[:, 0:1]), r=[S.wsum], w=[S.wsum])
                yield
                ts(P.dve, S.wd[:], S.wd[:], S.wsum[:, 1:2], 2.5, ALU.mult, ALU.mult, [S.wd, S.wsum], [S.wd])
                pp = bank()
                mm(pp[:, 0:NE], ustr[:], S.smb[:], True, True, [ustr, S.smb], [pp])
                mm(pp[:, NE:2 * NE], onesb[:], S.smb[:], True, True, [onesb, S.smb], [pp])
                tt(P.dve, S.key[:], pp[:, 0:NE], basec[:], ALU.add, [pp, basec], [S.key])
                tt(P.dve, basec[:], basec[:], pp[:, NE:2 * NE], ALU.add, [pp, basec], [basec])
                yield
                tt(P.dve, S.key[:], S.key[:], eoff[:], ALU.add, [S.key, eoff], [S.key])
                yield
                tt(P.dve, S.key[:], S.key[:], S.smask[:], ALU.mult, [S.key, S.smask], [S.key])
                yield
                P.op(P.dve, lambda h: h.max(out=S.k8[:], in_=S.key[:]), r=[S.key], w=[S.k8])
                yield
                for k in range(8):
                    stt(P.dve, S.jk[:], S.key[:], S.k8[:, k:k + 1], S.wd[:], ALU.is_equal, ALU.mult, [S.key, S.k8, S.wd], [S.jk, w8b[tg]], accum_out=w8[:, tg, k:k + 1])
                    yield
                ts(P.dve, S.kz[:], S.k8[:], 0.0, float(NSLOT), ALU.is_equal, ALU.mult, [S.k8], [S.kz])
                yield
                stt(P.dve, dest8[:, tg, :], S.k8[:], -1.0, S.kz[:], ALU.add, ALU.add, [S.k8, S.kz], [d8b[tg]])
                yield
                for k in range(8):
                    l = LP[lpi[0] % len(LP)]; lpi[0] += 1
                    P.dma(P.pool, l, lambda h: h.indirect_dma_start(out=xg_d.t, out_offset=bass.IndirectOffsetOnAxis(ap=dest8[:, tg, k:k + 1], axis=0), in_=hr[:], in_offset=None), r=[hr, d8b[tg]], w=[xg_d])
                yield
                xt = xts[ti % 2]; xo_ = xo[ti % 2]
                ld(xt[:], x1_d[t0 + ti * 128:t0 + (ti + 1) * 128, :], [xt])
                for hf in range(2):
                    pb = bank()
                    for fc in range(2):
                        mm(pb[:, :], hsT[:, fc, tsl], sw2[:, fc, hf * 512:(hf + 1) * 512], fc == 0, fc == 1, [hsT, sw2], [pb])
                    tt(P.dve, S.t2_[:], pb[:, :], g2bc[:, hf * 512:(hf + 1) * 512], ALU.mult, [pb, g2bc], [S.t2_])
                    yield
                    tt(P.dve, xo_[:, hf * 512:(hf + 1) * 512], S.t2_[:], xt[:, hf * 512:(hf + 1) * 512], ALU.add, [S.t2_, xt], [xo_])
                    yield
                stq(x1_d[t0 + ti * 128:t0 + (ti + 1) * 128, :], xo_[:], [xo_], [x1_d], q=P.act)
                yield

            for si in range(nst):
                norm_T(xts, hT, x1_d.t, si, s2, sh2, ntmp)
                for fc in range(2):
                    p1 = bank()
                    for dc in range(8):
                        mm(p1[:, :], sw1[:, dc, fc * 128:(fc + 1) * 128], hT[:, dc, :], dc == 0, dc == 7, [sw1, hT], [p1])
                    p3 = bank()
                    for dc in range(8):
                        mm(p3[:, :], sw3[:, dc, fc * 128:(fc + 1) * 128], hT[:, dc, :], dc == 0, dc == 7, [sw3, hT], [p3])
                    act(t1[:], p1[:, :], AF.Silu, [p1], [t1])
                    tt(P.dve, hsT[:, fc, :], t1[:], p3[:, :], ALU.mult, [t1, p3], [hsT])
                drive(gen_p2(si, 0, SS[0]), gen_p2(si, 1, SS[1]))
                drive(gen_p2(si, 2, SS[0]), gen_p2(si, 3, SS[1]))
            P.barrier()
            if dbg == "pB":
                P.finish()
                raise StopBuild()
        P.stack = gs

        with ExitStack() as ph:
            P.stack = ph
            w1v = exp_w1.rearrange("e (p c) f -> (e p) (c f)", c=8)
            w3v = exp_w3.rearrange("e (p c) f -> (e p) (c f)", c=8)
            w2v = exp_w2.rearrange("e (p c) d -> (e p) (c d)", c=2)
            xgt = [P.tile(f"xgt{i}", [128, 2, D], BF16) for i in range(3)]
            xgT = [P.tile(f"xgT{i}", [128, 8, BLK], BF16) for i in range(2)]
            w1b = [P.tile(f"w1b{i}", [128, 2048], BF16) for i in range(3)]
            w3b = [P.tile(f"w3b{i}", [128, 2048], BF16) for i in range(3)]
            w2b = [P.tile(f"w2b{i}", [128, 2048], BF16) for i in range(3)]
            hid = [P.tile(f"hid{i}", [128, 2, BLK], BF16) for i in range(2)]
            st1 = [P.tile(f"st1{i}", [128, BLK], F32) for i in range(2)]
            yrow = [P.tile(f"yrow{i}", [128, 2, D], BF16) for i in range(2)]
            LY = P.lanes(2, "ly")

            bc_reg = nc.gpsimd.to_reg(NE * 128 - 1)
            def wgather(dst, src, i_):
                l = LP[lpi[0] % len(LP)]; lpi[0] += 1
                P.dma(P.pool, l, lambda h: h.indirect_dma_start(out=dst[:], out_offset=None, in_=src, in_offset=bass.IndirectOffsetOnAxis(ap=idxw[:, i_:i_ + 1], axis=0), bounds_check=bc_reg, oob_is_err=False), r=[idxw], w=[dst])

            def c_loads(i_):
                i3 = i_ % 3
                ld(xgt[i3][:], xg_d[i_ * BLK:(i_ + 1) * BLK, :].rearrange("(b p) d -> p b d", p=128), [xgt[i3]], r=[xg_d])
                wgather(w1b[i3], w1v, i_); wgather(w3b[i3], w3v, i_); wgather(w2b[i3], w2v, i_)

            def c_T(i_):
                i3 = i_ % 3; i2 = i_ % 2
                xv = xgt[i3][:].rearrange("p b (q c) -> p b c q", c=8)
                for dc in range(8):
                    bb = bbank()
                    for b_ in range(2):
                        tr(bb[:, b_ * 128:(b_ + 1) * 128], xv[:, b_, dc, :], identb[:], [xgt[i3], identb], [bb])
                    cp(P.dve, xgT[i2][:, dc, :], bb[:, 0:BLK], [bb], [xgT[i2]])

            def c_H(i_):
                i3 = i_ % 3; i2 = i_ % 2
                w1r = w1b[i3][:].rearrange("p (c m two) -> p c two m", c=8, two=2)
                w3r = w3b[i3][:].rearrange("p (c m two) -> p c two m", c=8, two=2)
                for fc in range(2):
                    p1 = bank()
                    for dc in range(8):
                        mm(p1[:, 0:BLK], w1r[:, dc, fc, :], xgT[i2][:, dc, :], dc == 0, dc == 7, [w1b[i3], xgT[i2]], [p1])
                    p3 = bank()
                    for dc in range(8):
                        mm(p3[:, 0:BLK], w3r[:, dc, fc, :], xgT[i2][:, dc, :], dc == 0, dc == 7, [w3b[i3], xgT[i2]], [p3])
                    act(st1[fc][:], p1[:, 0:BLK], AF.Silu, [p1], [st1[fc]])
                    tt(P.dve, hid[i2][:, fc, :], st1[fc][:], p3[:, 0:BLK], ALU.mult, [st1[fc], p3], [hid[i2]])

            def c_Y(i_):
                i3 = i_ % 3; i2 = i_ % 2
                w2r = w2b[i3][:].rearrange("p (c d) -> p c d", c=2)
                for b_ in range(2):
                    for hf in range(2):
                        pb = bank()
                        for fc in range(2):
                            mm(pb[:, :], hid[i2][:, fc, b_ * 128:(b_ + 1) * 128], w2r[:, fc, hf * 512:(hf + 1) * 512], fc == 0, fc == 1, [hid[i2], w2b[i3]], [pb])
                        act(yrow[i2][:, b_, hf * 512:(hf + 1) * 512], pb[:, :], AF.Copy, [pb], [yrow[i2]])
                P.dma(P.act, LY[i2], lambda h: h.dma_start(out=yg_d[i_ * BLK:(i_ + 1) * BLK, :].rearrange("(b p) d -> p b d", p=128), in_=yrow[i2][:]), r=[yrow[i2]], w=[yg_d])

            nb_ = nblk_run
            c_loads(0)
            if nb_ > 1:
                c_loads(1)
            c_T(0)
            for i_ in range(nb_):
                if i_ + 1 < nb_:
                    c_T(i_ + 1)
                c_H(i_)
                if i_ >= 1:
                    c_Y(i_ - 1)
                if i_ + 2 < nb_:
                    c_loads(i_ + 2)
            c_Y(nb_ - 1)
            P.barrier()
            if dbg == "pC":
                P.finish()
                raise StopBuild()
        P.stack = gs

        with ExitStack() as ph:
            P.stack = ph
            nfbc = P.tile("nfbc", [128, D], F32)
            ld(nfbc[:], normf_g.partition_broadcast(128), [nfbc])
            xts = [P.tile(f"dxt{i}", [128, D], F32) for i in range(2)]
            gat = [P.tile(f"gat{i}", [128, D], BF16) for i in range(16)]
            acc = P.tile("acc", [128, D], F32)
            ot = [P.tile(f"ot{i}", [128, D], F32) for i in range(2)]
            fs = P.tile("fs", [128, 2], F32)
            jk2 = P.tile("jk2", [128, D], BF16)
            for tg in range(NT):
                xt = xts[tg % 2]; o_ = ot[tg % 2]
                ld(xt[:], x1_d[tg * 128:(tg + 1) * 128, :], [xt])
                for k in range(8):
                    gt = gat[(tg * 8 + k) % 16]
                    l = LP[lpi[0] % len(LP)]; lpi[0] += 1
                    P.dma(P.pool, l, lambda h: h.indirect_dma_start(out=gt[:], out_offset=None, in_=yg_d.t, in_offset=bass.IndirectOffsetOnAxis(ap=dest8[:, tg, k:k + 1], axis=0)), r=[yg_d, d8b[tg]], w=[gt])
                    if k == 0:
                        ts(P.dve, acc[:], gt[:], w8[:, tg, 0:1], None, ALU.mult, None, [gt, w8b[tg]], [acc])
                    else:
                        stt(P.dve, acc[:], gt[:], w8[:, tg, k:k + 1], acc[:], ALU.mult, ALU.add, [gt, w8b[tg], acc], [acc])
                tt(P.dve, acc[:], acc[:], g2bc[:], ALU.mult, [acc, g2bc], [acc])
                tt(P.dve, acc[:], acc[:], xt[:], ALU.add, [acc, xt], [acc])
                act(jk2[:], acc[:], AF.Square, [acc], [jk2, fs], accum_out=fs[:, 0:1])
                ts(P.dve, fs[:, 1:2], fs[:, 0:1], 1.0 / D, 1e-6, ALU.mult, ALU.add, [fs], [fs])
                act(fs[:, 1:2], fs[:, 1:2], AF.Sqrt, [fs], [fs])
                P.op(P.dve, lambda h: h.reciprocal(out=fs[:, 1:2], in_=fs[:, 1:2]), r=[fs], w=[fs])
                stt(P.dve, o_[:], acc[:], fs[:, 1:2], nfbc[:], ALU.mult, ALU.mult, [acc, fs, nfbc], [o_])
                stq(out[tg * 128:(tg + 1) * 128, :], o_[:], [o_], is_out=True, q=P.act)
            P.finish()
        P.stack = gs
        return nc, P


_NAMES = ["ada_w", "ada_b", "norm1_g", "norm2_g", "w_in", "tshift_mu", "rwkv_w0", "rwkv_w_up", "rwkv_a0", "rwkv_a_up",
          "rwkv_g_up", "rwkv_k_k", "rwkv_k_a", "rwkv_r_k", "rwkv_ln_g", "rwkv_ln_b", "gmlp_ln_g", "gmlp_ln_b", "gmlp_ws",
          "gmlp_bs", "w_out_a", "w_out_b", "w_out", "router_w", "router_bias", "exp_w1", "exp_w3", "exp_w2",
          "shared_w1", "shared_w3", "shared_w2"]


def kernel(**inputs):
    nc, _ = build()
    shared = {}
    for k in _NAMES:
        a = np.asarray(inputs[k], dtype=np.float32)[0]
        if k == "rwkv_r_k":
            a = a.reshape(512)
        shared[k] = np.ascontiguousarray(a)
    shared["normf_g"] = np.ascontiguousarray(np.asarray(inputs["normf_g"], dtype=np.float32))
    x = np.asarray(inputs["x"], dtype=np.float32)
    c = np.asarray(inputs["c"], dtype=np.float32)
    in_maps = []
    for b in range(8):
        m = dict(shared)
        m["x"] = np.ascontiguousarray(x[b])
        m["c"] = np.ascontiguousarray(c[b:b + 1])
        in_maps.append(m)
    res = run_bass_kernel_spmd(nc, in_maps, core_ids=list(range(8)))
    return np.stack([np.asarray(r["out"], dtype=np.float32) for r in res.results], axis=0)
```
